# Optimizing a Trainium2 kernel written in Bass

```python
import math
import jax, jax.numpy as jnp
from jax import lax
import numpy as np

D_MODEL = 1024
BATCH = 4
SEQ = 4096
DEPTH = 2

N_META = 16
EPS = 1e-6
ATT_HEADS = 4
ATT_QK_DIM = 64
ATT_V_DIM = 2 * ATT_QK_DIM
ATT_QK_COLS = 2 * ATT_HEADS * ATT_QK_DIM
ATT_WIDTH = ATT_HEADS * ATT_V_DIM
Q_BLOCK = 128
REL_BUCKETS = 32
REL_MAX_DIST = 128
CONV_WIDTH = 512
CONV_K = 3
HGRN_HEADS = 4
HGRN_DK = 128
HGRN_DV = 128
HGRN_WIDTH = HGRN_HEADS * HGRN_DV
HGRN_CHUNK = 64
N_BRANCH = 3
BRANCH_WIDTH = 512
IN_SPLITS = (ATT_QK_COLS, ATT_QK_COLS, ATT_WIDTH,
             CONV_WIDTH, CONV_WIDTH, CONV_WIDTH,
             HGRN_HEADS * HGRN_DK, HGRN_HEADS * HGRN_DK, HGRN_WIDTH, HGRN_WIDTH,
             N_BRANCH * D_MODEL)
IN_COLS = sum(IN_SPLITS)
D_FF_DENSE = 2816
N_EXPERTS = 8
TOP_K = 2
D_FF_EXPERT = 3584
N_DENSE = (DEPTH + 1) // 2
N_MOE = DEPTH // 2

kernel_name = "hybrid_diffattn_shortconv_hgrn2_moe"


def rms_norm(x, gain):
    xf = x.astype(jnp.float32)
    y = xf * lax.rsqrt(jnp.mean(xf * xf, axis=-1, keepdims=True) + EPS)
    return (y * gain.astype(jnp.float32)).astype(x.dtype)


def rel_bucket(q_pos, k_pos):
    n = jnp.maximum(q_pos[:, None] - k_pos[None, :], 0)
    max_exact = REL_BUCKETS // 2
    nf = jnp.maximum(n, 1).astype(jnp.float32)
    large = max_exact + (jnp.log(nf / max_exact) / math.log(REL_MAX_DIST / max_exact)
                         * (REL_BUCKETS - max_exact)).astype(jnp.int32)
    large = jnp.minimum(large, REL_BUCKETS - 1)
    return jnp.where(n < max_exact, n, large)


def diff_attn_block(q, q_pos, k, v, k_pos, rel_bias, lam):
    bias = jnp.moveaxis(rel_bias[rel_bucket(q_pos, k_pos)], -1, 0).astype(jnp.float32)
    logits = jnp.einsum('bmhqd,bmhkd->bmhqk', q, k, preferred_element_type=jnp.float32)
    logits = logits * (ATT_QK_DIM ** -0.5) + bias
    causal = k_pos[None, :] <= q_pos[:, None]
    probs = jax.nn.softmax(jnp.where(causal, logits, -jnp.inf), axis=-1)
    attn = probs[:, 0] - lam * probs[:, 1]
    return jnp.einsum('bhqk,bhkd->bhqd', attn.astype(v.dtype), v)


def diff_attention(q, k, v, q_gain, k_gain, lam_params, sub_gain, rel_bias, layer):
    bsz, t_len, _ = q.shape
    n_real = t_len - N_META
    n_blk = n_real // Q_BLOCK
    q = rms_norm(q.reshape(bsz, t_len, 2, ATT_HEADS, ATT_QK_DIM), q_gain).transpose(0, 2, 3, 1, 4)
    k = rms_norm(k.reshape(bsz, t_len, 2, ATT_HEADS, ATT_QK_DIM), k_gain).transpose(0, 2, 3, 1, 4)
    v = v.reshape(bsz, t_len, ATT_HEADS, ATT_V_DIM).transpose(0, 2, 1, 3)
    lam_init = 0.8 - 0.6 * math.exp(-0.3 * layer)
    lp = lam_params.astype(jnp.float32)
    lam = jnp.exp(jnp.sum(lp[0] * lp[1])) - jnp.exp(jnp.sum(lp[2] * lp[3])) + lam_init
    pos = jnp.arange(t_len)
    o_meta = diff_attn_block(q[:, :, :, :N_META], pos[:N_META], k[:, :, :, :N_META],
                             v[:, :, :N_META], pos[:N_META], rel_bias, lam)
    q_blocks = jnp.moveaxis(q[:, :, :, N_META:].reshape(bsz, 2, ATT_HEADS, n_blk, Q_BLOCK, ATT_QK_DIM), 3, 0)
    qpos_blocks = pos[N_META:].reshape(n_blk, Q_BLOCK)
    o_real = lax.map(lambda a: diff_attn_block(a[0], a[1], k, v, pos, rel_bias, lam), (q_blocks, qpos_blocks))
    o_real = jnp.moveaxis(o_real, 0, 2).reshape(bsz, ATT_HEADS, n_real, ATT_V_DIM)
    o = jnp.concatenate([o_meta, o_real], axis=2)
    o = rms_norm(o, sub_gain) * (1.0 - lam_init)
    return o.transpose(0, 2, 1, 3).reshape(bsz, t_len, ATT_WIDTH)


def short_conv(b, c, h, conv_w):
    z = c * h
    y = lax.conv_general_dilated(z, conv_w[:, None, :].astype(z.dtype), window_strides=(1,),
                                 padding=[(CONV_K - 1, 0)], dimension_numbers=('NWC', 'WIO', 'NWC'),
                                 feature_group_count=CONV_WIDTH)
    return b * y


def gla_chunk(state, q, k, v, log_f):
    c_len = q.shape[2]
    g_cum = jnp.cumsum(log_f, axis=2)
    o_inter = jnp.einsum('bhcd,bhde->bhce', q * jnp.exp(g_cum), state)
    diff = g_cum[:, :, :, None, :] - g_cum[:, :, None, :, :]
    mask = jnp.tril(jnp.ones((c_len, c_len), dtype=bool))[:, :, None]
    decay = jnp.exp(jnp.where(mask, diff, -jnp.inf))
    scores = jnp.einsum('bhtd,bhsd,bhtsd->bhts', q, k, decay)
    o_intra = jnp.einsum('bhts,bhse->bhte', scores, v)
    g_last = g_cum[:, :, -1, :]
    new_state = jnp.exp(g_last)[..., None] * state + jnp.einsum(
        'bhsd,bhse->bhde', k * jnp.exp(g_last[:, :, None, :] - g_cum), v)
    return new_state, o_inter + o_intra


def hgrn2(q, f_pre, i, g, lb, out_gain):
    bsz, t_len, _ = q.shape
    out_dtype = q.dtype
    n_real = t_len - N_META
    n_chunk = n_real // HGRN_CHUNK
    f32 = jnp.float32
    qh = jax.nn.silu(q.astype(f32)).reshape(bsz, t_len, HGRN_HEADS, HGRN_DK)
    z = f_pre.astype(f32).reshape(bsz, t_len, HGRN_HEADS, HGRN_DK)
    lb = lb.astype(f32).reshape(HGRN_HEADS, HGRN_DK)
    log_f = jnp.logaddexp(jnp.log(lb), jnp.log1p(-lb) + jax.nn.log_sigmoid(z))
    kh = (1.0 - lb) * jax.nn.sigmoid(-z)
    vh = i.astype(f32).reshape(bsz, t_len, HGRN_HEADS, HGRN_DV)
    qh, kh, vh, log_f = (a.transpose(0, 2, 1, 3) for a in (qh, kh, vh, log_f))
    state0 = jnp.zeros((bsz, HGRN_HEADS, HGRN_DK, HGRN_DV), f32)
    state, o_meta = gla_chunk(state0, qh[:, :, :N_META], kh[:, :, :N_META],
                              vh[:, :, :N_META], log_f[:, :, :N_META])

    def to_chunks(a):
        return jnp.moveaxis(a[:, :, N_META:].reshape(bsz, HGRN_HEADS, n_chunk, HGRN_CHUNK, a.shape[-1]), 2, 0)

    _, o_real = lax.scan(lambda s, xs: gla_chunk(s, *xs), state,
                         (to_chunks(qh), to_chunks(kh), to_chunks(vh), to_chunks(log_f)))
    o_real = jnp.moveaxis(o_real, 0, 2).reshape(bsz, HGRN_HEADS, n_real, HGRN_DV)
    o = jnp.concatenate([o_meta, o_real], axis=2).transpose(0, 2, 1, 3)
    gate = g.astype(f32).reshape(bsz, t_len, HGRN_HEADS, HGRN_DV)
    o = rms_norm(o, out_gain) * jax.nn.silu(gate)
    return o.reshape(bsz, t_len, HGRN_WIDTH).astype(out_dtype)


def swiglu(h, w_gate, w_up, w_down):
    return (jax.nn.silu(h @ w_gate) * (h @ w_up)) @ w_down


def moe_swiglu(h, router_w, w_gate, w_up, w_down):
    logits = jnp.einsum('btd,de->bte', h, router_w).astype(jnp.float32)
    top_logits, top_idx = lax.top_k(logits, TOP_K)
    top_w = jax.nn.softmax(top_logits, axis=-1)
    combine = jnp.sum(jax.nn.one_hot(top_idx, N_EXPERTS, dtype=jnp.float32) * top_w[..., None], axis=-2)
    combine = combine.astype(h.dtype)
    out = jnp.zeros_like(h)
    for e in range(N_EXPERTS):
        out = out + combine[..., e:e + 1] * swiglu(h, w_gate[e], w_up[e], w_down[e])
    return out


def setup_inputs(seed: int = 0) -> dict:
    key = jax.random.key(seed)
    ks = iter(jax.random.split(key, 32))

    def nrm(shape, scale):
        return scale * jax.random.normal(next(ks), shape, jnp.float32)

    def gain(shape):
        return 1.0 + nrm(shape, 0.05)

    return {
        "x": nrm((BATCH, SEQ, D_MODEL), 1.0),
        "meta_tokens": nrm((N_META, D_MODEL), 1.0),
        "norm1_gain": gain((DEPTH, D_MODEL)),
        "norm2_gain": gain((DEPTH, D_MODEL)),
        "w_in": nrm((DEPTH, D_MODEL, IN_COLS), D_MODEL ** -0.5),
        "q_norm_gain": gain((DEPTH, ATT_QK_DIM)),
        "k_norm_gain": gain((DEPTH, ATT_QK_DIM)),
        "diff_lambda": nrm((DEPTH, 4, ATT_QK_DIM), 0.1),
        "attn_sub_gain": gain((DEPTH, ATT_V_DIM)),
        "rel_bias": nrm((REL_BUCKETS, ATT_HEADS), 0.5),
        "conv_w": nrm((DEPTH, CONV_K, CONV_WIDTH), CONV_K ** -0.5),
        "hgrn_lb_logits": nrm((DEPTH, HGRN_HEADS * HGRN_DK), 1.0),
        "hgrn_out_gain": gain((DEPTH, HGRN_DV)),
        "w_branch": nrm((DEPTH, N_BRANCH, BRANCH_WIDTH, D_MODEL), BRANCH_WIDTH ** -0.5),
        "w_out": nrm((DEPTH, D_MODEL, D_MODEL), D_MODEL ** -0.5),
        "ffn_w_gate": nrm((N_DENSE, D_MODEL, D_FF_DENSE), D_MODEL ** -0.5),
        "ffn_w_up": nrm((N_DENSE, D_MODEL, D_FF_DENSE), D_MODEL ** -0.5),
        "ffn_w_down": nrm((N_DENSE, D_FF_DENSE, D_MODEL), D_FF_DENSE ** -0.5),
        "router_w": nrm((N_MOE, D_MODEL, N_EXPERTS), D_MODEL ** -0.5),
        "moe_w_gate": nrm((N_MOE, N_EXPERTS, D_MODEL, D_FF_EXPERT), D_MODEL ** -0.5),
        "moe_w_up": nrm((N_MOE, N_EXPERTS, D_MODEL, D_FF_EXPERT), D_MODEL ** -0.5),
        "moe_w_down": nrm((N_MOE, N_EXPERTS, D_FF_EXPERT, D_MODEL), D_FF_EXPERT ** -0.5),
    }


def reference(x, meta_tokens, norm1_gain, norm2_gain, w_in, q_norm_gain, k_norm_gain,
              diff_lambda, attn_sub_gain, rel_bias, conv_w, hgrn_lb_logits, hgrn_out_gain,
              w_branch, w_out, ffn_w_gate, ffn_w_up, ffn_w_down, router_w,
              moe_w_gate, moe_w_up, moe_w_down):
    bsz = x.shape[0]
    meta = jnp.broadcast_to(meta_tokens.astype(x.dtype)[None], (bsz, N_META, D_MODEL))
    hs = jnp.concatenate([meta, x], axis=1)
    t_len = hs.shape[1]
    lb_all = jnp.cumsum(jax.nn.softmax(hgrn_lb_logits.astype(jnp.float32), axis=0), axis=0)
    lb_all = lb_all - lb_all[0]
    split_at = [int(s) for s in np.cumsum(IN_SPLITS)[:-1]]
    for layer in range(DEPTH):
        hn = rms_norm(hs, norm1_gain[layer])
        proj = jnp.einsum('btd,dn->btn', hn, w_in[layer])
        (a_q, a_k, a_v, c_b, c_c, c_h, r_q, r_f, r_i, r_g, gates) = jnp.split(proj, split_at, axis=-1)
        u_att = diff_attention(a_q, a_k, a_v, q_norm_gain[layer], k_norm_gain[layer],
                               diff_lambda[layer], attn_sub_gain[layer], rel_bias, layer)
        u_conv = short_conv(c_b, c_c, c_h, conv_w[layer])
        u_hgrn = hgrn2(r_q, r_f, r_i, r_g, lb_all[layer], hgrn_out_gain[layer])
        branches = jnp.stack([u_att, u_conv, u_hgrn], axis=2)
        up = jnp.einsum('btnw,nwd->btnd', branches, w_branch[layer])
        gate = jax.nn.sigmoid(gates.reshape(bsz, t_len, N_BRANCH, D_MODEL))
        mixed = jnp.sum(gate * up, axis=2)
        hs = hs + jnp.einsum('btd,de->bte', mixed, w_out[layer])
        hn2 = rms_norm(hs, norm2_gain[layer])
        if layer % 2 == 0:
            j = layer // 2
            ffn = swiglu(hn2, ffn_w_gate[j], ffn_w_up[j], ffn_w_down[j])
        else:
            j = layer // 2
            ffn = moe_swiglu(hn2, router_w[j], moe_w_gate[j], moe_w_up[j], moe_w_down[j])
        hs = hs + ffn
    return hs[:, N_META:]
```

```python
import contextlib
import math
import numpy as np
import ml_dtypes
import concourse.bass as bass
import concourse.mybir as mybir
from concourse.bass_utils import run_bass_kernel_spmd

F32 = mybir.dt.float32
BF16 = mybir.dt.bfloat16
AF = mybir.ActivationFunctionType
ALU = mybir.AluOpType
AX = mybir.AxisListType

D = 1024
T_ALL = 4112
N_META = 16
EPS = 1e-6
DEBUG = False
FDEBUG = False
HG_BF16_STATE = False
DEBUG_G = 0
ENGINES = ("tensor", "vector", "scalar", "gpsimd", "sync")


class Buf:
    __slots__ = ("name", "last_w", "readers")

    def __init__(self, name):
        self.name = name
        self.last_w = None
        self.readers = []


class Op:
    __slots__ = ("eng", "fn", "deps", "is_dma", "dma_key", "signal", "sig_cnt", "idx", "dma_deps", "epoch")

    def __init__(self, eng, fn, is_dma=False, dma_key=None):
        self.eng = eng
        self.fn = fn
        self.deps = []
        self.dma_deps = []
        self.is_dma = is_dma
        self.dma_key = dma_key
        self.signal = False
        self.sig_cnt = 0


class Sched:
    def __init__(self, nc):
        self.nc = nc
        self.ops = []
        self.dma_counts = {}
        self.all_bufs = []
        self.epoch = 0

    def buf(self, name="b"):
        b = Buf(name)
        self.all_bufs.append(b)
        return b

    def add(self, eng, fn, reads=(), writes=(), dma_key=None):
        is_dma = dma_key is not None
        op = Op(eng, fn, is_dma, dma_key)
        op.idx = len(self.ops)
        op.epoch = self.epoch
        deps = {}

        def add_dep(p, kind):
            if p is None or p is op:
                return
            if p.eng == eng and not p.is_dma and not is_dma:
                if eng == "tensor" or kind != "RAW":
                    return
            deps[p.idx] = p

        for b in reads:
            add_dep(b.last_w, "RAW")
        for b in writes:
            add_dep(b.last_w, "WAW")
            for r in b.readers:
                add_dep(r, "WAR")
        for b in reads:
            b.readers.append(op)
        for b in writes:
            b.last_w = op
            b.readers = []
        for p in deps.values():
            if p.is_dma:
                op.dma_deps.append((p.dma_key, self.dma_counts[p.dma_key]))
            else:
                p.signal = True
                op.deps.append(p)
        if is_dma:
            self.dma_counts[dma_key] = self.dma_counts.get(dma_key, 0) + 16
        self.ops.append(op)
        return op

    def fence(self, new_epoch=False):
        last = {}
        for op in self.ops:
            if not op.is_dma and op.fn is not None:
                last[op.eng] = op
        dma_now = dict(self.dma_counts)
        for e in ENGINES:
            op = Op(e, None)
            op.idx = len(self.ops)
            op.epoch = self.epoch
            for pe, p in last.items():
                if pe != e:
                    p.signal = True
                    op.deps.append(p)
            for k, c in dma_now.items():
                op.dma_deps.append((k, c))
            self.ops.append(op)
        for b in self.all_bufs:
            b.last_w = None
            b.readers = []
        if new_epoch:
            self.epoch += 1

    def finalize(self, final_dma_keys=()):
        nc = self.nc
        cnt = {}
        for op in self.ops:
            if op.signal and not op.is_dma:
                k = (op.epoch, op.eng)
                cnt[k] = cnt.get(k, 0) + 1
                op.sig_cnt = cnt[k]
        self.max_counts = max(cnt.values()) if cnt else 0
        with contextlib.ExitStack() as st:
            sems = {(ep, e): st.enter_context(nc.semaphore(f"s_{e}{ep}")) for ep in range(self.epoch + 1) for e in ENGINES
                    if (ep, e) in cnt}
            dsems = {k: st.enter_context(nc.semaphore(f"d_{k}")) for k in self.dma_counts}
            block = st.enter_context(nc.Block())
            per_eng = {e: [op for op in self.ops if op.eng == e] for e in ENGINES}

            def emit(eng_name, eng):
                waited = {}
                for op in per_eng[eng_name]:
                    for p in op.deps:
                        key = ("c", p.epoch, p.eng)
                        if waited.get(key, 0) < p.sig_cnt:
                            eng.wait_ge(sems[(p.epoch, p.eng)], p.sig_cnt)
                            waited[key] = p.sig_cnt
                    for (k, c) in op.dma_deps:
                        key = ("d", k)
                        if waited.get(key, 0) < c:
                            eng.wait_ge(dsems[k], c)
                            waited[key] = c
                    if op.fn is None:
                        continue
                    ins = op.fn(eng)
                    if op.is_dma:
                        ins.then_inc(dsems[op.dma_key], 16)
                    elif op.signal:
                        ins.then_inc(sems[(op.epoch, eng_name)], 1)
                if eng_name == "sync":
                    for k in final_dma_keys:
                        eng.wait_ge(dsems[k], self.dma_counts[k])

            block.tensor(lambda e: emit("tensor", e))
            block.vector(lambda e: emit("vector", e))
            block.scalar(lambda e: emit("scalar", e))
            block.gpsimd(lambda e: emit("gpsimd", e))
            block.sync(lambda e: emit("sync", e))


class Ctx:
    ARENA_BYTES = 212480

    def __init__(self, nc, st):
        self.nc = nc
        self.st = st
        self.S = Sched(nc)
        self.arena = st.enter_context(nc.sbuf_tensor("arena", [128, self.ARENA_BYTES // 4], F32))
        self.arena_bf = self.arena.bitcast(BF16)
        self.off = 0
        self.psum = []
        for i in range(8):
            t = st.enter_context(nc.psum_tensor(f"ps{i}", [128, 512], F32))
            self.psum.append((t, self.S.buf(f"ps{i}")))
        self.pi = 0
        self.pool = list(range(8))
        self.tmps = {}

    def alloc(self, nelem, dt):
        sz = 4 if dt == F32 else 2
        nbytes = (nelem * sz + 3) // 4 * 4
        assert self.off + nbytes <= self.ARENA_BYTES, f"arena overflow {self.off + nbytes}"
        o = self.off
        self.off += nbytes
        if dt == F32:
            return self.arena[:, o // 4: o // 4 + nelem]
        return self.arena_bf[:, o // 2: o // 2 + nelem]

    def mark(self):
        return self.off

    def reset(self, m):
        self.off = m

    def ps(self):
        self.pi = (self.pi + 1) % len(self.pool)
        return self.psum[self.pool[self.pi]]

    def reserve(self, k):
        r = [self.psum[i] for i in self.pool[-k:]]
        self.pool = self.pool[:-k]
        self.pi = 0
        return r

    def release(self):
        self.pool = list(range(8))
        self.pi = 0

    def tmp(self, kind, nelem, dt, n=2):
        if kind not in self.tmps:
            self.tmps[kind] = [[(self.alloc(nelem, dt), self.S.buf(f"{kind}{i}")) for i in range(n)], 0]
        lst, i = self.tmps[kind]
        self.tmps[kind][1] = (i + 1) % len(lst)
        return lst[i]

    def drop_tmps(self):
        self.tmps = {}


GS = 512


def make_groups(n, gs=None):
    gs = gs or GS
    g = []
    t = 0
    while t < n:
        w = min(gs, n - t)
        g.append((t, w))
        t += w
    return g


def kview(ap, p=128):
    return ap.rearrange("(kc p) n -> p kc n", p=p)


def emit_consts(C):
    S = C.S
    k = {}
    k["ones"] = C.alloc(128, BF16)
    k["B_ones"] = S.buf("ones")
    S.add("vector", lambda e: e.memset(k["ones"], 1.0 / 1024.0), writes=[k["B_ones"]])
    k["eps"] = C.alloc(1, F32)
    k["B_eps"] = S.buf("eps")
    S.add("vector", lambda e: e.memset(k["eps"], EPS), writes=[k["B_eps"]])
    return k


def emit_rmsnorm_group(C, K, hs, N, B_hs, gain, B_gain, out, out_stride, B_out, gi, t0, w, out_t0):
    S = C.S
    ps, Bps = C.ps()
    for c in range(8):
        sq, Bsq = C.tmp("sq", 512, BF16, 2)
        S.add("scalar", lambda e, c=c, sq=sq: e.activation(out=sq[:, :w], in_=hs[:, c * N + t0: c * N + t0 + w], func=AF.Square),
              reads=[B_hs[c][gi]], writes=[Bsq])
        S.add("tensor", lambda e, c=c, sq=sq: e.matmul(ps[:, :w], K["ones"], sq[:, :w], start=(c == 0), stop=(c == 7)),
              reads=[Bsq, K["B_ones"]], writes=[Bps])
    rstd, Brstd = C.tmp("rstd", 512, F32, 2)
    S.add("scalar", lambda e: e.activation(out=rstd[:, :w], in_=ps[:, :w], func=AF.Ln, bias=K["eps"], scale=1.0),
          reads=[Bps, K["B_eps"]], writes=[Brstd])
    S.add("scalar", lambda e: e.activation(out=rstd[:, :w], in_=rstd[:, :w], func=AF.Exp, scale=-0.5),
          reads=[Brstd], writes=[Brstd])
    for c in range(8):
        S.add("vector", lambda e, c=c: e.scalar_tensor_tensor(
            out=out[:, c * out_stride + out_t0: c * out_stride + out_t0 + w], in0=hs[:, c * N + t0: c * N + t0 + w],
            scalar=gain[:, c:c + 1], in1=rstd[:, :w], op0=ALU.mult, op1=ALU.mult),
            reads=[B_hs[c][gi], Brstd, B_gain], writes=[B_out[c]])


def build_phase2(N, moe):
    nc = bass.Bass("TRN2", target_bir_lowering=False)
    dr = {}
    dr["hsT"] = nc.dram_tensor("hsT", [D, N], F32, kind="ExternalInput").ap()
    dr["uT"] = nc.dram_tensor("uT", [1536, N], BF16, kind="ExternalInput").ap()
    dr["wgate"] = nc.dram_tensor("wgate", [D, 3072], F32, kind="ExternalInput").ap()
    dr["wbr"] = nc.dram_tensor("wbr", [1536, D], F32, kind="ExternalInput").ap()
    dr["wout"] = nc.dram_tensor("wout", [D, D], F32, kind="ExternalInput").ap()
    dr["gains"] = nc.dram_tensor("gains", [128, 16], F32, kind="ExternalInput").ap()
    if moe:
        FF = 3584
        dr["fg"] = nc.dram_tensor("fg", [8, D, FF], F32, kind="ExternalInput").ap()
        dr["fu"] = nc.dram_tensor("fu", [8, D, FF], F32, kind="ExternalInput").ap()
        dr["fd"] = nc.dram_tensor("fd", [8, FF, D], F32, kind="ExternalInput").ap()
        dr["wr"] = nc.dram_tensor("wr", [D, 8], F32, kind="ExternalInput").ap()
        dr["sel"] = nc.dram_tensor("sel", [8, 8 * 128], F32, kind="ExternalInput").ap()
        dr["ident"] = nc.dram_tensor("ident", [128, 128], F32, kind="ExternalInput").ap()
    else:
        FF = 2816
        dr["fg"] = nc.dram_tensor("fg", [1, D, FF], F32, kind="ExternalInput").ap()
        dr["fu"] = nc.dram_tensor("fu", [1, D, FF], F32, kind="ExternalInput").ap()
        dr["fd"] = nc.dram_tensor("fd", [1, FF, D], F32, kind="ExternalInput").ap()
    dr["out"] = nc.dram_tensor("hsT_out", [D, N], F32, kind="ExternalOutput").ap()
    if DEBUG:
        dr["dbg1"] = nc.dram_tensor("dbg1", [128, 8 * 512], BF16, kind="ExternalOutput").ap()
        dr["dbg2"] = nc.dram_tensor("dbg2", [D, N], F32, kind="ExternalOutput").ap()
        dr["dbg3"] = nc.dram_tensor("dbg3", [128, 8 * 512], BF16, kind="ExternalOutput").ap()
        dr["dbg4"] = nc.dram_tensor("dbg4", [128, 512], F32, kind="ExternalOutput").ap()
        dr["dbg5"] = nc.dram_tensor("dbg5", [128, 512], F32, kind="ExternalOutput").ap()

    with contextlib.ExitStack() as st:
        C = Ctx(nc, st)
        K = emit_consts(C)
        emit_phase2(C, K, dr, N, moe)
        C.S.finalize(final_dma_keys=["out"])
        print("phase2 ops", len(C.S.ops), "sig", C.S.max_counts)
    return nc


def emit_phase2(C, K, dr, N, moe):
    S = C.S
    FF = 3584 if moe else 2816
    m_base = C.mark()
    groups = make_groups(N)
    NG = len(groups)
    hs = C.alloc(8 * N, F32)
    B_hs = [[S.buf(f"hs{c}_{g}") for g in range(NG)] for c in range(8)]
    gains = C.alloc(16, F32)
    B_gain = S.buf("gains")
    S.add("sync", lambda e: e.dma_start(out=gains, in_=dr["gains"]), writes=[B_gain], dma_key="gains")
    selv = None
    if isinstance(dr["hsT"], tuple):
        selv = C.alloc(2, F32)
        B_selv = S.buf("selv")
        S.add("sync", lambda e: e.dma_start(out=selv, in_=dr["selv"]), writes=[B_selv], dma_key="selv")
        hvA, hvB = kview(dr["hsT"][0]), kview(dr["hsT"][1])
        m_tmp = C.mark()
        tb = C.alloc(N, F32)
        B_tb = S.buf("tb")

        def hs_blend(c):
            S.add("sync", lambda e: e.dma_start(out=hs[:, c * N:(c + 1) * N], in_=hvA[:, c, :]), writes=B_hs[c], dma_key="hs_in")
            S.add("sync", lambda e: e.dma_start(out=tb, in_=hvB[:, c, :]), writes=[B_tb], dma_key="hs_inb")
            S.add("vector", lambda e: e.tensor_scalar(tb, tb, selv[:, 1:2], None, ALU.mult), reads=[B_tb, B_selv], writes=[B_tb])
            S.add("vector", lambda e: e.scalar_tensor_tensor(out=hs[:, c * N:(c + 1) * N], in0=hs[:, c * N:(c + 1) * N], scalar=selv[:, 0:1],
                                                             in1=tb, op0=ALU.mult, op1=ALU.add), reads=B_hs[c] + [B_tb, B_selv], writes=B_hs[c])
        for c in range(8):
            hs_blend(c)
        S.fence()
        C.reset(m_tmp)
    else:
        hsv = kview(dr["hsT"])
        for c in range(8):
            S.add("sync", lambda e, c=c: e.dma_start(out=hs[:, c * N:(c + 1) * N], in_=hsv[:, c, :]),
                  writes=B_hs[c], dma_key="hs_in")
    m_persist = C.mark()

    wg = C.alloc(8 * 3072, BF16)
    wb = C.alloc(12 * 1024, BF16)
    wo = C.alloc(8 * 1024, BF16)
    B_wg, B_wb, B_wo = S.buf("wg"), S.buf("wb"), S.buf("wo")
    wgv = kview(dr["wgate"])
    for b in range(3):
        S.add("gpsimd", lambda e, b=b: e.dma_start(
            out=wg.rearrange("p (k n) -> p k n", k=8)[:, :, b * 1024:(b + 1) * 1024], in_=wgv[:, :, b * 1024:(b + 1) * 1024]),
            writes=[B_wg], dma_key="wg")
    S.add("gpsimd", lambda e: e.dma_start(out=wb.rearrange("p (k n) -> p k n", k=12), in_=kview(dr["wbr"])),
          writes=[B_wb], dma_key="wb")
    S.add("gpsimd", lambda e: e.dma_start(out=wo.rearrange("p (k n) -> p k n", k=8), in_=kview(dr["wout"])),
          writes=[B_wo], dma_key="wo")
    hn_g = C.alloc(8 * 512, BF16)
    B_hng = [S.buf(f"hng{c}") for c in range(8)]
    u_g = C.alloc(12 * 512, BF16)
    B_ug = S.buf("ug")
    mixed = C.alloc(8 * 512, BF16)
    B_mixed = [S.buf(f"mixed{j}") for j in range(8)]
    if selv is None:
        uv = kview(dr["uT"])
    else:
        uvA, uvB = kview(dr["uT"][0]), kview(dr["uT"][1])
        u_b = C.alloc(12 * 512, BF16)
        B_ub = S.buf("ub")
    def stage_b_group(gi, t0, w):
        if selv is None:
            S.add("sync", lambda e: e.dma_start(
                out=u_g.rearrange("p (k n) -> p k n", k=12)[:, :, :w], in_=uv[:, :, t0:t0 + w]),
                writes=[B_ug], dma_key="ug")
        else:
            S.add("sync", lambda e: e.dma_start(
                out=u_g.rearrange("p (k n) -> p k n", k=12)[:, :, :w], in_=uvA[:, :, t0:t0 + w]), writes=[B_ug], dma_key="ug")
            S.add("sync", lambda e: e.dma_start(
                out=u_b.rearrange("p (k n) -> p k n", k=12)[:, :, :w], in_=uvB[:, :, t0:t0 + w]), writes=[B_ub], dma_key="ugb")
            S.add("vector", lambda e: e.tensor_scalar(u_b, u_b, selv[:, 1:2], None, ALU.mult), reads=[B_ub, B_selv], writes=[B_ub])
            S.add("vector", lambda e: e.scalar_tensor_tensor(out=u_g, in0=u_g, scalar=selv[:, 0:1], in1=u_b, op0=ALU.mult, op1=ALU.add),
                  reads=[B_ug, B_ub, B_selv], writes=[B_ug])
        emit_rmsnorm_group(C, K, hs, N, B_hs, gains, B_gain, hn_g, 512, B_hng, gi, t0, w, 0)
        for j in range(8):
            acc, Bacc = C.tmp("acc", 512, F32, 1)
            for b in range(3):
                psg, Bpsg = C.ps()
                for kc in range(8):
                    S.add("tensor", lambda e, kc=kc, b=b, j=j, psg=psg: e.matmul(
                        psg[:, :w], wg[:, kc * 3072 + b * 1024 + j * 128: kc * 3072 + b * 1024 + (j + 1) * 128],
                        hn_g[:, kc * 512: kc * 512 + w], start=(kc == 0), stop=(kc == 7)),
                        reads=[B_wg, B_hng[kc]], writes=[Bpsg])
                sg, Bsg = C.tmp("sg", 512, F32, 2)
                S.add("scalar", lambda e, psg=psg, sg=sg: e.activation(out=sg[:, :w], in_=psg[:, :w], func=AF.Sigmoid),
                      reads=[Bpsg], writes=[Bsg])
                psu, Bpsu = C.ps()
                for kc in range(4):
                    S.add("tensor", lambda e, kc=kc, b=b, j=j, psu=psu: e.matmul(
                        psu[:, :w], wb[:, (b * 4 + kc) * 1024 + j * 128: (b * 4 + kc) * 1024 + (j + 1) * 128],
                        u_g[:, (b * 4 + kc) * 512: (b * 4 + kc) * 512 + w], start=(kc == 0), stop=(kc == 3)),
                        reads=[B_wb, B_ug], writes=[Bpsu])
                if b == 0:
                    S.add("vector", lambda e, psu=psu, sg=sg, acc=acc: e.tensor_tensor(
                        out=acc[:, :w], in0=psu[:, :w], in1=sg[:, :w], op=ALU.mult), reads=[Bpsu, Bsg], writes=[Bacc])
                    if DEBUG and gi == DEBUG_G and j == 0:
                        S.add("sync", lambda e, sg=sg: e.dma_start(out=dr["dbg4"], in_=sg), reads=[Bsg], dma_key="dbg")
                        S.add("sync", lambda e, acc=acc: e.dma_start(out=dr["dbg5"], in_=acc), reads=[Bacc], dma_key="dbg")
                else:
                    t2, Bt2 = C.tmp("t2", 512, F32, 1)
                    S.add("vector", lambda e, psu=psu, sg=sg, t2=t2: e.tensor_tensor(
                        out=t2[:, :w], in0=psu[:, :w], in1=sg[:, :w], op=ALU.mult), reads=[Bpsu, Bsg], writes=[Bt2])
                    if b == 1:
                        S.add("vector", lambda e, t2=t2, acc=acc: e.tensor_tensor(
                            out=acc[:, :w], in0=acc[:, :w], in1=t2[:, :w], op=ALU.add), reads=[Bacc, Bt2], writes=[Bacc])
                    else:
                        S.add("vector", lambda e, t2=t2, acc=acc, j=j: e.tensor_tensor(
                            out=mixed[:, j * 512: j * 512 + w], in0=acc[:, :w], in1=t2[:, :w], op=ALU.add),
                            reads=[Bacc, Bt2], writes=[B_mixed[j]])
        if DEBUG and gi == DEBUG_G:
            S.add("sync", lambda e: e.dma_start(out=dr["dbg1"], in_=hn_g), reads=B_hng, dma_key="dbg")
            S.add("sync", lambda e: e.dma_start(out=dr["dbg3"], in_=mixed), reads=B_mixed, dma_key="dbg")
        for j2 in range(8):
            pso, Bpso = C.ps()
            for kc in range(8):
                S.add("tensor", lambda e, kc=kc, j2=j2, pso=pso: e.matmul(
                    pso[:, :w], wo[:, kc * 1024 + j2 * 128: kc * 1024 + (j2 + 1) * 128],
                    mixed[:, kc * 512: kc * 512 + w], start=(kc == 0), stop=(kc == 7)),
                    reads=[B_wo, B_mixed[kc]], writes=[Bpso])
            S.add("vector", lambda e, j2=j2, pso=pso, t0=t0: e.tensor_tensor(
                out=hs[:, j2 * N + t0: j2 * N + t0 + w], in0=pso[:, :w], in1=hs[:, j2 * N + t0: j2 * N + t0 + w], op=ALU.add),
                reads=[Bpso, B_hs[j2][gi]], writes=[B_hs[j2][gi]])

    for gi, (t0, w) in enumerate(groups):
        stage_b_group(gi, t0, w)
    if DEBUG:
        for c in range(8):
            S.add("sync", lambda e, c=c: e.dma_start(out=kview(dr["dbg2"])[:, c, :], in_=hs[:, c * N:(c + 1) * N]),
                  reads=B_hs[c], dma_key="dbg")
    S.fence()
    C.reset(m_persist)
    C.drop_tmps()
    hn2 = C.alloc(8 * N, BF16)
    B_hn2 = [[S.buf(f"hn2_{c}_{g}") for g in range(NG)] for c in range(8)]
    for gi, (t0, w) in enumerate(groups):
        emit_rmsnorm_group(C, K, hs, N, B_hs, gains[:, 8:16], B_gain, hn2, N, [B_hn2[c][gi] for c in range(8)], gi, t0, w, t0)
    n_exp = 8 if moe else 1
    nch = FF // 128
    slabs = []
    c0 = 0
    while c0 < nch:
        n = min(4, nch - c0)
        slabs.append((c0, n))
        c0 += n
    NB = 2
    wgs = [C.alloc(8 * 512, BF16) for _ in range(NB)]
    wus = [C.alloc(8 * 512, BF16) for _ in range(NB)]
    wds = [C.alloc(4 * 1024, BF16) for _ in range(NB)]
    B_wgs = [S.buf("wgs") for _ in range(NB)]
    B_wus = [S.buf("wus") for _ in range(NB)]
    B_wds = [S.buf("wds") for _ in range(NB)]
    acts = [C.alloc(4 * 512, BF16) for _ in range(2)]
    B_act = [[S.buf("act") for _ in range(4)] for _ in range(2)]
    if moe:
        emit_router(C, K, dr, hn2, B_hn2, N, groups)
    def stage_d_group(ex, n, sl, a, gi, t0, w):
        act = acts[a]
        if moe:
            cb, Bcb = emit_cb(C, K, ex, gi, t0, w)
        for i in range(n):
            ps1, Bps1 = C.ps()
            for kc in range(8):
                S.add("tensor", lambda e, kc=kc, i=i, ps1=ps1, sl=sl, t0=t0: e.matmul(
                    ps1[:, :w], wgs[sl][:, kc * 512 + i * 128: kc * 512 + (i + 1) * 128],
                    hn2[:, kc * N + t0: kc * N + t0 + w], start=(kc == 0), stop=(kc == 7)),
                    reads=[B_wgs[sl], B_hn2[kc][gi]], writes=[Bps1])
            ps2, Bps2 = C.ps()
            for kc in range(8):
                S.add("tensor", lambda e, kc=kc, i=i, ps2=ps2, sl=sl, t0=t0: e.matmul(
                    ps2[:, :w], wus[sl][:, kc * 512 + i * 128: kc * 512 + (i + 1) * 128],
                    hn2[:, kc * N + t0: kc * N + t0 + w], start=(kc == 0), stop=(kc == 7)),
                    reads=[B_wus[sl], B_hn2[kc][gi]], writes=[Bps2])
            sl_t, Bsl = C.tmp("silu", 512, F32, 2)
            S.add("scalar", lambda e, ps1=ps1, sl_t=sl_t: e.activation(out=sl_t[:, :w], in_=ps1[:, :w], func=AF.Silu),
                  reads=[Bps1], writes=[Bsl])
            if moe:
                S.add("vector", lambda e, sl_t=sl_t, cb=cb: e.tensor_tensor(
                    out=sl_t[:, :w], in0=sl_t[:, :w], in1=cb[:, :w], op=ALU.mult), reads=[Bsl, Bcb], writes=[Bsl])
            S.add("vector", lambda e, ps2=ps2, sl_t=sl_t, act=act, i=i: e.tensor_tensor(
                out=act[:, i * 512: i * 512 + w], in0=ps2[:, :w], in1=sl_t[:, :w], op=ALU.mult),
                reads=[Bps2, Bsl], writes=[B_act[a][i]])
        for j in range(8):
            psd, Bpsd = C.ps()
            for i in range(n):
                S.add("tensor", lambda e, i=i, j=j, psd=psd, sl=sl, act=act: e.matmul(
                    psd[:, :w], wds[sl][:, i * 1024 + j * 128: i * 1024 + (j + 1) * 128],
                    act[:, i * 512: i * 512 + w], start=(i == 0), stop=(i == n - 1)),
                    reads=[B_wds[sl], B_act[a][i]], writes=[Bpsd])
            S.add("vector", lambda e, j=j, psd=psd, t0=t0: e.tensor_tensor(
                out=hs[:, j * N + t0: j * N + t0 + w], in0=psd[:, :w], in1=hs[:, j * N + t0: j * N + t0 + w], op=ALU.add),
                reads=[Bpsd, B_hs[j][gi]], writes=[B_hs[j][gi]])

    si = 0
    ai = 0
    for ex in range(n_exp):
        for (c0, n) in slabs:
            sl = si % NB
            si += 1
            S.add("gpsimd", lambda e, ex=ex, c0=c0, n=n, sl=sl: e.dma_start(
                out=wgs[sl].rearrange("p (k n) -> p k n", k=8)[:, :, :n * 128],
                in_=kview(dr["fg"][ex])[:, :, c0 * 128:(c0 + n) * 128]), writes=[B_wgs[sl]], dma_key=f"wgs{sl}")
            S.add("gpsimd", lambda e, ex=ex, c0=c0, n=n, sl=sl: e.dma_start(
                out=wus[sl].rearrange("p (k n) -> p k n", k=8)[:, :, :n * 128],
                in_=kview(dr["fu"][ex])[:, :, c0 * 128:(c0 + n) * 128]), writes=[B_wus[sl]], dma_key=f"wus{sl}")
            S.add("gpsimd", lambda e, ex=ex, c0=c0, n=n, sl=sl: e.dma_start(
                out=wds[sl].rearrange("p (k n) -> p k n", k=4)[:, :n, :],
                in_=kview(dr["fd"][ex][c0 * 128:(c0 + n) * 128, :])), writes=[B_wds[sl]], dma_key=f"wds{sl}")
            for gi, (t0, w) in enumerate(groups):
                a = ai % 2
                ai += 1
                stage_d_group(ex, n, sl, a, gi, t0, w)
    ov = kview(dr["out"])
    for c in range(8):
        S.add("sync", lambda e, c=c: e.dma_start(out=ov[:, c, :], in_=hs[:, c * N:(c + 1) * N]),
              reads=B_hs[c], dma_key="out")
    S.fence(new_epoch=True)
    C.reset(m_base)
    C.drop_tmps()


def emit_router(C, K, dr, hn2, B_hn2, N, groups):
    S = C.S
    wr = C.alloc(8 * 8, BF16)
    B_wr = S.buf("wr")
    S.add("gpsimd", lambda e: e.dma_start(out=wr.rearrange("p (k n) -> p k n", k=8), in_=kview(dr["wr"])), writes=[B_wr], dma_key="wr")
    ident = C.alloc(128, F32)
    B_id = S.buf("ident")
    S.add("sync", lambda e: e.dma_start(out=ident, in_=dr["ident"]), writes=[B_id], dma_key="ident")
    sel = C.alloc(1024, BF16)
    B_sel = S.buf("sel")
    S.add("gpsimd", lambda e: e.dma_start(out=sel[0:8, :], in_=dr["sel"]), writes=[B_sel], dma_key="sel")
    combT = C.alloc(N, BF16)
    B_combT = [S.buf(f"combT{g}") for g in range(len(groups))]
    K["sel"], K["B_sel"], K["combT"], K["B_combT"] = sel, B_sel, combT, B_combT

    def tile(tt):
        c0 = tt * 128
        gi = c0 // 512
        ps, Bps = C.ps()
        for kc in range(8):
            S.add("tensor", lambda e, kc=kc: e.matmul(ps[:, 0:8], hn2[:, kc * N + c0: kc * N + c0 + 128], wr[:, kc * 8:(kc + 1) * 8],
                                                       start=(kc == 0), stop=(kc == 7)), reads=[B_hn2[kc][gi], B_wr], writes=[Bps])
        lg, Blg = C.tmp("lg", 8, F32, 2)
        S.add("vector", lambda e: e.tensor_copy(out=lg, in_=ps[:, 0:8]), reads=[Bps], writes=[Blg])
        mx, Bmx = C.tmp("mx", 8, F32, 2)
        S.add("vector", lambda e: e.max(out=mx, in_=lg), reads=[Blg], writes=[Bmx])
        nm1, Bnm1 = C.tmp("nm1", 1, F32, 2)
        S.add("vector", lambda e: e.tensor_scalar(nm1, mx[:, 0:1], -1.0, None, ALU.mult), reads=[Bmx], writes=[Bnm1])
        ex, Bex = C.tmp("ex", 8, F32, 2)
        S.add("scalar", lambda e: e.activation(out=ex, in_=lg, func=AF.Exp, bias=nm1, scale=1.0), reads=[Blg, Bnm1], writes=[Bex])
        mask, Bmask = C.tmp("mask", 8, F32, 2)
        S.add("vector", lambda e: e.tensor_scalar(mask, lg, mx[:, 1:2], None, ALU.is_ge), reads=[Blg, Bmx], writes=[Bmask])
        num, Bnum = C.tmp("num", 8, F32, 2)
        S.add("vector", lambda e: e.tensor_tensor(out=num, in0=mask, in1=ex, op=ALU.mult), reads=[Bmask, Bex], writes=[Bnum])
        den, Bden = C.tmp("den", 1, F32, 2)
        S.add("vector", lambda e: e.tensor_reduce(out=den, in_=num, axis=AX.X, op=ALU.add), reads=[Bnum], writes=[Bden])
        S.add("vector", lambda e: e.reciprocal(out=den, in_=den), reads=[Bden], writes=[Bden])
        comb, Bcomb = C.tmp("comb", 8, F32, 2)
        S.add("vector", lambda e: e.tensor_scalar(comb, num, den[:, 0:1], None, ALU.mult), reads=[Bnum, Bden], writes=[Bcomb])
        pt, Bpt = C.ps()
        S.add("tensor", lambda e: e.matmul(pt[0:8, 0:128], comb, ident, start=True, stop=True), reads=[Bcomb, B_id], writes=[Bpt])
        S.add("scalar", lambda e: e.activation(out=combT[0:8, c0:c0 + 128], in_=pt[0:8, 0:128], func=AF.Copy),
              reads=[Bpt], writes=[B_combT[gi]])

    for tt in range(N // 128):
        tile(tt)


def emit_cb(C, K, ex, gi, t0, w):
    S = C.S
    ps, Bps = C.ps()
    S.add("tensor", lambda e: e.matmul(ps[:, :w], K["sel"][0:8, ex * 128:(ex + 1) * 128], K["combT"][0:8, t0:t0 + w], start=True, stop=True),
          reads=[K["B_sel"], K["B_combT"][gi]], writes=[Bps])
    cb, Bcb = C.tmp("cb", 512, F32, 2)
    S.add("scalar", lambda e: e.activation(out=cb[:, :w], in_=ps[:, :w], func=AF.Copy), reads=[Bps], writes=[Bcb])
    return cb, Bcb


TP = 4224
NT = 33
RL = 1152


def build_phase1(layer, parts=("conv", "hgrn", "att")):
    T = T_ALL
    nc = bass.Bass("TRN2", target_bir_lowering=False)
    dr = {}
    dr["hsT"] = nc.dram_tensor("hsT", [D, T], F32, kind="ExternalInput").ap()
    dr["params"] = nc.dram_tensor("params", [128, 280], F32, kind="ExternalInput").ap()
    dr["consts"] = nc.dram_tensor("consts", [128, 896], F32, kind="ExternalInput").ap()
    dr["oh"] = nc.dram_tensor("oh", [33, RL], F32, kind="ExternalInput").ap()
    dr["relb"] = nc.dram_tensor("relb", [32, 2], F32, kind="ExternalInput").ap()
    dr["wqk"] = nc.dram_tensor("wqk", [D, 512], F32, kind="ExternalInput").ap()
    dr["wv"] = nc.dram_tensor("wv", [D, 256], F32, kind="ExternalInput").ap()
    dr["wconv"] = nc.dram_tensor("wconv", [D, 768], F32, kind="ExternalInput").ap()
    dr["whg"] = nc.dram_tensor("whg", [D, 768], F32, kind="ExternalInput").ap()
    dr["wi"] = nc.dram_tensor("wi", [D, 256], F32, kind="ExternalInput").ap()
    rscr_t = nc.dram_tensor("rscr", [2, RL], F32, kind="Internal")
    dr["rscr"] = rscr_t.ap()
    dr["rscr_t"] = rscr_t
    dr["out"] = nc.dram_tensor("uT", [768, T], BF16, kind="ExternalOutput").ap()
    dr["o_att"] = [dr["out"][hd * 128:(hd + 1) * 128, :] for hd in range(2)]
    dr["o_conv"] = [dr["out"][256 + ci * 128: 256 + (ci + 1) * 128, :] for ci in range(2)]
    dr["o_hgrn"] = [dr["out"][512 + hd * 128: 512 + (hd + 1) * 128, :] for hd in range(2)]

    with contextlib.ExitStack() as st:
        C = Ctx(nc, st)
        K = emit_consts(C)
        emit_phase1(C, K, dr, layer, parts)
        C.S.finalize(final_dma_keys=["out"])
        print("phase1 ops", len(C.S.ops), "sig", C.S.max_counts)
    return nc


def emit_phase1(C, K, dr, layer, parts=("conv", "hgrn", "att")):
    S = C.S
    T = T_ALL
    lam_init = 0.8 - 0.6 * math.exp(-0.3 * layer)
    m_base = C.mark()
    groups = make_groups(T)
    NG = len(groups)
    prm = C.alloc(280, F32)
    B_prm = S.buf("prm")
    S.add("sync", lambda e: e.dma_start(out=prm, in_=dr["params"]), writes=[B_prm], dma_key="prm")
    cst = C.alloc(896, F32)
    B_cst = S.buf("cst")
    S.add("sync", lambda e: e.dma_start(out=cst, in_=dr["consts"]), writes=[B_cst], dma_key="cst")
    ident_f = cst[:, 0:128]
    J_f = cst[:, 128:256]
    hmask = cst[:, 256:384]
    scanmask = cst[:, 384:896]
    ident_b = C.alloc(128, BF16)
    B_idb = S.buf("identb")
    S.add("vector", lambda e: e.tensor_copy(out=ident_b, in_=ident_f), reads=[B_cst], writes=[B_idb])
    ones1 = C.alloc(128, BF16)
    B_ones1 = S.buf("ones1")
    S.add("vector", lambda e: e.memset(ones1, 1.0), writes=[B_ones1])
    onesd = C.alloc(128, BF16)
    B_onesd = S.buf("onesd")
    S.add("vector", lambda e: e.memset(onesd, 1.0 / 128.0), writes=[B_onesd])
    blk64 = C.alloc(128, BF16)
    B_blk = S.buf("blk64")
    S.add("vector", lambda e: e.memset(blk64, 0.0), writes=[B_blk])
    S.add("vector", lambda e: e.memset(blk64[0:64, 0:64], 1.0 / 64.0), writes=[B_blk])
    S.add("vector", lambda e: e.memset(blk64[64:128, 64:128], 1.0 / 64.0), writes=[B_blk])

    hn = C.alloc(8 * TP, BF16)
    B_hn = [[S.buf(f"hn{c}_{g}") for g in range(NG + 1)] for c in range(8)]
    for c in range(8):
        S.add("gpsimd", lambda e, c=c: e.memset(hn[:, c * TP + T: (c + 1) * TP], 0.0), writes=[B_hn[c][NG]])
    m_main = C.mark()
    hsb = [C.alloc(8 * 512, F32) for _ in range(2)]
    B_hsb = [[S.buf(f"hsb{i}_{c}") for c in range(8)] for i in range(2)]
    hsv = kview(dr["hsT"])

    def norm_group(gi, t0, w):
        i = gi % 2
        S.add("sync", lambda e: e.dma_start(out=hsb[i].rearrange("p (k n) -> p k n", k=8)[:, :, :w], in_=hsv[:, :, t0:t0 + w]),
              writes=B_hsb[i], dma_key=f"hsb{i}")
        emit_rmsnorm_group(C, K, hsb[i], 512, [[B_hsb[i][c]] for c in range(8)], prm, B_prm, hn, TP,
                           [B_hn[c][gi] for c in range(8)], 0, 0, w, t0)

    for gi, (t0, w) in enumerate(groups):
        norm_group(gi, t0, w)
    S.fence()
    C.reset(m_main)
    C.drop_tmps()

    def hn_bufs(kc, t0, w):
        g0 = t0 // 512
        g1 = min((t0 + w - 1) // 512, NG)
        return [B_hn[kc][g] for g in range(g0, g1 + 1)]

    def rstd_from_ms(ps, w, bias_ap, B_bias):
        r, Br = C.tmp("rstd1", 512, F32, 2)
        S.add("scalar", lambda e: e.activation(out=r[:, :w], in_=ps[:, :w], func=AF.Ln, bias=K["eps"], scale=1.0),
              reads=[K["B_eps"]] + [ps_b for ps_b in []], writes=[Br])
        return r, Br

    if "conv" in parts:
        m0 = C.mark()
        wc = C.alloc(8 * 768, BF16)
        B_wc = S.buf("wc")
        S.add("gpsimd", lambda e: e.dma_start(out=wc.rearrange("p (k n) -> p k n", k=8), in_=kview(dr["wconv"])), writes=[B_wc], dma_key="wc")
        zb = [C.alloc(516, F32) for _ in range(2)]
        B_z = [S.buf("z0"), S.buf("z1")]
        for ci in range(2):
            S.add("vector", lambda e, ci=ci: e.memset(zb[ci][:, 0:2], 0.0), writes=[B_z[ci]])

        def conv_group(gi, t0, w):
            for ci in range(2):
                conv_chunk(gi, t0, w, ci)

        def conv_chunk(gi, t0, w, ci):
            if True:
                pss = []
                for br in range(3):
                    ps, Bps = C.ps()
                    for kc in range(8):
                        S.add("tensor", lambda e, kc=kc, br=br, ps=ps: e.matmul(
                            ps[:, :w], wc[:, kc * 768 + br * 256 + ci * 128: kc * 768 + br * 256 + (ci + 1) * 128],
                            hn[:, kc * TP + t0: kc * TP + t0 + w], start=(kc == 0), stop=(kc == 7)),
                            reads=[B_wc] + hn_bufs(kc, t0, w), writes=[Bps])
                    pss.append((ps, Bps))
                (pb, Bpb), (pc, Bpc), (ph, Bph) = pss
                z = zb[ci]
                th, Bth = C.tmp("ch", 512, F32, 2)
                S.add("scalar", lambda e: e.activation(out=th[:, :w], in_=ph[:, :w], func=AF.Copy), reads=[Bph], writes=[Bth])
                S.add("vector", lambda e: e.tensor_tensor(out=z[:, 2:2 + w], in0=pc[:, :w], in1=th[:, :w], op=ALU.mult),
                      reads=[Bpc, Bth], writes=[B_z[ci]])
                y, By = C.tmp("y", 512, F32, 2)
                wcol = 14 + ci * 3
                S.add("vector", lambda e: e.tensor_scalar(y[:, :w], z[:, 2:2 + w], prm[:, wcol + 2:wcol + 3], None, ALU.mult),
                      reads=[B_z[ci], B_prm], writes=[By])
                S.add("vector", lambda e: e.scalar_tensor_tensor(out=y[:, :w], in0=z[:, 1:1 + w], scalar=prm[:, wcol + 1:wcol + 2],
                                                                 in1=y[:, :w], op0=ALU.mult, op1=ALU.add),
                      reads=[B_z[ci], B_prm, By], writes=[By])
                S.add("vector", lambda e: e.scalar_tensor_tensor(out=y[:, :w], in0=z[:, 0:w], scalar=prm[:, wcol:wcol + 1],
                                                                 in1=y[:, :w], op0=ALU.mult, op1=ALU.add),
                      reads=[B_z[ci], B_prm, By], writes=[By])
                uo, Buo = C.tmp("uo", 512, BF16, 3)
                S.add("vector", lambda e: e.tensor_tensor(out=uo[:, :w], in0=pb[:, :w], in1=y[:, :w], op=ALU.mult),
                      reads=[Bpb, By], writes=[Buo])
                S.add("sync", lambda e: e.dma_start(out=dr["o_conv"][ci][:, t0:t0 + w], in_=uo[:, :w]),
                      reads=[Buo], dma_key="out")
                if w >= 2:
                    S.add("vector", lambda e: e.tensor_copy(out=z[:, 0:2], in_=z[:, w:w + 2]), reads=[B_z[ci]], writes=[B_z[ci]])

        for gi, (t0, w) in enumerate(groups):
            conv_group(gi, t0, w)
        S.fence()
        C.reset(m0)
        C.drop_tmps()

    if "hgrn" in parts:
        m0 = C.mark()
        whg = C.alloc(8 * 768, BF16)
        B_whg = S.buf("whg")
        S.add("gpsimd", lambda e: e.dma_start(out=whg.rearrange("p (k n) -> p k n", k=8), in_=kview(dr["whg"])), writes=[B_whg], dma_key="whg")
        wi = C.alloc(8 * 256, BF16)
        B_wi = S.buf("wi")
        S.add("gpsimd", lambda e: e.dma_start(out=wi.rearrange("p (k n) -> p k n", k=8), in_=kview(dr["wi"])), writes=[B_wi], dma_key="wi")
        hmask_b = hmask
        lb = C.alloc(2, F32)
        oml = C.alloc(2, F32)
        B_lb = S.buf("lb")
        if layer == 0:
            S.add("vector", lambda e: e.memset(lb, 0.0), writes=[B_lb])
        else:
            dl = C.alloc(2, F32)
            B_dl = S.buf("dl")
            for hd in range(2):
                S.add("vector", lambda e, hd=hd: e.tensor_tensor(out=dl[:, hd:hd + 1], in0=prm[:, 21 + 2 * hd:22 + 2 * hd],
                                                               in1=prm[:, 20 + 2 * hd:21 + 2 * hd], op=ALU.subtract),
                      reads=[B_prm], writes=[B_dl])
            S.add("scalar", lambda e: e.activation(out=lb, in_=dl, func=AF.Sigmoid), reads=[B_dl], writes=[B_lb])
        S.add("vector", lambda e: e.tensor_scalar(oml, lb, -1.0, 1.0, ALU.mult, ALU.add), reads=[B_lb], writes=[B_lb])
        hgroups = make_groups(TP)
        rb = C.reserve(2)
        po_b = [rb[0], rb[1]]
        kv_s = [[C.alloc(128, F32) for _ in range(8)] for _ in range(2)]
        B_kv = [[S.buf("kv") for _ in range(8)] for _ in range(2)]
        Sf = [[C.alloc(128, F32) for _ in range(2)] for _ in range(2)]
        B_Sf = [[S.buf("Sf") for _ in range(2)] for _ in range(2)]
        for hd in range(2):
            S.add("vector", lambda e, hd=hd: e.memset(Sf[hd][0], 0.0), writes=[B_Sf[hd][0]])
        kTz = [[[C.alloc(128, BF16) for _ in range(2)] for _ in range(4)] for _ in range(2)]
        B_kTz = [[[S.buf("kTz") for _ in range(2)] for _ in range(4)] for _ in range(2)]
        for hd in range(2):
            for tl in range(4):
                for cc in range(2):
                    S.add("gpsimd", lambda e, hd=hd, tl=tl, cc=cc: e.memset(kTz[hd][tl][cc], 0.0), writes=[B_kTz[hd][tl][cc]])
        vts = [[C.alloc(128, BF16) for _ in range(4)] for _ in range(2)]
        B_vts = [[S.buf("vt") for _ in range(4)] for _ in range(2)]
        Ats = [[C.alloc(128, BF16) for _ in range(4)] for _ in range(2)]
        B_Ats = [[S.buf("At") for _ in range(4)] for _ in range(2)]
        step = [0, 0]

        def phase_a2(gi, t0, w):
            nch = w // 64
            H = (0, 1)
            Xs = [{"nch": nch, "ntl": w // 128, "t0": t0, "w": w, "gi": gi} for _ in H]
            CARRY = ("qi", "q2", "k2", "kht", "gs")

            def T_(kind, hd, dt=F32):
                return C.tmp(f"{kind}{hd}", 512, dt, 2 if kind in CARRY else 1)

            def proj(br, kind, func):
                outs = []
                pss = []
                for hd in H:
                    ps, Bps = C.ps()
                    for kc in range(8):
                        S.add("tensor", lambda e, kc=kc, hd=hd, ps=ps: e.matmul(
                            ps[:, :w], whg[:, kc * 768 + br * 256 + hd * 128: kc * 768 + br * 256 + (hd + 1) * 128],
                            hn[:, kc * TP + t0: kc * TP + t0 + w], start=(kc == 0), stop=(kc == 7)),
                            reads=[B_whg] + hn_bufs(kc, t0, w), writes=[Bps])
                    pss.append((ps, Bps))
                for hd in H:
                    ps, Bps = pss[hd]
                    o, Bo = T_(kind, hd)
                    S.add("scalar", lambda e, o=o, ps=ps: e.activation(out=o[:, :w], in_=ps[:, :w], func=func), reads=[Bps], writes=[Bo])
                    outs.append((o, Bo))
                return outs

            def act(kind, src, func, scale=1.0, dt=F32, extra=()):
                outs = []
                for hd in H:
                    o, Bo = T_(kind, hd, dt)
                    a, Ba = src[hd]
                    S.add("scalar", lambda e, o=o, a=a: e.activation(out=o[:, :w], in_=a[:, :w], func=func, scale=scale),
                          reads=[Ba] + [x[hd][1] for x in extra], writes=[Bo])
                    outs.append((o, Bo))
                return outs

            def mul(kind, a_, b_, dt=F32):
                outs = []
                for hd in H:
                    o, Bo = T_(kind, hd, dt)
                    (a, Ba), (b, Bb) = a_[hd], b_[hd]
                    S.add("vector", lambda e, o=o, a=a, b=b: e.tensor_tensor(out=o[:, :w], in0=a[:, :w], in1=b[:, :w], op=ALU.mult),
                          reads=[Ba, Bb], writes=[Bo])
                    outs.append((o, Bo))
                return outs

            qs = proj(0, "qs", AF.Silu)
            gs = proj(2, "gs", AF.Silu)
            f = proj(1, "f", AF.Sigmoid)
            for hd in H:
                ff, Bf = f[hd]
                S.add("vector", lambda e, ff=ff, hd=hd: e.tensor_scalar(ff[:, :w], ff[:, :w], oml[:, hd:hd + 1], lb[:, hd:hd + 1], ALU.mult, ALU.add),
                      reads=[Bf, B_lb], writes=[Bf])
            lf = act("lf", f, AF.Ln)
            kh = []
            for hd in H:
                o, Bo = T_("kh", hd)
                ff, Bf = f[hd]
                S.add("vector", lambda e, o=o, ff=ff: e.tensor_scalar(o[:, :w], ff[:, :w], -1.0, 1.0, ALU.mult, ALU.add), reads=[Bf], writes=[Bo])
                kh.append((o, Bo))
            G = []
            for hd in H:
                o, Bo = T_("G", hd)
                l_, Bl = lf[hd]
                S.add("vector", lambda e, o=o, l_=l_: e.tensor_tensor_scan(out=o[:, :w], data0=scanmask[:, :w], data1=l_[:, :w], initial=0.0,
                                                                         op0=ALU.mult, op1=ALU.add), reads=[Bl, B_cst], writes=[Bo])
                G.append((o, Bo))
            G3 = [G[hd][0][:, :w].rearrange("p (c t) -> p c t", t=64) for hd in H]
            E = act("E", G, AF.Exp)
            qi = mul("qi", qs, E)
            scl = []
            for hd in H:
                o, Bo = C.tmp(f"scl{hd}", 8, F32, 2)
                e_, Be = E[hd]
                S.add("vector", lambda e, o=o, e_=e_: e.tensor_copy(out=o[:, :nch].rearrange("p (c o) -> p c o", o=1),
                                                                     in_=e_[:, :w].rearrange("p (c t) -> p c t", t=64)[:, :, 63:64]), reads=[Be], writes=[Bo])
                scl.append((o, Bo))
            Dm = []
            for hd in H:
                o, Bo = T_("Dm", hd)
                S.add("vector", lambda e, o=o, hd=hd: e.tensor_tensor(out=o[:, :w].rearrange("p (c t) -> p c t", t=64), in0=G3[hd],
                                                                      in1=G3[hd][:, :, 31:32].broadcast_to([128, nch, 64]), op=ALU.subtract),
                      reads=[G[hd][1]], writes=[Bo])
                Dm.append((o, Bo))
            E2 = act("E2", Dm, AF.Exp)
            q2 = mul("q2", qs, E2, BF16)
            E3 = act("E3", Dm, AF.Exp, scale=-1.0)
            k2 = mul("k2", kh, E3, BF16)
            Dl = []
            for hd in H:
                o, Bo = T_("Dl", hd)
                S.add("vector", lambda e, o=o, hd=hd: e.tensor_tensor(out=o[:, :w].rearrange("p (c t) -> p c t", t=64), in0=G3[hd],
                                                                      in1=G3[hd][:, :, 63:64].broadcast_to([128, nch, 64]), op=ALU.subtract),
                      reads=[G[hd][1]], writes=[Bo])
                Dl.append((o, Bo))
            E4 = act("E4", Dl, AF.Exp, scale=-1.0)
            kht = mul("kht", kh, E4, BF16)
            for hd in H:
                Xs[hd].update(dict(qi=qi[hd][0], Bqi=qi[hd][1], q2=q2[hd][0], Bq2=q2[hd][1], k2=k2[hd][0], Bk2=k2[hd][1],
                                   kht=kht[hd][0], Bkht=kht[hd][1], scl=scl[hd][0], Bscl=scl[hd][1], gs=gs[hd][0], Bgs=gs[hd][1]))
            return Xs

        def phase_b(hd, X):
            t0 = X["t0"]
            for tl in range(X["ntl"]):
                def tile(tl):
                    c0 = tl * 128
                    pv, Bpv = C.ps()
                    for kc in range(8):
                        S.add("tensor", lambda e, kc=kc: e.matmul(
                            pv[:, 0:128], hn[:, kc * TP + t0 + c0: kc * TP + t0 + c0 + 128], wi[:, kc * 256 + hd * 128: kc * 256 + (hd + 1) * 128],
                            start=(kc == 0), stop=(kc == 7)), reads=[B_wi] + hn_bufs(kc, t0 + c0, 128), writes=[Bpv])
                    vt, Bvt = vts[hd][tl], B_vts[hd][tl]
                    S.add("scalar", lambda e: e.activation(out=vt, in_=pv[:, 0:128], func=AF.Copy), reads=[Bpv], writes=[Bvt])
                    pk, Bpk = C.ps()
                    S.add("tensor", lambda e: e.matmul(pk[:, 0:128], X["kht"][:, c0:c0 + 128], ident_b, start=True, stop=True),
                          reads=[X["Bkht"], B_idb], writes=[Bpk])
                    for cc in range(2):
                        S.add("vector", lambda e, cc=cc: e.tensor_copy(out=kTz[hd][tl][cc][cc * 64:(cc + 1) * 64, :], in_=pk[cc * 64:(cc + 1) * 64, 0:128]),
                              reads=[Bpk], writes=[B_kTz[hd][tl][cc]])
                    pa, Bpa = C.ps()
                    S.add("tensor", lambda e: e.matmul(pa[:, 0:128], X["k2"][:, c0:c0 + 128], X["q2"][:, c0:c0 + 128], start=True, stop=True),
                          reads=[X["Bk2"], X["Bq2"]], writes=[Bpa])
                    S.add("vector", lambda e: e.tensor_tensor(out=Ats[hd][tl], in0=pa[:, 0:128], in1=hmask_b, op=ALU.mult),
                          reads=[Bpa, B_cst], writes=[B_Ats[hd][tl]])
                    for cc in range(2):
                        def kvprod(cc):
                            ch = tl * 2 + cc
                            pkv, Bpkv = C.ps()
                            S.add("tensor", lambda e: e.matmul(pkv[:, 0:128], kTz[hd][tl][cc], vt, start=True, stop=True),
                                  reads=[B_kTz[hd][tl][cc], Bvt], writes=[Bpkv])
                            S.add("scalar", lambda e: e.activation(out=kv_s[hd][ch], in_=pkv[:, 0:128], func=AF.Copy), reads=[Bpkv], writes=[B_kv[hd][ch]])
                        kvprod(cc)
                tile(tl)

        def chain_step(hd, X, ch):
            po, Bpo = po_b[hd]
            cs = ch * 64
            i = step[hd] % 2
            Sc, BSc = Sf[hd][i], B_Sf[hd][i]
            Sn, BSn = Sf[hd][1 - i], B_Sf[hd][1 - i]
            step[hd] += 1
            if HG_BF16_STATE:
                Sbb, BSbb = C.tmp(f"Sbb{hd}", 128, BF16, 2)
                qib, Bqib = C.tmp(f"qib{hd}", 64, BF16, 2)
                S.add("scalar", lambda e: e.activation(out=Sbb, in_=Sc, func=AF.Copy), reads=[BSc], writes=[BSbb])
                S.add("gpsimd", lambda e: e.tensor_copy(out=qib, in_=X["qi"][:, cs:cs + 64]), reads=[X["Bqi"]], writes=[Bqib])
                S.add("tensor", lambda e: e.matmul(po[:, cs:cs + 64], Sbb, qib, start=(cs == 0), stop=False, skip_group_check=True),
                      reads=[BSbb, Bqib], writes=[Bpo])
            else:
                S.add("tensor", lambda e: e.matmul(po[:, cs:cs + 64], Sc, X["qi"][:, cs:cs + 64], start=(cs == 0), stop=False, skip_group_check=True),
                      reads=[BSc, X["Bqi"]], writes=[Bpo])
            S.add("vector", lambda e: e.scalar_tensor_tensor(out=Sn, in0=Sc, scalar=X["scl"][:, ch:ch + 1], in1=kv_s[hd][ch],
                                                             op0=ALU.mult, op1=ALU.add), reads=[BSc, X["Bscl"], B_kv[hd][ch]], writes=[BSn])
            if ch % 2 == 1:
                tl = ch // 2
                c0 = tl * 128
                S.add("tensor", lambda e: e.matmul(po[:, c0:c0 + 128], vts[hd][tl], Ats[hd][tl], start=False, stop=True, skip_group_check=True),
                      reads=[B_vts[hd][tl], B_Ats[hd][tl]], writes=[Bpo])

        def phase_d(hd, X):
            t0, w = X["t0"], X["w"]
            po, Bpo = po_b[hd]
            wv_ = min(w, T - t0)
            if wv_ <= 0:
                return
            osb, Bosb = C.tmp(f"osb{hd}", 512, F32, 1)
            S.add("scalar", lambda e: e.activation(out=osb[:, :w], in_=po[:, :w], func=AF.Copy), reads=[Bpo], writes=[Bosb])
            sq, Bsq = C.tmp("sq", 512, BF16, 2)
            S.add("scalar", lambda e: e.activation(out=sq[:, :w], in_=osb[:, :w], func=AF.Square), reads=[Bosb], writes=[Bsq])
            pm, Bpm = C.ps()
            S.add("tensor", lambda e: e.matmul(pm[:, :w], onesd, sq[:, :w], start=True, stop=True), reads=[Bsq, B_onesd], writes=[Bpm])
            rs, Brs = C.tmp("rs", 512, F32, 2)
            S.add("scalar", lambda e: e.activation(out=rs[:, :w], in_=pm[:, :w], func=AF.Ln, bias=K["eps"], scale=1.0),
                  reads=[Bpm, K["B_eps"]], writes=[Brs])
            S.add("scalar", lambda e: e.activation(out=rs[:, :w], in_=rs[:, :w], func=AF.Exp, scale=-0.5), reads=[Brs], writes=[Brs])
            S.add("vector", lambda e: e.scalar_tensor_tensor(out=osb[:, :w], in0=osb[:, :w], scalar=prm[:, 11:12], in1=rs[:, :w],
                                                             op0=ALU.mult, op1=ALU.mult), reads=[Bosb, Brs, B_prm], writes=[Bosb])
            uo, Buo = C.tmp("uoh", 512, BF16, 2)
            S.add("vector", lambda e: e.tensor_tensor(out=uo[:, :w], in0=osb[:, :w], in1=X["gs"][:, :w], op=ALU.mult),
                  reads=[Bosb, X["Bgs"]], writes=[Buo])
            S.add("sync", lambda e: e.dma_start(out=dr["o_hgrn"][hd][:, t0:t0 + wv_], in_=uo[:, :wv_]), reads=[Buo], dma_key="out")

        Xn = phase_a2(0, hgroups[0][0], hgroups[0][1])
        for gi, (t0, w) in enumerate(hgroups):
            Xs = Xn
            if gi + 1 < len(hgroups):
                Xn = phase_a2(gi + 1, hgroups[gi + 1][0], hgroups[gi + 1][1])
            for hd in range(2):
                phase_b(hd, Xs[hd])
            for ch in range(Xs[0]["nch"]):
                for hd in range(2):
                    chain_step(hd, Xs[hd], ch)
            for hd in range(2):
                phase_d(hd, Xs[hd])
        C.release()
        S.fence()
        C.reset(m0)
        C.drop_tmps()

    if "att" in parts:
        emit_attention(C, K, dr, hn, hn_bufs, prm, B_prm, cst, B_cst, dr["rscr_t"], lam_init, groups,
                       dict(ones1=ones1, B_ones1=B_ones1, onesd=onesd, B_onesd=B_onesd, blk64=blk64, B_blk=B_blk))
    S.fence(new_epoch=True)
    C.reset(m_base)
    C.drop_tmps()


def emit_attention(C, K, dr, hn, hn_bufs, prm, B_prm, cst, B_cst, rscr_t, lam_init, groups, X):
    S = C.S
    T = T_ALL
    ones1, B_ones1, onesd, B_onesd, blk64, B_blk = X["ones1"], X["B_ones1"], X["onesd"], X["B_onesd"], X["blk64"], X["B_blk"]
    J_f = cst[:, 128:256]
    wqk = C.alloc(8 * 512, BF16)
    B_wqk = S.buf("wqk")
    S.add("gpsimd", lambda e: e.dma_start(out=wqk.rearrange("p (k n) -> p k n", k=8), in_=kview(dr["wqk"])), writes=[B_wqk], dma_key="wqk")
    wv = C.alloc(8 * 256, BF16)
    B_wv = S.buf("wv")
    S.add("gpsimd", lambda e: e.dma_start(out=wv.rearrange("p (k n) -> p k n", k=8), in_=kview(dr["wv"])), writes=[B_wv], dma_key="wv")

    lt = C.alloc(128, F32)
    ssum = C.alloc(2, F32)
    nlam = C.alloc(1, F32)
    lnc = C.alloc(1, F32)
    B_l = S.buf("lam")
    B_nlam = S.buf("nlam")
    S.add("vector", lambda e: e.tensor_tensor(out=lt[:, 0:64], in0=prm[:, 24:88], in1=prm[:, 88:152], op=ALU.mult), reads=[B_prm], writes=[B_l])
    S.add("vector", lambda e: e.tensor_tensor(out=lt[:, 64:128], in0=prm[:, 152:216], in1=prm[:, 216:280], op=ALU.mult), reads=[B_prm], writes=[B_l])
    S.add("vector", lambda e: e.tensor_reduce(out=ssum[:, 0:1], in_=lt[:, 0:64], axis=AX.X, op=ALU.add), reads=[B_l], writes=[B_l])
    S.add("vector", lambda e: e.tensor_reduce(out=ssum[:, 1:2], in_=lt[:, 64:128], axis=AX.X, op=ALU.add), reads=[B_l], writes=[B_l])
    S.add("scalar", lambda e: e.activation(out=ssum, in_=ssum, func=AF.Exp), reads=[B_l], writes=[B_l])
    S.add("vector", lambda e: e.tensor_tensor(out=nlam, in0=ssum[:, 1:2], in1=ssum[:, 0:1], op=ALU.subtract), reads=[B_l], writes=[B_nlam])
    S.add("vector", lambda e: e.tensor_scalar(nlam, nlam, -lam_init, None, ALU.add), reads=[B_nlam], writes=[B_nlam])
    S.add("vector", lambda e: e.memset(lnc, math.log(1.0 - lam_init)), writes=[B_nlam])

    relb = C.alloc(2, F32)
    B_relb = S.buf("relb")
    S.add("vector", lambda e: e.memset(relb[32:33, :], 1.0), writes=[B_relb])
    S.add("sync", lambda e: e.dma_start(out=relb[0:32, :], in_=dr["relb"]), writes=[B_relb], dma_key="relb")
    oh = C.alloc(RL, F32)
    B_oh = S.buf("oh")
    S.add("sync", lambda e: e.dma_start(out=oh[0:33, :], in_=dr["oh"]), writes=[B_oh], dma_key="oh")
    rsb = C.alloc(RL, F32)
    B_rsb = S.buf("rsb")
    for cc in range(3):
        def rchunk(cc):
            ps, Bps = C.ps()
            S.add("tensor", lambda e: e.matmul(ps[0:2, 0:384], relb[0:33, :], oh[0:33, cc * 384:(cc + 1) * 384], start=True, stop=True),
                  reads=[B_relb, B_oh], writes=[Bps])
            S.add("vector", lambda e: e.tensor_copy(out=rsb[0:2, cc * 384:(cc + 1) * 384], in_=ps[0:2, 0:384]), reads=[Bps], writes=[B_rsb])
        rchunk(cc)
    B_rscr = S.buf("rscr")
    S.add("sync", lambda e: e.dma_start(out=dr["rscr"], in_=rsb[0:2, :]), reads=[B_rsb], writes=[B_rscr], dma_key="rscr")
    Bt = {}
    B_Bt = S.buf("Bt")
    for dl in (1, 0, -1, -2, -3):
        for hd in range(2):
            def mk(dl, hd):
                Hs, BHs = C.tmp("Hs", 512, F32, 1)
                src = bass.AP(rscr_t, hd * RL + 128 * dl + 384, [[1, 128], [1, 512]])
                S.add("sync", lambda e: e.dma_start(out=Hs, in_=src), reads=[B_rscr], writes=[BHs], dma_key="hank")
                ps, Bps = C.ps()
                S.add("tensor", lambda e: e.matmul(ps[:, :], J_f, Hs, start=True, stop=True), reads=[BHs, B_cst], writes=[Bps])
                bt = C.alloc(512, F32)
                S.add("scalar", lambda e: e.activation(out=bt, in_=ps[:, :], func=AF.Copy), reads=[Bps], writes=[B_Bt])
                Bt[(dl, hd)] = bt
            mk(dl, hd)

    qn = C.alloc(4 * TP, BF16)
    B_qz = S.buf("qz")
    S.add("gpsimd", lambda e: e.memset(qn, 0.0), writes=[B_qz])
    kn = C.alloc(2 * TP, BF16)
    v_sb = C.alloc(NT * 256, BF16)
    hgroups = make_groups(TP)
    B_qn = [[S.buf("qn") for _ in hgroups] for _ in range(2)]
    B_kn = [[S.buf("kn") for _ in hgroups] for _ in range(2)]
    B_v = [S.buf("v") for _ in range(NT)]

    def qk_group(gi, t0, w, x, hd):
        ps, Bps = C.ps()
        col = x * 256 + hd * 128
        for kc in range(8):
            S.add("tensor", lambda e, kc=kc: e.matmul(ps[:, :w], wqk[:, kc * 512 + col: kc * 512 + col + 128],
                                                       hn[:, kc * TP + t0: kc * TP + t0 + w], start=(kc == 0), stop=(kc == 7)),
                  reads=[B_wqk] + hn_bufs(kc, t0, w), writes=[Bps])
        sq, Bsq = C.tmp("sq", 512, BF16, 2)
        S.add("scalar", lambda e: e.activation(out=sq[:, :w], in_=ps[:, :w], func=AF.Square), reads=[Bps], writes=[Bsq])
        pm, Bpm = C.ps()
        S.add("tensor", lambda e: e.matmul(pm[:, :w], blk64, sq[:, :w], start=True, stop=True), reads=[Bsq, B_blk], writes=[Bpm])
        rs, Brs = C.tmp("rs", 512, F32, 2)
        S.add("scalar", lambda e: e.activation(out=rs[:, :w], in_=pm[:, :w], func=AF.Ln, bias=K["eps"], scale=1.0),
              reads=[Bpm, K["B_eps"]], writes=[Brs])
        S.add("scalar", lambda e: e.activation(out=rs[:, :w], in_=rs[:, :w], func=AF.Exp, scale=-0.5), reads=[Brs], writes=[Brs])
        if x == 1:
            S.add("vector", lambda e: e.scalar_tensor_tensor(out=kn[:, hd * TP + t0: hd * TP + t0 + w], in0=ps[:, :w], scalar=prm[:, 9:10],
                                                             in1=rs[:, :w], op0=ALU.mult, op1=ALU.mult), reads=[Bps, Brs, B_prm], writes=[B_kn[hd][gi]])
        else:
            for m in range(2):
                def qwrite(m):
                    r0, r1 = m * 64, (m + 1) * 64
                    o0 = (hd * 2 + m) * TP + t0
                    S.add("vector", lambda e: e.scalar_tensor_tensor(out=qn[r0:r1, o0:o0 + w], in0=ps[r0:r1, :w], scalar=prm[r0:r1, 8:9],
                                                                     in1=rs[r0:r1, :w], op0=ALU.mult, op1=ALU.mult),
                          reads=[Bps, Brs, B_prm, B_qz], writes=[B_qn[hd][gi]])
                qwrite(m)

    for gi, (t0, w) in enumerate(hgroups):
        for x in range(2):
            for hd in range(2):
                qk_group(gi, t0, w, x, hd)

    def v_tile(tt):
        ps, Bps = C.ps()
        for kc in range(8):
            S.add("tensor", lambda e, kc=kc: e.matmul(ps[:, 0:256], hn[:, kc * TP + tt * 128: kc * TP + (tt + 1) * 128], wv[:, kc * 256:(kc + 1) * 256],
                                                       start=(kc == 0), stop=(kc == 7)), reads=[B_wv] + hn_bufs(kc, tt * 128, 128), writes=[Bps])
        S.add("vector", lambda e: e.tensor_copy(out=v_sb[:, tt * 256:(tt + 1) * 256], in_=ps[:, 0:256]), reads=[Bps], writes=[B_v[tt]])

    for tt in range(NT):
        v_tile(tt)

    acc = C.reserve(4)

    def att_group(hd, g, t0, w):
        nk = min(4 * g + 4, NT)
        (O1, BO1), (O2, BO2), (s1, Bs1), (s2, Bs2) = acc
        Os = ((O1, BO1), (O2, BO2))
        ss = ((s1, Bs1), (s2, Bs2))

        def s_unit(j, m):
            dl = 4 * g - j
            ps, Bps = C.ps()
            S.add("tensor", lambda e: e.matmul(ps[:, :w], kn[:, hd * TP + j * 128: hd * TP + (j + 1) * 128],
                                               qn[:, (hd * 2 + m) * TP + t0: (hd * 2 + m) * TP + t0 + w], start=True, stop=True),
                  reads=[B_kn[hd][j // 4], B_qn[hd][g]], writes=[Bps])
            P, BP = C.tmp("P", 512, BF16, 5)
            if dl >= 2:
                S.add("scalar", lambda e: e.activation(out=P[:, :w], in_=ps[:, :w], func=AF.Exp, bias=prm[:, 12 + hd:13 + hd], scale=0.125),
                      reads=[Bps, B_prm], writes=[BP])
            else:
                nb, Bnb = C.tmp("nb", 512, F32, 2)
                bt = Bt[(dl, hd)]
                S.add("vector", lambda e: e.scalar_tensor_tensor(out=nb[:, :w], in0=ps[:, :w], scalar=0.125, in1=bt[:, :w],
                                                                 op0=ALU.mult, op1=ALU.add), reads=[Bps, B_Bt], writes=[Bnb])
                S.add("scalar", lambda e: e.activation(out=P[:, :w], in_=nb[:, :w], func=AF.Exp), reads=[Bnb], writes=[BP])
            return P, BP

        def pv_unit(j, m, P, BP):
            Om, BOm = Os[m]
            sm, Bsm = ss[m]
            S.add("tensor", lambda e: e.matmul(Om[:, :w], v_sb[:, j * 256 + hd * 128: j * 256 + (hd + 1) * 128], P[:, :w],
                                               start=(j == 0), stop=(j == nk - 1)), reads=[B_v[j], BP], writes=[BOm])
            S.add("tensor", lambda e: e.matmul(sm[:, :w], ones1, P[:, :w], start=(j == 0), stop=(j == nk - 1)),
                  reads=[B_ones1, BP], writes=[Bsm])

        LA = 2
        pend = []
        for j in range(nk):
            for m in range(2):
                P, BP = s_unit(j, m)
                pend.append((j, m, P, BP))
                if len(pend) > LA:
                    pv_unit(*pend.pop(0))
        for u in pend:
            pv_unit(*u)
        r1, Br1 = C.tmp("r1", 512, F32, 1)
        r2, Br2 = C.tmp("r2", 512, F32, 1)
        S.add("vector", lambda e: e.reciprocal(out=r1[:, :w], in_=s1[:, :w]), reads=[Bs1], writes=[Br1])
        S.add("vector", lambda e: e.reciprocal(out=r2[:, :w], in_=s2[:, :w]), reads=[Bs2], writes=[Br2])
        a1, Ba1 = C.tmp("a1", 512, F32, 1)
        a2, Ba2 = C.tmp("a2", 512, F32, 1)
        S.add("vector", lambda e: e.tensor_tensor(out=a1[:, :w], in0=O1[:, :w], in1=r1[:, :w], op=ALU.mult), reads=[BO1, Br1], writes=[Ba1])
        S.add("vector", lambda e: e.tensor_tensor(out=a2[:, :w], in0=O2[:, :w], in1=r2[:, :w], op=ALU.mult), reads=[BO2, Br2], writes=[Ba2])
        S.add("vector", lambda e: e.scalar_tensor_tensor(out=a1[:, :w], in0=a2[:, :w], scalar=nlam[:, 0:1], in1=a1[:, :w], op0=ALU.mult, op1=ALU.add),
              reads=[Ba1, Ba2, B_nlam], writes=[Ba1])
        sq, Bsq = C.tmp("sq", 512, BF16, 2)
        S.add("scalar", lambda e: e.activation(out=sq[:, :w], in_=a1[:, :w], func=AF.Square), reads=[Ba1], writes=[Bsq])
        pm, Bpm = C.ps()
        S.add("tensor", lambda e: e.matmul(pm[:, :w], onesd, sq[:, :w], start=True, stop=True), reads=[Bsq, B_onesd], writes=[Bpm])
        rs, Brs = C.tmp("rs", 512, F32, 2)
        S.add("scalar", lambda e: e.activation(out=rs[:, :w], in_=pm[:, :w], func=AF.Ln, bias=K["eps"], scale=1.0),
              reads=[Bpm, K["B_eps"]], writes=[Brs])
        S.add("scalar", lambda e: e.activation(out=rs[:, :w], in_=rs[:, :w], func=AF.Exp, bias=lnc, scale=-0.5), reads=[Brs, B_nlam], writes=[Brs])
        uo, Buo = C.tmp("uoa", 512, BF16, 2)
        S.add("vector", lambda e: e.scalar_tensor_tensor(out=uo[:, :w], in0=a1[:, :w], scalar=prm[:, 10:11], in1=rs[:, :w], op0=ALU.mult, op1=ALU.mult),
              reads=[Ba1, Brs, B_prm], writes=[Buo])
        S.add("sync", lambda e: e.dma_start(out=dr["o_att"][hd][:, t0:t0 + w], in_=uo[:, :w]), reads=[Buo], dma_key="out")

    for hd in range(2):
        for g, (t0, w) in enumerate(groups):
            att_group(hd, g, t0, w)
    C.release()


def _rel_bucket_np(n):
    n = np.maximum(n, 0)
    nf = np.maximum(n, 1).astype(np.float32)
    large = 16 + (np.log(nf / np.float32(16)) / np.float32(math.log(128 / 16)) * np.float32(16)).astype(np.int32)
    large = np.minimum(large, 31)
    return np.where(n < 16, n, large)


_CONST_CACHE = {}


def phase1_consts():
    if "c" in _CONST_CACHE:
        return _CONST_CACHE["c"]
    cst = np.zeros((128, 896), np.float32)
    cst[:, 0:128] = np.eye(128, dtype=np.float32)
    cst[:, 128:256] = np.eye(128, dtype=np.float32)[::-1]
    s_i = np.arange(128)[:, None]
    t_i = np.arange(128)[None, :]
    cst[:, 256:384] = ((s_i // 64 == t_i // 64) & (s_i <= t_i)).astype(np.float32)
    cst[:, 384:896] = (np.arange(512) % 64 != 0).astype(np.float32)[None, :]
    oh = np.zeros((33, RL), np.float32)
    n = np.arange(RL) - 511
    bk = _rel_bucket_np(n)
    for i in range(RL):
        if n[i] >= 0:
            oh[bk[i], i] = 1.0
        else:
            oh[32, i] = -30000.0
    _CONST_CACHE["c"] = (cst, oh)
    return cst, oh


def phase1_inputs(d, L, h, hsT):
    w_in = d["w_in"][L]
    prm = np.zeros((128, 280), np.float32)
    prm[:, 0:8] = d["norm1_gain"][L].reshape(8, 128).T
    prm[:, 8] = np.tile(d["q_norm_gain"][L], 2)
    prm[:, 9] = np.tile(d["k_norm_gain"][L], 2)
    prm[:, 10] = d["attn_sub_gain"][L]
    prm[:, 11] = d["hgrn_out_gain"][L]
    for hd in range(2):
        prm[:, 12 + hd] = d["rel_bias"][31, 2 * h + hd]
        for lp in range(2):
            prm[:, 20 + hd * 2 + lp] = d["hgrn_lb_logits"][lp, (2 * h + hd) * 128:(2 * h + hd + 1) * 128]
    for ci in range(2):
        for k in range(3):
            prm[:, 14 + ci * 3 + k] = d["conv_w"][L][k, 256 * h + ci * 128: 256 * h + (ci + 1) * 128]
    prm[:, 24:280] = d["diff_lambda"][L].reshape(1, 256)
    cst, oh = phase1_consts()
    qk_cols = []
    for base in (0, 512):
        for hd in range(2):
            for m in range(2):
                c0 = base + m * 256 + (2 * h + hd) * 64
                qk_cols.append(np.arange(c0, c0 + 64))
    qk_cols = np.concatenate(qk_cols)
    sl = lambda base: w_in[:, base + 256 * h: base + 256 * (h + 1)]
    return {
        "hsT": hsT, "params": prm, "consts": cst, "oh": oh,
        "relb": np.ascontiguousarray(d["rel_bias"][:, 2 * h:2 * h + 2]),
        "wqk": np.ascontiguousarray(w_in[:, qk_cols]),
        "wv": np.ascontiguousarray(sl(1024)),
        "wconv": np.ascontiguousarray(np.concatenate([sl(1536), sl(2048), sl(2560)], axis=1)),
        "whg": np.ascontiguousarray(np.concatenate([sl(3072), sl(3584), sl(4608)], axis=1)),
        "wi": np.ascontiguousarray(sl(4096)),
    }


_NC_CACHE = {}


def _get_nc(key, fn):
    if key not in _NC_CACHE:
        _NC_CACHE[key] = fn()
    return _NC_CACHE[key]


def _lay_gain(g):
    return np.ascontiguousarray(g.reshape(8, 128).T)


P1_W = (("wqk", 512), ("wv", 256), ("wconv", 768), ("whg", 768), ("wi", 256))


def build_fused():
    T = T_ALL
    nc = bass.Bass("TRN2", target_bir_lowering=False)
    inp = lambda name, shape, dt=F32: nc.dram_tensor(name, shape, dt, kind="ExternalInput").ap()
    g = {}
    g["hsT"] = inp("hsT", [D, T])
    g["consts"] = inp("consts", [128, 896])
    g["oh"] = inp("oh", [33, RL])
    g["selv"] = inp("selv", [128, 2])
    for h in range(2):
        g[f"relb_{h}"] = inp(f"relb_{h}", [32, 2])
    for L in range(2):
        for h in range(2):
            g[f"params_{L}_{h}"] = inp(f"params_{L}_{h}", [128, 280])
            for nm, wd in P1_W:
                g[f"{nm}_{L}_{h}"] = inp(f"{nm}_{L}_{h}", [D, wd])
        g[f"wgate_{L}"] = inp(f"wgate_{L}", [D, 3072])
        g[f"wbr_{L}"] = inp(f"wbr_{L}", [1536, D])
        g[f"wout_{L}"] = inp(f"wout_{L}", [D, D])
        g[f"gains_{L}"] = inp(f"gains_{L}", [128, 16])
    g["fg0"] = inp("fg0", [1, D, 2816])
    g["fu0"] = inp("fu0", [1, D, 2816])
    g["fd0"] = inp("fd0", [1, 2816, D])
    g["fg1"] = inp("fg1", [8, D, 3584])
    g["fu1"] = inp("fu1", [8, D, 3584])
    g["fd1"] = inp("fd1", [8, 3584, D])
    g["wr"] = inp("wr", [D, 8])
    g["sel"] = inp("sel", [8, 1024])
    g["ident"] = inp("ident", [128, 128])
    out = nc.dram_tensor("out", [D, 2048], F32, kind="ExternalOutput").ap()
    rscr_t = nc.dram_tensor("rscr", [2, RL], F32, kind="Internal")
    ikind = "ExternalOutput" if FDEBUG else "Internal"
    u_scr = [nc.dram_tensor(f"u_scr{L}", [1536, T], BF16, kind=ikind).ap() for L in range(2)]
    hs1 = nc.dram_tensor("hs1", [D, T], F32, kind=ikind).ap()

    with contextlib.ExitStack() as st:
        C = Ctx(nc, st)
        K = emit_consts(C)
        for L in range(2):
            hs_src = g["hsT"] if L == 0 else hs1
            for h in range(2):
                dr = {"hsT": hs_src, "params": g[f"params_{L}_{h}"], "consts": g["consts"], "oh": g["oh"], "relb": g[f"relb_{h}"],
                      "rscr": rscr_t.ap(), "rscr_t": rscr_t}
                for nm, _ in P1_W:
                    dr[nm] = g[f"{nm}_{L}_{h}"]
                u = u_scr[L]
                dr["o_att"] = [u[256 * h + hd * 128: 256 * h + (hd + 1) * 128, :] for hd in range(2)]
                dr["o_conv"] = [u[512 + 256 * h + ci * 128: 512 + 256 * h + (ci + 1) * 128, :] for ci in range(2)]
                dr["o_hgrn"] = [u[1024 + 256 * h + hd * 128: 1024 + 256 * h + (hd + 1) * 128, :] for hd in range(2)]
                emit_phase1(C, K, dr, L)
            base = {"wgate": g[f"wgate_{L}"], "wbr": g[f"wbr_{L}"], "wout": g[f"wout_{L}"], "gains": g[f"gains_{L}"]}
            if L == 0:
                for (a0, a1) in ((0, 2064), (2064, 4112)):
                    dr = dict(base)
                    dr.update({"hsT": g["hsT"][:, a0:a1], "uT": u_scr[0][:, a0:a1], "out": hs1[:, a0:a1],
                               "fg": g["fg0"], "fu": g["fu0"], "fd": g["fd0"]})
                    emit_phase2(C, K, dr, a1 - a0, False)
            else:
                dr = dict(base)
                dr.update({"hsT": (hs1[:, 16:2064], hs1[:, 2064:4112]), "uT": (u_scr[1][:, 16:2064], u_scr[1][:, 2064:4112]),
                           "out": out, "selv": g["selv"], "fg": g["fg1"], "fu": g["fu1"], "fd": g["fd1"],
                           "wr": g["wr"], "sel": g["sel"], "ident": g["ident"]})
                emit_phase2(C, K, dr, 2048, True)
        C.S.finalize(final_dma_keys=["out"])
        print("fused ops", len(C.S.ops), "sig", C.S.max_counts, "dma", max(C.S.dma_counts.values()))
    return nc


def kernel(**inputs):
    d = {k: np.asarray(v) for k, v in inputs.items()}
    x = d["x"].astype(np.float32, copy=False)
    B = x.shape[0]
    cores = list(range(8))
    nc = _get_nc("fused", build_fused)
    hsT = [np.ascontiguousarray(np.concatenate([d["meta_tokens"].astype(np.float32), x[b]], axis=0).T) for b in range(B)]
    sel = np.zeros((8, 1024), np.float32)
    for e in range(8):
        sel[e, e * 128:(e + 1) * 128] = 1.0
    cst, oh = phase1_consts()
    common = {"consts": cst, "oh": oh, "sel": sel, "ident": np.eye(128, dtype=np.float32),
              "fg0": d["ffn_w_gate"][0:1], "fu0": d["ffn_w_up"][0:1], "fd0": d["ffn_w_down"][0:1],
              "fg1": d["moe_w_gate"][0], "fu1": d["moe_w_up"][0], "fd1": d["moe_w_down"][0],
              "wr": np.ascontiguousarray(d["router_w"][0])}
    for L in range(2):
        for h in range(2):
            pi = phase1_inputs(d, L, h, None)
            common[f"params_{L}_{h}"] = pi["params"]
            common[f"relb_{h}"] = pi["relb"]
            for nm, _ in P1_W:
                common[f"{nm}_{L}_{h}"] = pi[nm]
        common[f"wgate_{L}"] = np.ascontiguousarray(d["w_in"][L][:, 5120:])
        common[f"wbr_{L}"] = np.ascontiguousarray(d["w_branch"][L].reshape(1536, 1024))
        common[f"wout_{L}"] = np.ascontiguousarray(d["w_out"][L])
        common[f"gains_{L}"] = np.concatenate([_lay_gain(d["norm1_gain"][L]), _lay_gain(d["norm2_gain"][L])], axis=1)
    ins = []
    for c in cores:
        b, h = c // 2, c % 2
        m = dict(common)
        m["hsT"] = hsT[b]
        selv = np.zeros((128, 2), np.float32)
        selv[:, 0] = 1.0 - h
        selv[:, 1] = float(h)
        m["selv"] = selv
        ins.append(m)
    if FDEBUG:
        return run_bass_kernel_spmd(nc, ins, core_ids=cores)
    res = run_bass_kernel_spmd(nc, ins, core_ids=cores)
    out = np.stack([np.concatenate([res.results[2 * b]["out"], res.results[2 * b + 1]["out"]], axis=1).T for b in range(B)])
    return np.ascontiguousarray(out.astype(np.float32))
```

```python
import contextlib
import math
import numpy as np
import ml_dtypes
import concourse.bass as bass
import concourse.mybir as mybir
from concourse.bass_utils import run_bass_kernel_spmd

F32 = mybir.dt.float32
BF16 = mybir.dt.bfloat16
AF = mybir.ActivationFunctionType
ALU = mybir.AluOpType
AX = mybir.AxisListType

D = 1024
T_ALL = 4112
N_META = 16
EPS = 1e-6
DEBUG = False
FDEBUG = False
HG_BF16_STATE = False
DEBUG_G = 0
ENGINES = ("tensor", "vector", "scalar", "gpsimd", "sync")


class Buf:
    __slots__ = ("name", "last_w", "readers")

    def __init__(self, name):
        self.name = name
        self.last_w = None
        self.readers = []


class Op:
    __slots__ = ("eng", "fn", "deps", "is_dma", "dma_key", "signal", "sig_cnt", "idx", "dma_deps", "epoch")

    def __init__(self, eng, fn, is_dma=False, dma_key=None):
        self.eng = eng
        self.fn = fn
        self.deps = []
        self.dma_deps = []
        self.is_dma = is_dma
        self.dma_key = dma_key
        self.signal = False
        self.sig_cnt = 0


class Sched:
    def __init__(self, nc):
        self.nc = nc
        self.ops = []
        self.dma_counts = {}
        self.all_bufs = []
        self.epoch = 0

    def buf(self, name="b"):
        b = Buf(name)
        self.all_bufs.append(b)
        return b

    def add(self, eng, fn, reads=(), writes=(), dma_key=None):
        is_dma = dma_key is not None
        op = Op(eng, fn, is_dma, dma_key)
        op.idx = len(self.ops)
        op.epoch = self.epoch
        deps = {}

        def add_dep(p, kind):
            if p is None or p is op:
                return
            if p.eng == eng and not p.is_dma and not is_dma:
                if eng == "tensor" or kind != "RAW":
                    return
            deps[p.idx] = p

        for b in reads:
            add_dep(b.last_w, "RAW")
        for b in writes:
            add_dep(b.last_w, "WAW")
            for r in b.readers:
                add_dep(r, "WAR")
        for b in reads:
            b.readers.append(op)
        for b in writes:
            b.last_w = op
            b.readers = []
        for p in deps.values():
            if p.is_dma:
                op.dma_deps.append((p.dma_key, self.dma_counts[p.dma_key]))
            else:
                p.signal = True
                op.deps.append(p)
        if is_dma:
            self.dma_counts[dma_key] = self.dma_counts.get(dma_key, 0) + 16
        self.ops.append(op)
        return op

    def fence(self, new_epoch=False):
        last = {}
        for op in self.ops:
            if not op.is_dma and op.fn is not None:
                last[op.eng] = op
        dma_now = dict(self.dma_counts)
        for e in ENGINES:
            op = Op(e, None)
            op.idx = len(self.ops)
            op.epoch = self.epoch
            for pe, p in last.items():
                if pe != e:
                    p.signal = True
                    op.deps.append(p)
            for k, c in dma_now.items():
                op.dma_deps.append((k, c))
            self.ops.append(op)
        for b in self.all_bufs:
            b.last_w = None
            b.readers = []
        if new_epoch:
            self.epoch += 1

    def finalize(self, final_dma_keys=()):
        nc = self.nc
        cnt = {}
        for op in self.ops:
            if op.signal and not op.is_dma:
                k = (op.epoch, op.eng)
                cnt[k] = cnt.get(k, 0) + 1
                op.sig_cnt = cnt[k]
        self.max_counts = max(cnt.values()) if cnt else 0
        with contextlib.ExitStack() as st:
            sems = {(ep, e): st.enter_context(nc.semaphore(f"s_{e}{ep}")) for ep in range(self.epoch + 1) for e in ENGINES
                    if (ep, e) in cnt}
            dsems = {k: st.enter_context(nc.semaphore(f"d_{k}")) for k in self.dma_counts}
            block = st.enter_context(nc.Block())
            per_eng = {e: [op for op in self.ops if op.eng == e] for e in ENGINES}

            def emit(eng_name, eng):
                waited = {}
                for op in per_eng[eng_name]:
                    for p in op.deps:
                        key = ("c", p.epoch, p.eng)
                        if waited.get(key, 0) < p.sig_cnt:
                            eng.wait_ge(sems[(p.epoch, p.eng)], p.sig_cnt)
                            waited[key] = p.sig_cnt
                    for (k, c) in op.dma_deps:
                        key = ("d", k)
                        if waited.get(key, 0) < c:
                            eng.wait_ge(dsems[k], c)
                            waited[key] = c
                    if op.fn is None:
                        continue
                    ins = op.fn(eng)
                    if op.is_dma:
                        ins.then_inc(dsems[op.dma_key], 16)
                    elif op.signal:
                        ins.then_inc(sems[(op.epoch, eng_name)], 1)
                if eng_name == "sync":
                    for k in final_dma_keys:
                        eng.wait_ge(dsems[k], self.dma_counts[k])

            block.tensor(lambda e: emit("tensor", e))
            block.vector(lambda e: emit("vector", e))
            block.scalar(lambda e: emit("scalar", e))
            block.gpsimd(lambda e: emit("gpsimd", e))
            block.sync(lambda e: emit("sync", e))


class Ctx:
    ARENA_BYTES = 212480

    def __init__(self, nc, st):
        self.nc = nc
        self.st = st
        self.S = Sched(nc)
        self.arena = st.enter_context(nc.sbuf_tensor("arena", [128, self.ARENA_BYTES // 4], F32))
        self.arena_bf = self.arena.bitcast(BF16)
        self.off = 0
        self.psum = []
        for i in range(8):
            t = st.enter_context(nc.psum_tensor(f"ps{i}", [128, 512], F32))
            self.psum.append((t, self.S.buf(f"ps{i}")))
        self.pi = 0
        self.pool = list(range(8))
        self.tmps = {}

    def alloc(self, nelem, dt):
        sz = 4 if dt == F32 else 2
        nbytes = (nelem * sz + 3) // 4 * 4
        assert self.off + nbytes <= self.ARENA_BYTES, f"arena overflow {self.off + nbytes}"
        o = self.off
        self.off += nbytes
        if dt == F32:
            return self.arena[:, o // 4: o // 4 + nelem]
        return self.arena_bf[:, o // 2: o // 2 + nelem]

    def mark(self):
        return self.off

    def reset(self, m):
        self.off = m

    def ps(self):
        self.pi = (self.pi + 1) % len(self.pool)
        return self.psum[self.pool[self.pi]]

    def reserve(self, k):
        r = [self.psum[i] for i in self.pool[-k:]]
        self.pool = self.pool[:-k]
        self.pi = 0
        return r

    def release(self):
        self.pool = list(range(8))
        self.pi = 0

    def tmp(self, kind, nelem, dt, n=2):
        if kind not in self.tmps:
            self.tmps[kind] = [[(self.alloc(nelem, dt), self.S.buf(f"{kind}{i}")) for i in range(n)], 0]
        lst, i = self.tmps[kind]
        self.tmps[kind][1] = (i + 1) % len(lst)
        return lst[i]

    def drop_tmps(self):
        self.tmps = {}


GS = 512


def make_groups(n, gs=None):
    gs = gs or GS
    g = []
    t = 0
    while t < n:
        w = min(gs, n - t)
        g.append((t, w))
        t += w
    return g


def kview(ap, p=128):
    return ap.rearrange("(kc p) n -> p kc n", p=p)


def emit_consts(C):
    S = C.S
    k = {}
    k["ones"] = C.alloc(128, BF16)
    k["B_ones"] = S.buf("ones")
    S.add("vector", lambda e: e.memset(k["ones"], 1.0 / 1024.0), writes=[k["B_ones"]])
    k["eps"] = C.alloc(1, F32)
    k["B_eps"] = S.buf("eps")
    S.add("vector", lambda e: e.memset(k["eps"], EPS), writes=[k["B_eps"]])
    return k


def emit_rmsnorm_group(C, K, hs, N, B_hs, gain, B_gain, out, out_stride, B_out, gi, t0, w, out_t0):
    S = C.S
    ps, Bps = C.ps()
    for c in range(8):
        sq, Bsq = C.tmp("sq", 512, BF16, 2)
        S.add("scalar", lambda e, c=c, sq=sq: e.activation(out=sq[:, :w], in_=hs[:, c * N + t0: c * N + t0 + w], func=AF.Square),
              reads=[B_hs[c][gi]], writes=[Bsq])
        S.add("tensor", lambda e, c=c, sq=sq: e.matmul(ps[:, :w], K["ones"], sq[:, :w], start=(c == 0), stop=(c == 7)),
              reads=[Bsq, K["B_ones"]], writes=[Bps])
    rstd, Brstd = C.tmp("rstd", 512, F32, 2)
    S.add("scalar", lambda e: e.activation(out=rstd[:, :w], in_=ps[:, :w], func=AF.Ln, bias=K["eps"], scale=1.0),
          reads=[Bps, K["B_eps"]], writes=[Brstd])
    S.add("scalar", lambda e: e.activation(out=rstd[:, :w], in_=rstd[:, :w], func=AF.Exp, scale=-0.5),
          reads=[Brstd], writes=[Brstd])
    for c in range(8):
        S.add("vector", lambda e, c=c: e.scalar_tensor_tensor(
            out=out[:, c * out_stride + out_t0: c * out_stride + out_t0 + w], in0=hs[:, c * N + t0: c * N + t0 + w],
            scalar=gain[:, c:c + 1], in1=rstd[:, :w], op0=ALU.mult, op1=ALU.mult),
            reads=[B_hs[c][gi], Brstd, B_gain], writes=[B_out[c]])


def build_phase2(N, moe):
    nc = bass.Bass("TRN2", target_bir_lowering=False)
    dr = {}
    dr["hsT"] = nc.dram_tensor("hsT", [D, N], F32, kind="ExternalInput").ap()
    dr["uT"] = nc.dram_tensor("uT", [1536, N], BF16, kind="ExternalInput").ap()
    dr["wgate"] = nc.dram_tensor("wgate", [D, 3072], F32, kind="ExternalInput").ap()
    dr["wbr"] = nc.dram_tensor("wbr", [1536, D], F32, kind="ExternalInput").ap()
    dr["wout"] = nc.dram_tensor("wout", [D, D], F32, kind="ExternalInput").ap()
    dr["gains"] = nc.dram_tensor("gains", [128, 16], F32, kind="ExternalInput").ap()
    if moe:
        FF = 3584
        dr["fg"] = nc.dram_tensor("fg", [8, D, FF], F32, kind="ExternalInput").ap()
        dr["fu"] = nc.dram_tensor("fu", [8, D, FF], F32, kind="ExternalInput").ap()
        dr["fd"] = nc.dram_tensor("fd", [8, FF, D], F32, kind="ExternalInput").ap()
        dr["wr"] = nc.dram_tensor("wr", [D, 8], F32, kind="ExternalInput").ap()
        dr["sel"] = nc.dram_tensor("sel", [8, 8 * 128], F32, kind="ExternalInput").ap()
        dr["ident"] = nc.dram_tensor("ident", [128, 128], F32, kind="ExternalInput").ap()
    else:
        FF = 2816
        dr["fg"] = nc.dram_tensor("fg", [1, D, FF], F32, kind="ExternalInput").ap()
        dr["fu"] = nc.dram_tensor("fu", [1, D, FF], F32, kind="ExternalInput").ap()
        dr["fd"] = nc.dram_tensor("fd", [1, FF, D], F32, kind="ExternalInput").ap()
    dr["out"] = nc.dram_tensor("hsT_out", [D, N], F32, kind="ExternalOutput").ap()
    if DEBUG:
        dr["dbg1"] = nc.dram_tensor("dbg1", [128, 8 * 512], BF16, kind="ExternalOutput").ap()
        dr["dbg2"] = nc.dram_tensor("dbg2", [D, N], F32, kind="ExternalOutput").ap()
        dr["dbg3"] = nc.dram_tensor("dbg3", [128, 8 * 512], BF16, kind="ExternalOutput").ap()
        dr["dbg4"] = nc.dram_tensor("dbg4", [128, 512], F32, kind="ExternalOutput").ap()
        dr["dbg5"] = nc.dram_tensor("dbg5", [128, 512], F32, kind="ExternalOutput").ap()

    with contextlib.ExitStack() as st:
        C = Ctx(nc, st)
        K = emit_consts(C)
        emit_phase2(C, K, dr, N, moe)
        C.S.finalize(final_dma_keys=["out"])
        print("phase2 ops", len(C.S.ops), "sig", C.S.max_counts)
    return nc


def emit_phase2(C, K, dr, N, moe):
    S = C.S
    FF = 3584 if moe else 2816
    m_base = C.mark()
    groups = make_groups(N)
    NG = len(groups)
    hs = C.alloc(8 * N, F32)
    B_hs = [[S.buf(f"hs{c}_{g}") for g in range(NG)] for c in range(8)]
    gains = C.alloc(16, F32)
    B_gain = S.buf("gains")
    S.add("sync", lambda e: e.dma_start(out=gains, in_=dr["gains"]), writes=[B_gain], dma_key="gains")
    selv = None
    if isinstance(dr["hsT"], tuple):
        selv = C.alloc(2, F32)
        B_selv = S.buf("selv")
        S.add("sync", lambda e: e.dma_start(out=selv, in_=dr["selv"]), writes=[B_selv], dma_key="selv")
        hvA, hvB = kview(dr["hsT"][0]), kview(dr["hsT"][1])
        m_tmp = C.mark()
        tb = C.alloc(N, F32)
        B_tb = S.buf("tb")

        def hs_blend(c):
            S.add("sync", lambda e: e.dma_start(out=hs[:, c * N:(c + 1) * N], in_=hvA[:, c, :]), writes=B_hs[c], dma_key="hs_in")
            S.add("sync", lambda e: e.dma_start(out=tb, in_=hvB[:, c, :]), writes=[B_tb], dma_key="hs_inb")
            S.add("vector", lambda e: e.tensor_scalar(tb, tb, selv[:, 1:2], None, ALU.mult), reads=[B_tb, B_selv], writes=[B_tb])
            S.add("vector", lambda e: e.scalar_tensor_tensor(out=hs[:, c * N:(c + 1) * N], in0=hs[:, c * N:(c + 1) * N], scalar=selv[:, 0:1],
                                                             in1=tb, op0=ALU.mult, op1=ALU.add), reads=B_hs[c] + [B_tb, B_selv], writes=B_hs[c])
        for c in range(8):
            hs_blend(c)
        S.fence()
        C.reset(m_tmp)
    else:
        hsv = kview(dr["hsT"])
        for c in range(8):
            S.add("sync", lambda e, c=c: e.dma_start(out=hs[:, c * N:(c + 1) * N], in_=hsv[:, c, :]),
                  writes=B_hs[c], dma_key="hs_in")
    m_persist = C.mark()

    wg = C.alloc(8 * 3072, BF16)
    wb = C.alloc(12 * 1024, BF16)
    wo = C.alloc(8 * 1024, BF16)
    B_wg, B_wb, B_wo = S.buf("wg"), S.buf("wb"), S.buf("wo")
    wgv = kview(dr["wgate"])
    for b in range(3):
        S.add("gpsimd", lambda e, b=b: e.dma_start(
            out=wg.rearrange("p (k n) -> p k n", k=8)[:, :, b * 1024:(b + 1) * 1024], in_=wgv[:, :, b * 1024:(b + 1) * 1024]),
            writes=[B_wg], dma_key="wg")
    S.add("gpsimd", lambda e: e.dma_start(out=wb.rearrange("p (k n) -> p k n", k=12), in_=kview(dr["wbr"])),
          writes=[B_wb], dma_key="wb")
    S.add("gpsimd", lambda e: e.dma_start(out=wo.rearrange("p (k n) -> p k n", k=8), in_=kview(dr["wout"])),
          writes=[B_wo], dma_key="wo")
    hn_g = C.alloc(8 * 512, BF16)
    B_hng = [S.buf(f"hng{c}") for c in range(8)]
    u_g = C.alloc(12 * 512, BF16)
    B_ug = S.buf("ug")
    mixed = C.alloc(8 * 512, BF16)
    B_mixed = [S.buf(f"mixed{j}") for j in range(8)]
    if selv is None:
        uv = kview(dr["uT"])
    else:
        uvA, uvB = kview(dr["uT"][0]), kview(dr["uT"][1])
        u_b = C.alloc(12 * 512, BF16)
        B_ub = S.buf("ub")
    def stage_b_group(gi, t0, w):
        if selv is None:
            S.add("sync", lambda e: e.dma_start(
                out=u_g.rearrange("p (k n) -> p k n", k=12)[:, :, :w], in_=uv[:, :, t0:t0 + w]),
                writes=[B_ug], dma_key="ug")
        else:
            S.add("sync", lambda e: e.dma_start(
                out=u_g.rearrange("p (k n) -> p k n", k=12)[:, :, :w], in_=uvA[:, :, t0:t0 + w]), writes=[B_ug], dma_key="ug")
            S.add("sync", lambda e: e.dma_start(
                out=u_b.rearrange("p (k n) -> p k n", k=12)[:, :, :w], in_=uvB[:, :, t0:t0 + w]), writes=[B_ub], dma_key="ugb")
            S.add("vector", lambda e: e.tensor_scalar(u_b, u_b, selv[:, 1:2], None, ALU.mult), reads=[B_ub, B_selv], writes=[B_ub])
            S.add("vector", lambda e: e.scalar_tensor_tensor(out=u_g, in0=u_g, scalar=selv[:, 0:1], in1=u_b, op0=ALU.mult, op1=ALU.add),
                  reads=[B_ug, B_ub, B_selv], writes=[B_ug])
        emit_rmsnorm_group(C, K, hs, N, B_hs, gains, B_gain, hn_g, 512, B_hng, gi, t0, w, 0)
        for j in range(8):
            acc, Bacc = C.tmp("acc", 512, F32, 1)
            for b in range(3):
                psg, Bpsg = C.ps()
                for kc in range(8):
                    S.add("tensor", lambda e, kc=kc, b=b, j=j, psg=psg: e.matmul(
                        psg[:, :w], wg[:, kc * 3072 + b * 1024 + j * 128: kc * 3072 + b * 1024 + (j + 1) * 128],
                        hn_g[:, kc * 512: kc * 512 + w], start=(kc == 0), stop=(kc == 7)),
                        reads=[B_wg, B_hng[kc]], writes=[Bpsg])
                sg, Bsg = C.tmp("sg", 512, F32, 2)
                S.add("scalar", lambda e, psg=psg, sg=sg: e.activation(out=sg[:, :w], in_=psg[:, :w], func=AF.Sigmoid),
                      reads=[Bpsg], writes=[Bsg])
                psu, Bpsu = C.ps()
                for kc in range(4):
                    S.add("tensor", lambda e, kc=kc, b=b, j=j, psu=psu: e.matmul(
                        psu[:, :w], wb[:, (b * 4 + kc) * 1024 + j * 128: (b * 4 + kc) * 1024 + (j + 1) * 128],
                        u_g[:, (b * 4 + kc) * 512: (b * 4 + kc) * 512 + w], start=(kc == 0), stop=(kc == 3)),
                        reads=[B_wb, B_ug], writes=[Bpsu])
                if b == 0:
                    S.add("vector", lambda e, psu=psu, sg=sg, acc=acc: e.tensor_tensor(
                        out=acc[:, :w], in0=psu[:, :w], in1=sg[:, :w], op=ALU.mult), reads=[Bpsu, Bsg], writes=[Bacc])
                    if DEBUG and gi == DEBUG_G and j == 0:
                        S.add("sync", lambda e, sg=sg: e.dma_start(out=dr["dbg4"], in_=sg), reads=[Bsg], dma_key="dbg")
                        S.add("sync", lambda e, acc=acc: e.dma_start(out=dr["dbg5"], in_=acc), reads=[Bacc], dma_key="dbg")
                else:
                    t2, Bt2 = C.tmp("t2", 512, F32, 1)
                    S.add("vector", lambda e, psu=psu, sg=sg, t2=t2: e.tensor_tensor(
                        out=t2[:, :w], in0=psu[:, :w], in1=sg[:, :w], op=ALU.mult), reads=[Bpsu, Bsg], writes=[Bt2])
                    if b == 1:
                        S.add("vector", lambda e, t2=t2, acc=acc: e.tensor_tensor(
                            out=acc[:, :w], in0=acc[:, :w], in1=t2[:, :w], op=ALU.add), reads=[Bacc, Bt2], writes=[Bacc])
                    else:
                        S.add("vector", lambda e, t2=t2, acc=acc, j=j: e.tensor_tensor(
                            out=mixed[:, j * 512: j * 512 + w], in0=acc[:, :w], in1=t2[:, :w], op=ALU.add),
                            reads=[Bacc, Bt2], writes=[B_mixed[j]])
        if DEBUG and gi == DEBUG_G:
            S.add("sync", lambda e: e.dma_start(out=dr["dbg1"], in_=hn_g), reads=B_hng, dma_key="dbg")
            S.add("sync", lambda e: e.dma_start(out=dr["dbg3"], in_=mixed), reads=B_mixed, dma_key="dbg")
        for j2 in range(8):
            pso, Bpso = C.ps()
            for kc in range(8):
                S.add("tensor", lambda e, kc=kc, j2=j2, pso=pso: e.matmul(
                    pso[:, :w], wo[:, kc * 1024 + j2 * 128: kc * 1024 + (j2 + 1) * 128],
                    mixed[:, kc * 512: kc * 512 + w], start=(kc == 0), stop=(kc == 7)),
                    reads=[B_wo, B_mixed[kc]], writes=[Bpso])
            S.add("vector", lambda e, j2=j2, pso=pso, t0=t0: e.tensor_tensor(
                out=hs[:, j2 * N + t0: j2 * N + t0 + w], in0=pso[:, :w], in1=hs[:, j2 * N + t0: j2 * N + t0 + w], op=ALU.add),
                reads=[Bpso, B_hs[j2][gi]], writes=[B_hs[j2][gi]])

    for gi, (t0, w) in enumerate(groups):
        stage_b_group(gi, t0, w)
    if DEBUG:
        for c in range(8):
            S.add("sync", lambda e, c=c: e.dma_start(out=kview(dr["dbg2"])[:, c, :], in_=hs[:, c * N:(c + 1) * N]),
                  reads=B_hs[c], dma_key="dbg")
    S.fence()
    C.reset(m_persist)
    C.drop_tmps()
    hn2 = C.alloc(8 * N, BF16)
    B_hn2 = [[S.buf(f"hn2_{c}_{g}") for g in range(NG)] for c in range(8)]
    for gi, (t0, w) in enumerate(groups):
        emit_rmsnorm_group(C, K, hs, N, B_hs, gains[:, 8:16], B_gain, hn2, N, [B_hn2[c][gi] for c in range(8)], gi, t0, w, t0)
    n_exp = 8 if moe else 1
    nch = FF // 128
    slabs = []
    c0 = 0
    while c0 < nch:
        n = min(4, nch - c0)
        slabs.append((c0, n))
        c0 += n
    NB = 2
    wgs = [C.alloc(8 * 512, BF16) for _ in range(NB)]
    wus = [C.alloc(8 * 512, BF16) for _ in range(NB)]
    wds = [C.alloc(4 * 1024, BF16) for _ in range(NB)]
    B_wgs = [S.buf("wgs") for _ in range(NB)]
    B_wus = [S.buf("wus") for _ in range(NB)]
    B_wds = [S.buf("wds") for _ in range(NB)]
    acts = [C.alloc(4 * 512, BF16) for _ in range(2)]
    B_act = [[S.buf("act") for _ in range(4)] for _ in range(2)]
    if moe:
        emit_router(C, K, dr, hn2, B_hn2, N, groups)
    def stage_d_group(ex, n, sl, a, gi, t0, w):
        act = acts[a]
        if moe:
            cb, Bcb = emit_cb(C, K, ex, gi, t0, w)
        for i in range(n):
            ps1, Bps1 = C.ps()
            for kc in range(8):
                S.add("tensor", lambda e, kc=kc, i=i, ps1=ps1, sl=sl, t0=t0: e.matmul(
                    ps1[:, :w], wgs[sl][:, kc * 512 + i * 128: kc * 512 + (i + 1) * 128],
                    hn2[:, kc * N + t0: kc * N + t0 + w], start=(kc == 0), stop=(kc == 7)),
                    reads=[B_wgs[sl], B_hn2[kc][gi]], writes=[Bps1])
            ps2, Bps2 = C.ps()
            for kc in range(8):
                S.add("tensor", lambda e, kc=kc, i=i, ps2=ps2, sl=sl, t0=t0: e.matmul(
                    ps2[:, :w], wus[sl][:, kc * 512 + i * 128: kc * 512 + (i + 1) * 128],
                    hn2[:, kc * N + t0: kc * N + t0 + w], start=(kc == 0), stop=(kc == 7)),
                    reads=[B_wus[sl], B_hn2[kc][gi]], writes=[Bps2])
            sl_t, Bsl = C.tmp("silu", 512, F32, 2)
            S.add("scalar", lambda e, ps1=ps1, sl_t=sl_t: e.activation(out=sl_t[:, :w], in_=ps1[:, :w], func=AF.Silu),
                  reads=[Bps1], writes=[Bsl])
            if moe:
                S.add("vector", lambda e, sl_t=sl_t, cb=cb: e.tensor_tensor(
                    out=sl_t[:, :w], in0=sl_t[:, :w], in1=cb[:, :w], op=ALU.mult), reads=[Bsl, Bcb], writes=[Bsl])
            S.add("vector", lambda e, ps2=ps2, sl_t=sl_t, act=act, i=i: e.tensor_tensor(
                out=act[:, i * 512: i * 512 + w], in0=ps2[:, :w], in1=sl_t[:, :w], op=ALU.mult),
                reads=[Bps2, Bsl], writes=[B_act[a][i]])
        for j in range(8):
            psd, Bpsd = C.ps()
            for i in range(n):
                S.add("tensor", lambda e, i=i, j=j, psd=psd, sl=sl, act=act: e.matmul(
                    psd[:, :w], wds[sl][:, i * 1024 + j * 128: i * 1024 + (j + 1) * 128],
                    act[:, i * 512: i * 512 + w], start=(i == 0), stop=(i == n - 1)),
                    reads=[B_wds[sl], B_act[a][i]], writes=[Bpsd])
            S.add("vector", lambda e, j=j, psd=psd, t0=t0: e.tensor_tensor(
                out=hs[:, j * N + t0: j * N + t0 + w], in0=psd[:, :w], in1=hs[:, j * N + t0: j * N + t0 + w], op=ALU.add),
                reads=[Bpsd, B_hs[j][gi]], writes=[B_hs[j][gi]])

    si = 0
    ai = 0
    for ex in range(n_exp):
        for (c0, n) in slabs:
            sl = si % NB
            si += 1
            S.add("gpsimd", lambda e, ex=ex, c0=c0, n=n, sl=sl: e.dma_start(
                out=wgs[sl].rearrange("p (k n) -> p k n", k=8)[:, :, :n * 128],
                in_=kview(dr["fg"][ex])[:, :, c0 * 128:(c0 + n) * 128]), writes=[B_wgs[sl]], dma_key=f"wgs{sl}")
            S.add("gpsimd", lambda e, ex=ex, c0=c0, n=n, sl=sl: e.dma_start(
                out=wus[sl].rearrange("p (k n) -> p k n", k=8)[:, :, :n * 128],
                in_=kview(dr["fu"][ex])[:, :, c0 * 128:(c0 + n) * 128]), writes=[B_wus[sl]], dma_key=f"wus{sl}")
            S.add("gpsimd", lambda e, ex=ex, c0=c0, n=n, sl=sl: e.dma_start(
                out=wds[sl].rearrange("p (k n) -> p k n", k=4)[:, :n, :],
                in_=kview(dr["fd"][ex][c0 * 128:(c0 + n) * 128, :])), writes=[B_wds[sl]], dma_key=f"wds{sl}")
            for gi, (t0, w) in enumerate(groups):
                a = ai % 2
                ai += 1
                stage_d_group(ex, n, sl, a, gi, t0, w)
    ov = kview(dr["out"])
    for c in range(8):
        S.add("sync", lambda e, c=c: e.dma_start(out=ov[:, c, :], in_=hs[:, c * N:(c + 1) * N]),
              reads=B_hs[c], dma_key="out")
    S.fence(new_epoch=True)
    C.reset(m_base)
    C.drop_tmps()


def emit_router(C, K, dr, hn2, B_hn2, N, groups):
    S = C.S
    wr = C.alloc(8 * 8, BF16)
    B_wr = S.buf("wr")
    S.add("gpsimd", lambda e: e.dma_start(out=wr.rearrange("p (k n) -> p k n", k=8), in_=kview(dr["wr"])), writes=[B_wr], dma_key="wr")
    ident = C.alloc(128, F32)
    B_id = S.buf("ident")
    S.add("sync", lambda e: e.dma_start(out=ident, in_=dr["ident"]), writes=[B_id], dma_key="ident")
    sel = C.alloc(1024, BF16)
    B_sel = S.buf("sel")
    S.add("gpsimd", lambda e: e.dma_start(out=sel[0:8, :], in_=dr["sel"]), writes=[B_sel], dma_key="sel")
    combT = C.alloc(N, BF16)
    B_combT = [S.buf(f"combT{g}") for g in range(len(groups))]
    K["sel"], K["B_sel"], K["combT"], K["B_combT"] = sel, B_sel, combT, B_combT

    def tile(tt):
        c0 = tt * 128
        gi = c0 // 512
        ps, Bps = C.ps()
        for kc in range(8):
            S.add("tensor", lambda e, kc=kc: e.matmul(ps[:, 0:8], hn2[:, kc * N + c0: kc * N + c0 + 128], wr[:, kc * 8:(kc + 1) * 8],
                                                       start=(kc == 0), stop=(kc == 7)), reads=[B_hn2[kc][gi], B_wr], writes=[Bps])
        lg, Blg = C.tmp("lg", 8, F32, 2)
        S.add("vector", lambda e: e.tensor_copy(out=lg, in_=ps[:, 0:8]), reads=[Bps], writes=[Blg])
        mx, Bmx = C.tmp("mx", 8, F32, 2)
        S.add("vector", lambda e: e.max(out=mx, in_=lg), reads=[Blg], writes=[Bmx])
        nm1, Bnm1 = C.tmp("nm1", 1, F32, 2)
        S.add("vector", lambda e: e.tensor_scalar(nm1, mx[:, 0:1], -1.0, None, ALU.mult), reads=[Bmx], writes=[Bnm1])
        ex, Bex = C.tmp("ex", 8, F32, 2)
        S.add("scalar", lambda e: e.activation(out=ex, in_=lg, func=AF.Exp, bias=nm1, scale=1.0), reads=[Blg, Bnm1], writes=[Bex])
        mask, Bmask = C.tmp("mask", 8, F32, 2)
        S.add("vector", lambda e: e.tensor_scalar(mask, lg, mx[:, 1:2], None, ALU.is_ge), reads=[Blg, Bmx], writes=[Bmask])
        num, Bnum = C.tmp("num", 8, F32, 2)
        S.add("vector", lambda e: e.tensor_tensor(out=num, in0=mask, in1=ex, op=ALU.mult), reads=[Bmask, Bex], writes=[Bnum])
        den, Bden = C.tmp("den", 1, F32, 2)
        S.add("vector", lambda e: e.tensor_reduce(out=den, in_=num, axis=AX.X, op=ALU.add), reads=[Bnum], writes=[Bden])
        S.add("vector", lambda e: e.reciprocal(out=den, in_=den), reads=[Bden], writes=[Bden])
        comb, Bcomb = C.tmp("comb", 8, F32, 2)
        S.add("vector", lambda e: e.tensor_scalar(comb, num, den[:, 0:1], None, ALU.mult), reads=[Bnum, Bden], writes=[Bcomb])
        pt, Bpt = C.ps()
        S.add("tensor", lambda e: e.matmul(pt[0:8, 0:128], comb, ident, start=True, stop=True), reads=[Bcomb, B_id], writes=[Bpt])
        S.add("scalar", lambda e: e.activation(out=combT[0:8, c0:c0 + 128], in_=pt[0:8, 0:128], func=AF.Copy),
              reads=[Bpt], writes=[B_combT[gi]])

    for tt in range(N // 128):
        tile(tt)


def emit_cb(C, K, ex, gi, t0, w):
    S = C.S
    ps, Bps = C.ps()
    S.add("tensor", lambda e: e.matmul(ps[:, :w], K["sel"][0:8, ex * 128:(ex + 1) * 128], K["combT"][0:8, t0:t0 + w], start=True, stop=True),
          reads=[K["B_sel"], K["B_combT"][gi]], writes=[Bps])
    cb, Bcb = C.tmp("cb", 512, F32, 2)
    S.add("scalar", lambda e: e.activation(out=cb[:, :w], in_=ps[:, :w], func=AF.Copy), reads=[Bps], writes=[Bcb])
    return cb, Bcb


TP = 4224
NT = 33
RL = 1152


def build_phase1(layer, parts=("conv", "hgrn", "att")):
    T = T_ALL
    nc = bass.Bass("TRN2", target_bir_lowering=False)
    dr = {}
    dr["hsT"] = nc.dram_tensor("hsT", [D, T], F32, kind="ExternalInput").ap()
    dr["params"] = nc.dram_tensor("params", [128, 280], F32, kind="ExternalInput").ap()
    dr["consts"] = nc.dram_tensor("consts", [128, 896], F32, kind="ExternalInput").ap()
    dr["oh"] = nc.dram_tensor("oh", [33, RL], F32, kind="ExternalInput").ap()
    dr["relb"] = nc.dram_tensor("relb", [32, 2], F32, kind="ExternalInput").ap()
    dr["wqk"] = nc.dram_tensor("wqk", [D, 512], F32, kind="ExternalInput").ap()
    dr["wv"] = nc.dram_tensor("wv", [D, 256], F32, kind="ExternalInput").ap()
    dr["wconv"] = nc.dram_tensor("wconv", [D, 768], F32, kind="ExternalInput").ap()
    dr["whg"] = nc.dram_tensor("whg", [D, 768], F32, kind="ExternalInput").ap()
    dr["wi"] = nc.dram_tensor("wi", [D, 256], F32, kind="ExternalInput").ap()
    rscr_t = nc.dram_tensor("rscr", [2, RL], F32, kind="Internal")
    dr["rscr"] = rscr_t.ap()
    dr["rscr_t"] = rscr_t
    dr["out"] = nc.dram_tensor("uT", [768, T], BF16, kind="ExternalOutput").ap()
    dr["o_att"] = [dr["out"][hd * 128:(hd + 1) * 128, :] for hd in range(2)]
    dr["o_conv"] = [dr["out"][256 + ci * 128: 256 + (ci + 1) * 128, :] for ci in range(2)]
    dr["o_hgrn"] = [dr["out"][512 + hd * 128: 512 + (hd + 1) * 128, :] for hd in range(2)]

    with contextlib.ExitStack() as st:
        C = Ctx(nc, st)
        K = emit_consts(C)
        emit_phase1(C, K, dr, layer, parts)
        C.S.finalize(final_dma_keys=["out"])
        print("phase1 ops", len(C.S.ops), "sig", C.S.max_counts)
    return nc


def emit_phase1(C, K, dr, layer, parts=("conv", "hgrn", "att")):
    S = C.S
    T = T_ALL
    lam_init = 0.8 - 0.6 * math.exp(-0.3 * layer)
    m_base = C.mark()
    groups = make_groups(T)
    NG = len(groups)
    reuse = bool(dr.get("reuse_hn"))
    prm = C.alloc(280, F32)
    B_prm = S.buf("prm")
    S.add("sync", lambda e: e.dma_start(out=prm, in_=dr["params"]), writes=[B_prm], dma_key="prm")
    cst = C.alloc(896, F32)
    B_cst = S.buf("cst")
    if not reuse:
        S.add("sync", lambda e: e.dma_start(out=cst, in_=dr["consts"]), writes=[B_cst], dma_key="cst")
    ident_f = cst[:, 0:128]
    J_f = cst[:, 128:256]
    hmask = cst[:, 256:384]
    scanmask = cst[:, 384:896]
    ident_b = C.alloc(128, BF16)
    B_idb = S.buf("identb")
    ones1 = C.alloc(128, BF16)
    B_ones1 = S.buf("ones1")
    onesd = C.alloc(128, BF16)
    B_onesd = S.buf("onesd")
    blk64 = C.alloc(128, BF16)
    B_blk = S.buf("blk64")
    if not reuse:
        S.add("vector", lambda e: e.tensor_copy(out=ident_b, in_=ident_f), reads=[B_cst], writes=[B_idb])
        S.add("vector", lambda e: e.memset(ones1, 1.0), writes=[B_ones1])
        S.add("vector", lambda e: e.memset(onesd, 1.0 / 128.0), writes=[B_onesd])
        S.add("vector", lambda e: e.memset(blk64, 0.0), writes=[B_blk])
        S.add("vector", lambda e: e.memset(blk64[0:64, 0:64], 1.0 / 64.0), writes=[B_blk])
        S.add("vector", lambda e: e.memset(blk64[64:128, 64:128], 1.0 / 64.0), writes=[B_blk])

    hn = C.alloc(8 * TP, BF16)
    B_hn = [[S.buf(f"hn{c}_{g}") for g in range(NG + 1)] for c in range(8)]
    if not reuse:
        for c in range(8):
            S.add("gpsimd", lambda e, c=c: e.memset(hn[:, c * TP + T: (c + 1) * TP], 0.0), writes=[B_hn[c][NG]])
    m_main = C.mark()
    hsb = [C.alloc(8 * 512, F32) for _ in range(2)]
    B_hsb = [[S.buf(f"hsb{i}_{c}") for c in range(8)] for i in range(2)]
    hsv = kview(dr["hsT"])

    def norm_group(gi, t0, w):
        i = gi % 2
        S.add("sync", lambda e: e.dma_start(out=hsb[i].rearrange("p (k n) -> p k n", k=8)[:, :, :w], in_=hsv[:, :, t0:t0 + w]),
              writes=B_hsb[i], dma_key=f"hsb{i}")
        emit_rmsnorm_group(C, K, hsb[i], 512, [[B_hsb[i][c]] for c in range(8)], prm, B_prm, hn, TP,
                           [B_hn[c][gi] for c in range(8)], 0, 0, w, t0)

    def hn_bufs(kc, t0, w):
        g0 = t0 // 512
        g1 = min((t0 + w - 1) // 512, NG)
        return [B_hn[kc][g] for g in range(g0, g1 + 1)]

    do_conv = "conv" in parts
    if do_conv:
        wc = C.alloc(8 * 768, BF16)
        B_wc = S.buf("wc")
        S.add("gpsimd", lambda e: e.dma_start(out=wc.rearrange("p (k n) -> p k n", k=8), in_=kview(dr["wconv"])), writes=[B_wc], dma_key="wc")
        zb = [C.alloc(516, F32) for _ in range(2)]
        B_z = [S.buf("z0"), S.buf("z1")]
        for ci in range(2):
            S.add("vector", lambda e, ci=ci: e.memset(zb[ci][:, 0:2], 0.0), writes=[B_z[ci]])

    def conv_group(gi, t0, w):
        for ci in range(2):
            conv_chunk(gi, t0, w, ci)

    def conv_chunk(gi, t0, w, ci):
        if True:
            pss = []
            for br in range(3):
                ps, Bps = C.ps()
                for kc in range(8):
                    S.add("tensor", lambda e, kc=kc, br=br, ps=ps: e.matmul(
                        ps[:, :w], wc[:, kc * 768 + br * 256 + ci * 128: kc * 768 + br * 256 + (ci + 1) * 128],
                        hn[:, kc * TP + t0: kc * TP + t0 + w], start=(kc == 0), stop=(kc == 7)),
                        reads=[B_wc] + hn_bufs(kc, t0, w), writes=[Bps])
                pss.append((ps, Bps))
            (pb, Bpb), (pc, Bpc), (ph, Bph) = pss
            z = zb[ci]
            th, Bth = C.tmp("ch", 512, F32, 2)
            S.add("scalar", lambda e: e.activation(out=th[:, :w], in_=ph[:, :w], func=AF.Copy), reads=[Bph], writes=[Bth])
            S.add("vector", lambda e: e.tensor_tensor(out=z[:, 2:2 + w], in0=pc[:, :w], in1=th[:, :w], op=ALU.mult),
                  reads=[Bpc, Bth], writes=[B_z[ci]])
            y, By = C.tmp("y", 512, F32, 2)
            wcol = 14 + ci * 3
            S.add("vector", lambda e: e.tensor_scalar(y[:, :w], z[:, 2:2 + w], prm[:, wcol + 2:wcol + 3], None, ALU.mult),
                  reads=[B_z[ci], B_prm], writes=[By])
            S.add("vector", lambda e: e.scalar_tensor_tensor(out=y[:, :w], in0=z[:, 1:1 + w], scalar=prm[:, wcol + 1:wcol + 2],
                                                             in1=y[:, :w], op0=ALU.mult, op1=ALU.add),
                  reads=[B_z[ci], B_prm, By], writes=[By])
            S.add("vector", lambda e: e.scalar_tensor_tensor(out=y[:, :w], in0=z[:, 0:w], scalar=prm[:, wcol:wcol + 1],
                                                             in1=y[:, :w], op0=ALU.mult, op1=ALU.add),
                  reads=[B_z[ci], B_prm, By], writes=[By])
            uo, Buo = C.tmp("uo", 512, BF16, 3)
            S.add("vector", lambda e: e.tensor_tensor(out=uo[:, :w], in0=pb[:, :w], in1=y[:, :w], op=ALU.mult),
                  reads=[Bpb, By], writes=[Buo])
            S.add("sync", lambda e: e.dma_start(out=dr["o_conv"][ci][:, t0:t0 + w], in_=uo[:, :w]),
                  reads=[Buo], dma_key="out")
            if w >= 2:
                S.add("vector", lambda e: e.tensor_copy(out=z[:, 0:2], in_=z[:, w:w + 2]), reads=[B_z[ci]], writes=[B_z[ci]])


    if not reuse:
        for gi, (t0, w) in enumerate(groups):
            norm_group(gi, t0, w)
    if do_conv:
        for gi, (t0, w) in enumerate(groups):
            conv_group(gi, t0, w)
    S.fence()
    C.reset(m_main)
    C.drop_tmps()

    if "hgrn" in parts:
        m0 = C.mark()
        whg = C.alloc(8 * 768, BF16)
        B_whg = S.buf("whg")
        S.add("gpsimd", lambda e: e.dma_start(out=whg.rearrange("p (k n) -> p k n", k=8), in_=kview(dr["whg"])), writes=[B_whg], dma_key="whg")
        wi = C.alloc(8 * 256, BF16)
        B_wi = S.buf("wi")
        S.add("gpsimd", lambda e: e.dma_start(out=wi.rearrange("p (k n) -> p k n", k=8), in_=kview(dr["wi"])), writes=[B_wi], dma_key="wi")
        hmask_b = hmask
        lb = C.alloc(2, F32)
        oml = C.alloc(2, F32)
        B_lb = S.buf("lb")
        if layer == 0:
            S.add("vector", lambda e: e.memset(lb, 0.0), writes=[B_lb])
        else:
            dl = C.alloc(2, F32)
            B_dl = S.buf("dl")
            for hd in range(2):
                S.add("vector", lambda e, hd=hd: e.tensor_tensor(out=dl[:, hd:hd + 1], in0=prm[:, 21 + 2 * hd:22 + 2 * hd],
                                                               in1=prm[:, 20 + 2 * hd:21 + 2 * hd], op=ALU.subtract),
                      reads=[B_prm], writes=[B_dl])
            S.add("scalar", lambda e: e.activation(out=lb, in_=dl, func=AF.Sigmoid), reads=[B_dl], writes=[B_lb])
        S.add("vector", lambda e: e.tensor_scalar(oml, lb, -1.0, 1.0, ALU.mult, ALU.add), reads=[B_lb], writes=[B_lb])
        hgroups = make_groups(TP)
        rb = C.reserve(2)
        po_b = [rb[0], rb[1]]
        kv_s = [[C.alloc(128, F32) for _ in range(8)] for _ in range(2)]
        B_kv = [[S.buf("kv") for _ in range(8)] for _ in range(2)]
        Sf = [[C.alloc(128, F32) for _ in range(2)] for _ in range(2)]
        B_Sf = [[S.buf("Sf") for _ in range(2)] for _ in range(2)]
        for hd in range(2):
            S.add("vector", lambda e, hd=hd: e.memset(Sf[hd][0], 0.0), writes=[B_Sf[hd][0]])
        kTz = [[[C.alloc(128, BF16) for _ in range(2)] for _ in range(4)] for _ in range(2)]
        B_kTz = [[[S.buf("kTz") for _ in range(2)] for _ in range(4)] for _ in range(2)]
        for hd in range(2):
            for tl in range(4):
                for cc in range(2):
                    S.add("gpsimd", lambda e, hd=hd, tl=tl, cc=cc: e.memset(kTz[hd][tl][cc], 0.0), writes=[B_kTz[hd][tl][cc]])
        vts = [[C.alloc(128, BF16) for _ in range(4)] for _ in range(2)]
        B_vts = [[S.buf("vt") for _ in range(4)] for _ in range(2)]
        Ats = [[C.alloc(128, BF16) for _ in range(4)] for _ in range(2)]
        B_Ats = [[S.buf("At") for _ in range(4)] for _ in range(2)]
        step = [0, 0]

        def phase_a2(gi, t0, w):
            nch = w // 64
            H = (0, 1)
            Xs = [{"nch": nch, "ntl": w // 128, "t0": t0, "w": w, "gi": gi} for _ in H]
            CARRY = ("qi", "q2", "k2", "kht", "gs")

            def T_(kind, hd, dt=F32):
                return C.tmp(f"{kind}{hd}", 512, dt, 2 if kind in CARRY else 1)

            def proj(br, kind, func):
                outs = []
                pss = []
                for hd in H:
                    ps, Bps = C.ps()
                    for kc in range(8):
                        S.add("tensor", lambda e, kc=kc, hd=hd, ps=ps: e.matmul(
                            ps[:, :w], whg[:, kc * 768 + br * 256 + hd * 128: kc * 768 + br * 256 + (hd + 1) * 128],
                            hn[:, kc * TP + t0: kc * TP + t0 + w], start=(kc == 0), stop=(kc == 7)),
                            reads=[B_whg] + hn_bufs(kc, t0, w), writes=[Bps])
                    pss.append((ps, Bps))
                for hd in H:
                    ps, Bps = pss[hd]
                    o, Bo = T_(kind, hd)
                    S.add("scalar", lambda e, o=o, ps=ps: e.activation(out=o[:, :w], in_=ps[:, :w], func=func), reads=[Bps], writes=[Bo])
                    outs.append((o, Bo))
                return outs

            def act(kind, src, func, scale=1.0, dt=F32, extra=()):
                outs = []
                for hd in H:
                    o, Bo = T_(kind, hd, dt)
                    a, Ba = src[hd]
                    S.add("scalar", lambda e, o=o, a=a: e.activation(out=o[:, :w], in_=a[:, :w], func=func, scale=scale),
                          reads=[Ba] + [x[hd][1] for x in extra], writes=[Bo])
                    outs.append((o, Bo))
                return outs

            def mul(kind, a_, b_, dt=F32):
                outs = []
                for hd in H:
                    o, Bo = T_(kind, hd, dt)
                    (a, Ba), (b, Bb) = a_[hd], b_[hd]
                    S.add("vector", lambda e, o=o, a=a, b=b: e.tensor_tensor(out=o[:, :w], in0=a[:, :w], in1=b[:, :w], op=ALU.mult),
                          reads=[Ba, Bb], writes=[Bo])
                    outs.append((o, Bo))
                return outs

            qs = proj(0, "qs", AF.Silu)
            gs = proj(2, "gs", AF.Silu)
            f = proj(1, "f", AF.Sigmoid)
            for hd in H:
                ff, Bf = f[hd]
                S.add("vector", lambda e, ff=ff, hd=hd: e.tensor_scalar(ff[:, :w], ff[:, :w], oml[:, hd:hd + 1], lb[:, hd:hd + 1], ALU.mult, ALU.add),
                      reads=[Bf, B_lb], writes=[Bf])
            lf = act("lf", f, AF.Ln)
            kh = []
            for hd in H:
                o, Bo = T_("kh", hd)
                ff, Bf = f[hd]
                S.add("vector", lambda e, o=o, ff=ff: e.tensor_scalar(o[:, :w], ff[:, :w], -1.0, 1.0, ALU.mult, ALU.add), reads=[Bf], writes=[Bo])
                kh.append((o, Bo))
            G = []
            for hd in H:
                o, Bo = T_("G", hd)
                l_, Bl = lf[hd]
                S.add("vector", lambda e, o=o, l_=l_: e.tensor_tensor_scan(out=o[:, :w], data0=scanmask[:, :w], data1=l_[:, :w], initial=0.0,
                                                                         op0=ALU.mult, op1=ALU.add), reads=[Bl, B_cst], writes=[Bo])
                G.append((o, Bo))
            G3 = [G[hd][0][:, :w].rearrange("p (c t) -> p c t", t=64) for hd in H]
            E = act("E", G, AF.Exp)
            qi = mul("qi", qs, E)
            scl = []
            for hd in H:
                o, Bo = C.tmp(f"scl{hd}", 8, F32, 2)
                e_, Be = E[hd]
                S.add("vector", lambda e, o=o, e_=e_: e.tensor_copy(out=o[:, :nch].rearrange("p (c o) -> p c o", o=1),
                                                                     in_=e_[:, :w].rearrange("p (c t) -> p c t", t=64)[:, :, 63:64]), reads=[Be], writes=[Bo])
                scl.append((o, Bo))
            Dm = []
            for hd in H:
                o, Bo = T_("Dm", hd)
                S.add("vector", lambda e, o=o, hd=hd: e.tensor_tensor(out=o[:, :w].rearrange("p (c t) -> p c t", t=64), in0=G3[hd],
                                                                      in1=G3[hd][:, :, 31:32].broadcast_to([128, nch, 64]), op=ALU.subtract),
                      reads=[G[hd][1]], writes=[Bo])
                Dm.append((o, Bo))
            E2 = act("E2", Dm, AF.Exp)
            q2 = mul("q2", qs, E2, BF16)
            E3 = act("E3", Dm, AF.Exp, scale=-1.0)
            k2 = mul("k2", kh, E3, BF16)
            Dl = []
            for hd in H:
                o, Bo = T_("Dl", hd)
                S.add("vector", lambda e, o=o, hd=hd: e.tensor_tensor(out=o[:, :w].rearrange("p (c t) -> p c t", t=64), in0=G3[hd],
                                                                      in1=G3[hd][:, :, 63:64].broadcast_to([128, nch, 64]), op=ALU.subtract),
                      reads=[G[hd][1]], writes=[Bo])
                Dl.append((o, Bo))
            E4 = act("E4", Dl, AF.Exp, scale=-1.0)
            kht = mul("kht", kh, E4, BF16)
            for hd in H:
                Xs[hd].update(dict(qi=qi[hd][0], Bqi=qi[hd][1], q2=q2[hd][0], Bq2=q2[hd][1], k2=k2[hd][0], Bk2=k2[hd][1],
                                   kht=kht[hd][0], Bkht=kht[hd][1], scl=scl[hd][0], Bscl=scl[hd][1], gs=gs[hd][0], Bgs=gs[hd][1]))
            return Xs

        def phase_b(hd, X):
            t0 = X["t0"]
            for tl in range(X["ntl"]):
                def tile(tl):
                    c0 = tl * 128
                    pv, Bpv = C.ps()
                    for kc in range(8):
                        S.add("tensor", lambda e, kc=kc: e.matmul(
                            pv[:, 0:128], hn[:, kc * TP + t0 + c0: kc * TP + t0 + c0 + 128], wi[:, kc * 256 + hd * 128: kc * 256 + (hd + 1) * 128],
                            start=(kc == 0), stop=(kc == 7)), reads=[B_wi] + hn_bufs(kc, t0 + c0, 128), writes=[Bpv])
                    vt, Bvt = vts[hd][tl], B_vts[hd][tl]
                    S.add("scalar", lambda e: e.activation(out=vt, in_=pv[:, 0:128], func=AF.Copy), reads=[Bpv], writes=[Bvt])
                    pk, Bpk = C.ps()
                    S.add("tensor", lambda e: e.matmul(pk[:, 0:128], X["kht"][:, c0:c0 + 128], ident_b, start=True, stop=True),
                          reads=[X["Bkht"], B_idb], writes=[Bpk])
                    for cc in range(2):
                        S.add("vector", lambda e, cc=cc: e.tensor_copy(out=kTz[hd][tl][cc][cc * 64:(cc + 1) * 64, :], in_=pk[cc * 64:(cc + 1) * 64, 0:128]),
                              reads=[Bpk], writes=[B_kTz[hd][tl][cc]])
                    pa, Bpa = C.ps()
                    S.add("tensor", lambda e: e.matmul(pa[:, 0:128], X["k2"][:, c0:c0 + 128], X["q2"][:, c0:c0 + 128], start=True, stop=True),
                          reads=[X["Bk2"], X["Bq2"]], writes=[Bpa])
                    S.add("vector", lambda e: e.tensor_tensor(out=Ats[hd][tl], in0=pa[:, 0:128], in1=hmask_b, op=ALU.mult),
                          reads=[Bpa, B_cst], writes=[B_Ats[hd][tl]])
                    for cc in range(2):
                        def kvprod(cc):
                            ch = tl * 2 + cc
                            pkv, Bpkv = C.ps()
                            S.add("tensor", lambda e: e.matmul(pkv[:, 0:128], kTz[hd][tl][cc], vt, start=True, stop=True),
                                  reads=[B_kTz[hd][tl][cc], Bvt], writes=[Bpkv])
                            S.add("scalar", lambda e: e.activation(out=kv_s[hd][ch], in_=pkv[:, 0:128], func=AF.Copy), reads=[Bpkv], writes=[B_kv[hd][ch]])
                        kvprod(cc)
                tile(tl)

        def chain_step(hd, X, ch):
            po, Bpo = po_b[hd]
            cs = ch * 64
            i = step[hd] % 2
            Sc, BSc = Sf[hd][i], B_Sf[hd][i]
            Sn, BSn = Sf[hd][1 - i], B_Sf[hd][1 - i]
            step[hd] += 1
            if HG_BF16_STATE:
                Sbb, BSbb = C.tmp(f"Sbb{hd}", 128, BF16, 2)
                qib, Bqib = C.tmp(f"qib{hd}", 64, BF16, 2)
                S.add("scalar", lambda e: e.activation(out=Sbb, in_=Sc, func=AF.Copy), reads=[BSc], writes=[BSbb])
                S.add("gpsimd", lambda e: e.tensor_copy(out=qib, in_=X["qi"][:, cs:cs + 64]), reads=[X["Bqi"]], writes=[Bqib])
                S.add("tensor", lambda e: e.matmul(po[:, cs:cs + 64], Sbb, qib, start=(cs == 0), stop=False, skip_group_check=True),
                      reads=[BSbb, Bqib], writes=[Bpo])
            else:
                S.add("tensor", lambda e: e.matmul(po[:, cs:cs + 64], Sc, X["qi"][:, cs:cs + 64], start=(cs == 0), stop=False, skip_group_check=True),
                      reads=[BSc, X["Bqi"]], writes=[Bpo])
            S.add("vector", lambda e: e.scalar_tensor_tensor(out=Sn, in0=Sc, scalar=X["scl"][:, ch:ch + 1], in1=kv_s[hd][ch],
                                                             op0=ALU.mult, op1=ALU.add), reads=[BSc, X["Bscl"], B_kv[hd][ch]], writes=[BSn])
            if ch % 2 == 1:
                tl = ch // 2
                c0 = tl * 128
                S.add("tensor", lambda e: e.matmul(po[:, c0:c0 + 128], vts[hd][tl], Ats[hd][tl], start=False, stop=True, skip_group_check=True),
                      reads=[B_vts[hd][tl], B_Ats[hd][tl]], writes=[Bpo])

        def phase_d(hd, X):
            t0, w = X["t0"], X["w"]
            po, Bpo = po_b[hd]
            wv_ = min(w, T - t0)
            if wv_ <= 0:
                return
            osb, Bosb = C.tmp(f"osb{hd}", 512, F32, 1)
            S.add("scalar", lambda e: e.activation(out=osb[:, :w], in_=po[:, :w], func=AF.Copy), reads=[Bpo], writes=[Bosb])
            sq, Bsq = C.tmp("sq", 512, BF16, 2)
            S.add("scalar", lambda e: e.activation(out=sq[:, :w], in_=osb[:, :w], func=AF.Square), reads=[Bosb], writes=[Bsq])
            pm, Bpm = C.ps()
            S.add("tensor", lambda e: e.matmul(pm[:, :w], onesd, sq[:, :w], start=True, stop=True), reads=[Bsq, B_onesd], writes=[Bpm])
            rs, Brs = C.tmp("rs", 512, F32, 2)
            S.add("scalar", lambda e: e.activation(out=rs[:, :w], in_=pm[:, :w], func=AF.Ln, bias=K["eps"], scale=1.0),
                  reads=[Bpm, K["B_eps"]], writes=[Brs])
            S.add("scalar", lambda e: e.activation(out=rs[:, :w], in_=rs[:, :w], func=AF.Exp, scale=-0.5), reads=[Brs], writes=[Brs])
            S.add("vector", lambda e: e.scalar_tensor_tensor(out=osb[:, :w], in0=osb[:, :w], scalar=prm[:, 11:12], in1=rs[:, :w],
                                                             op0=ALU.mult, op1=ALU.mult), reads=[Bosb, Brs, B_prm], writes=[Bosb])
            uo, Buo = C.tmp("uoh", 512, BF16, 2)
            S.add("vector", lambda e: e.tensor_tensor(out=uo[:, :w], in0=osb[:, :w], in1=X["gs"][:, :w], op=ALU.mult),
                  reads=[Bosb, X["Bgs"]], writes=[Buo])
            S.add("sync", lambda e: e.dma_start(out=dr["o_hgrn"][hd][:, t0:t0 + wv_], in_=uo[:, :wv_]), reads=[Buo], dma_key="out")

        Xn = phase_a2(0, hgroups[0][0], hgroups[0][1])
        for gi, (t0, w) in enumerate(hgroups):
            Xs = Xn
            if gi + 1 < len(hgroups):
                Xn = phase_a2(gi + 1, hgroups[gi + 1][0], hgroups[gi + 1][1])
            for hd in range(2):
                phase_b(hd, Xs[hd])
            for ch in range(Xs[0]["nch"]):
                for hd in range(2):
                    chain_step(hd, Xs[hd], ch)
            for hd in range(2):
                phase_d(hd, Xs[hd])
        C.release()
        S.fence()
        C.reset(m0)
        C.drop_tmps()

    if "att" in parts:
        emit_attention(C, K, dr, hn, hn_bufs, prm, B_prm, cst, B_cst, dr["rscr_t"], lam_init, groups,
                       dict(ones1=ones1, B_ones1=B_ones1, onesd=onesd, B_onesd=B_onesd, blk64=blk64, B_blk=B_blk))
    S.fence(new_epoch=True)
    C.reset(m_base)
    C.drop_tmps()


def emit_attention(C, K, dr, hn, hn_bufs, prm, B_prm, cst, B_cst, rscr_t, lam_init, groups, X):
    S = C.S
    T = T_ALL
    ones1, B_ones1, onesd, B_onesd, blk64, B_blk = X["ones1"], X["B_ones1"], X["onesd"], X["B_onesd"], X["blk64"], X["B_blk"]
    J_f = cst[:, 128:256]
    wqk = C.alloc(8 * 512, BF16)
    B_wqk = S.buf("wqk")
    S.add("gpsimd", lambda e: e.dma_start(out=wqk.rearrange("p (k n) -> p k n", k=8), in_=kview(dr["wqk"])), writes=[B_wqk], dma_key="wqk")
    wv = C.alloc(8 * 256, BF16)
    B_wv = S.buf("wv")
    S.add("gpsimd", lambda e: e.dma_start(out=wv.rearrange("p (k n) -> p k n", k=8), in_=kview(dr["wv"])), writes=[B_wv], dma_key="wv")

    lt = C.alloc(128, F32)
    ssum = C.alloc(2, F32)
    nlam = C.alloc(1, F32)
    lnc = C.alloc(1, F32)
    B_l = S.buf("lam")
    B_nlam = S.buf("nlam")
    S.add("vector", lambda e: e.tensor_tensor(out=lt[:, 0:64], in0=prm[:, 24:88], in1=prm[:, 88:152], op=ALU.mult), reads=[B_prm], writes=[B_l])
    S.add("vector", lambda e: e.tensor_tensor(out=lt[:, 64:128], in0=prm[:, 152:216], in1=prm[:, 216:280], op=ALU.mult), reads=[B_prm], writes=[B_l])
    S.add("vector", lambda e: e.tensor_reduce(out=ssum[:, 0:1], in_=lt[:, 0:64], axis=AX.X, op=ALU.add), reads=[B_l], writes=[B_l])
    S.add("vector", lambda e: e.tensor_reduce(out=ssum[:, 1:2], in_=lt[:, 64:128], axis=AX.X, op=ALU.add), reads=[B_l], writes=[B_l])
    S.add("scalar", lambda e: e.activation(out=ssum, in_=ssum, func=AF.Exp), reads=[B_l], writes=[B_l])
    S.add("vector", lambda e: e.tensor_tensor(out=nlam, in0=ssum[:, 1:2], in1=ssum[:, 0:1], op=ALU.subtract), reads=[B_l], writes=[B_nlam])
    S.add("vector", lambda e: e.tensor_scalar(nlam, nlam, -lam_init, None, ALU.add), reads=[B_nlam], writes=[B_nlam])
    S.add("vector", lambda e: e.memset(lnc, math.log(1.0 - lam_init)), writes=[B_nlam])

    relb = C.alloc(2, F32)
    B_relb = S.buf("relb")
    S.add("vector", lambda e: e.memset(relb[32:33, :], 1.0), writes=[B_relb])
    S.add("sync", lambda e: e.dma_start(out=relb[0:32, :], in_=dr["relb"]), writes=[B_relb], dma_key="relb")
    oh = C.alloc(RL, F32)
    B_oh = S.buf("oh")
    S.add("sync", lambda e: e.dma_start(out=oh[0:33, :], in_=dr["oh"]), writes=[B_oh], dma_key="oh")
    rsb = C.alloc(RL, F32)
    B_rsb = S.buf("rsb")
    for cc in range(3):
        def rchunk(cc):
            ps, Bps = C.ps()
            S.add("tensor", lambda e: e.matmul(ps[0:2, 0:384], relb[0:33, :], oh[0:33, cc * 384:(cc + 1) * 384], start=True, stop=True),
                  reads=[B_relb, B_oh], writes=[Bps])
            S.add("vector", lambda e: e.tensor_copy(out=rsb[0:2, cc * 384:(cc + 1) * 384], in_=ps[0:2, 0:384]), reads=[Bps], writes=[B_rsb])
        rchunk(cc)
    B_rscr = S.buf("rscr")
    S.add("sync", lambda e: e.dma_start(out=dr["rscr"], in_=rsb[0:2, :]), reads=[B_rsb], writes=[B_rscr], dma_key="rscr")
    Bt = {}
    B_Bt = S.buf("Bt")
    for dl in (1, 0, -1, -2, -3):
        for hd in range(2):
            def mk(dl, hd):
                Hs, BHs = C.tmp("Hs", 512, F32, 1)
                src = bass.AP(rscr_t, hd * RL + 128 * dl + 384, [[1, 128], [1, 512]])
                S.add("sync", lambda e: e.dma_start(out=Hs, in_=src), reads=[B_rscr], writes=[BHs], dma_key="hank")
                ps, Bps = C.ps()
                S.add("tensor", lambda e: e.matmul(ps[:, :], J_f, Hs, start=True, stop=True), reads=[BHs, B_cst], writes=[Bps])
                bt = C.alloc(512, F32)
                S.add("scalar", lambda e: e.activation(out=bt, in_=ps[:, :], func=AF.Copy), reads=[Bps], writes=[B_Bt])
                Bt[(dl, hd)] = bt
            mk(dl, hd)

    qn = C.alloc(4 * TP, BF16)
    B_qz = S.buf("qz")
    S.add("gpsimd", lambda e: e.memset(qn, 0.0), writes=[B_qz])
    kn = C.alloc(2 * TP, BF16)
    v_sb = C.alloc(NT * 256, BF16)
    hgroups = make_groups(TP)
    B_qn = [[S.buf("qn") for _ in hgroups] for _ in range(2)]
    B_kn = [[S.buf("kn") for _ in hgroups] for _ in range(2)]
    B_v = [S.buf("v") for _ in range(NT)]

    def qk_group(gi, t0, w, x, hd):
        ps, Bps = C.ps()
        col = x * 256 + hd * 128
        for kc in range(8):
            S.add("tensor", lambda e, kc=kc: e.matmul(ps[:, :w], wqk[:, kc * 512 + col: kc * 512 + col + 128],
                                                       hn[:, kc * TP + t0: kc * TP + t0 + w], start=(kc == 0), stop=(kc == 7)),
                  reads=[B_wqk] + hn_bufs(kc, t0, w), writes=[Bps])
        sq, Bsq = C.tmp("sq", 512, BF16, 2)
        S.add("scalar", lambda e: e.activation(out=sq[:, :w], in_=ps[:, :w], func=AF.Square), reads=[Bps], writes=[Bsq])
        pm, Bpm = C.ps()
        S.add("tensor", lambda e: e.matmul(pm[:, :w], blk64, sq[:, :w], start=True, stop=True), reads=[Bsq, B_blk], writes=[Bpm])
        rs, Brs = C.tmp("rs", 512, F32, 2)
        S.add("scalar", lambda e: e.activation(out=rs[:, :w], in_=pm[:, :w], func=AF.Ln, bias=K["eps"], scale=1.0),
              reads=[Bpm, K["B_eps"]], writes=[Brs])
        S.add("scalar", lambda e: e.activation(out=rs[:, :w], in_=rs[:, :w], func=AF.Exp, scale=-0.5), reads=[Brs], writes=[Brs])
        if x == 1:
            S.add("vector", lambda e: e.scalar_tensor_tensor(out=kn[:, hd * TP + t0: hd * TP + t0 + w], in0=ps[:, :w], scalar=prm[:, 9:10],
                                                             in1=rs[:, :w], op0=ALU.mult, op1=ALU.mult), reads=[Bps, Brs, B_prm], writes=[B_kn[hd][gi]])
        else:
            for m in range(2):
                def qwrite(m):
                    r0, r1 = m * 64, (m + 1) * 64
                    o0 = (hd * 2 + m) * TP + t0
                    S.add("vector", lambda e: e.scalar_tensor_tensor(out=qn[r0:r1, o0:o0 + w], in0=ps[r0:r1, :w], scalar=prm[r0:r1, 8:9],
                                                                     in1=rs[r0:r1, :w], op0=ALU.mult, op1=ALU.mult),
                          reads=[Bps, Brs, B_prm, B_qz], writes=[B_qn[hd][gi]])
                qwrite(m)

    for gi, (t0, w) in enumerate(hgroups):
        for x in range(2):
            for hd in range(2):
                qk_group(gi, t0, w, x, hd)

    def v_tile(tt):
        ps, Bps = C.ps()
        for kc in range(8):
            S.add("tensor", lambda e, kc=kc: e.matmul(ps[:, 0:256], hn[:, kc * TP + tt * 128: kc * TP + (tt + 1) * 128], wv[:, kc * 256:(kc + 1) * 256],
                                                       start=(kc == 0), stop=(kc == 7)), reads=[B_wv] + hn_bufs(kc, tt * 128, 128), writes=[Bps])
        S.add("vector", lambda e: e.tensor_copy(out=v_sb[:, tt * 256:(tt + 1) * 256], in_=ps[:, 0:256]), reads=[Bps], writes=[B_v[tt]])

    for tt in range(NT):
        v_tile(tt)

    acc = C.reserve(4)

    def att_group(hd, g, t0, w):
        nk = min(4 * g + 4, NT)
        (O1, BO1), (O2, BO2), (s1, Bs1), (s2, Bs2) = acc
        Os = ((O1, BO1), (O2, BO2))
        ss = ((s1, Bs1), (s2, Bs2))

        def s_unit(j, m):
            dl = 4 * g - j
            ps, Bps = C.ps()
            S.add("tensor", lambda e: e.matmul(ps[:, :w], kn[:, hd * TP + j * 128: hd * TP + (j + 1) * 128],
                                               qn[:, (hd * 2 + m) * TP + t0: (hd * 2 + m) * TP + t0 + w], start=True, stop=True),
                  reads=[B_kn[hd][j // 4], B_qn[hd][g]], writes=[Bps])
            P, BP = C.tmp("P", 512, BF16, 5)
            if dl >= 2:
                S.add("scalar", lambda e: e.activation(out=P[:, :w], in_=ps[:, :w], func=AF.Exp, bias=prm[:, 12 + hd:13 + hd], scale=0.125),
                      reads=[Bps, B_prm], writes=[BP])
            else:
                nb, Bnb = C.tmp("nb", 512, F32, 2)
                bt = Bt[(dl, hd)]
                S.add("vector", lambda e: e.scalar_tensor_tensor(out=nb[:, :w], in0=ps[:, :w], scalar=0.125, in1=bt[:, :w],
                                                                 op0=ALU.mult, op1=ALU.add), reads=[Bps, B_Bt], writes=[Bnb])
                S.add("scalar", lambda e: e.activation(out=P[:, :w], in_=nb[:, :w], func=AF.Exp), reads=[Bnb], writes=[BP])
            return P, BP

        def pv_unit(j, m, P, BP):
            Om, BOm = Os[m]
            sm, Bsm = ss[m]
            S.add("tensor", lambda e: e.matmul(Om[:, :w], v_sb[:, j * 256 + hd * 128: j * 256 + (hd + 1) * 128], P[:, :w],
                                               start=(j == 0), stop=(j == nk - 1)), reads=[B_v[j], BP], writes=[BOm])
            S.add("tensor", lambda e: e.matmul(sm[:, :w], ones1, P[:, :w], start=(j == 0), stop=(j == nk - 1)),
                  reads=[B_ones1, BP], writes=[Bsm])

        LA = 3
        pend = []
        for j in range(nk):
            for m in range(2):
                P, BP = s_unit(j, m)
                pend.append((j, m, P, BP))
                if len(pend) > LA:
                    pv_unit(*pend.pop(0))
        for u in pend:
            pv_unit(*u)
        r1, Br1 = C.tmp("r1", 512, F32, 1)
        r2, Br2 = C.tmp("r2", 512, F32, 1)
        S.add("vector", lambda e: e.reciprocal(out=r1[:, :w], in_=s1[:, :w]), reads=[Bs1], writes=[Br1])
        S.add("vector", lambda e: e.reciprocal(out=r2[:, :w], in_=s2[:, :w]), reads=[Bs2], writes=[Br2])
        a1, Ba1 = C.tmp("a1", 512, F32, 1)
        a2, Ba2 = C.tmp("a2", 512, F32, 1)
        S.add("vector", lambda e: e.tensor_tensor(out=a1[:, :w], in0=O1[:, :w], in1=r1[:, :w], op=ALU.mult), reads=[BO1, Br1], writes=[Ba1])
        S.add("vector", lambda e: e.tensor_tensor(out=a2[:, :w], in0=O2[:, :w], in1=r2[:, :w], op=ALU.mult), reads=[BO2, Br2], writes=[Ba2])
        S.add("vector", lambda e: e.scalar_tensor_tensor(out=a1[:, :w], in0=a2[:, :w], scalar=nlam[:, 0:1], in1=a1[:, :w], op0=ALU.mult, op1=ALU.add),
              reads=[Ba1, Ba2, B_nlam], writes=[Ba1])
        sq, Bsq = C.tmp("sq", 512, BF16, 2)
        S.add("scalar", lambda e: e.activation(out=sq[:, :w], in_=a1[:, :w], func=AF.Square), reads=[Ba1], writes=[Bsq])
        pm, Bpm = C.ps()
        S.add("tensor", lambda e: e.matmul(pm[:, :w], onesd, sq[:, :w], start=True, stop=True), reads=[Bsq, B_onesd], writes=[Bpm])
        rs, Brs = C.tmp("rs", 512, F32, 2)
        S.add("scalar", lambda e: e.activation(out=rs[:, :w], in_=pm[:, :w], func=AF.Ln, bias=K["eps"], scale=1.0),
              reads=[Bpm, K["B_eps"]], writes=[Brs])
        S.add("scalar", lambda e: e.activation(out=rs[:, :w], in_=rs[:, :w], func=AF.Exp, bias=lnc, scale=-0.5), reads=[Brs, B_nlam], writes=[Brs])
        uo, Buo = C.tmp("uoa", 512, BF16, 2)
        S.add("vector", lambda e: e.scalar_tensor_tensor(out=uo[:, :w], in0=a1[:, :w], scalar=prm[:, 10:11], in1=rs[:, :w], op0=ALU.mult, op1=ALU.mult),
              reads=[Ba1, Brs, B_prm], writes=[Buo])
        S.add("sync", lambda e: e.dma_start(out=dr["o_att"][hd][:, t0:t0 + w], in_=uo[:, :w]), reads=[Buo], dma_key="out")

    for hd in range(2):
        for g, (t0, w) in enumerate(groups):
            att_group(hd, g, t0, w)
    C.release()


def _rel_bucket_np(n):
    n = np.maximum(n, 0)
    nf = np.maximum(n, 1).astype(np.float32)
    large = 16 + (np.log(nf / np.float32(16)) / np.float32(math.log(128 / 16)) * np.float32(16)).astype(np.int32)
    large = np.minimum(large, 31)
    return np.where(n < 16, n, large)


_CONST_CACHE = {}


def phase1_consts():
    if "c" in _CONST_CACHE:
        return _CONST_CACHE["c"]
    cst = np.zeros((128, 896), np.float32)
    cst[:, 0:128] = np.eye(128, dtype=np.float32)
    cst[:, 128:256] = np.eye(128, dtype=np.float32)[::-1]
    s_i = np.arange(128)[:, None]
    t_i = np.arange(128)[None, :]
    cst[:, 256:384] = ((s_i // 64 == t_i // 64) & (s_i <= t_i)).astype(np.float32)
    cst[:, 384:896] = (np.arange(512) % 64 != 0).astype(np.float32)[None, :]
    oh = np.zeros((33, RL), np.float32)
    n = np.arange(RL) - 511
    bk = _rel_bucket_np(n)
    for i in range(RL):
        if n[i] >= 0:
            oh[bk[i], i] = 1.0
        else:
            oh[32, i] = -30000.0
    _CONST_CACHE["c"] = (cst, oh)
    return cst, oh


def phase1_inputs(d, L, h, hsT):
    w_in = d["w_in"][L]
    prm = np.zeros((128, 280), np.float32)
    prm[:, 0:8] = d["norm1_gain"][L].reshape(8, 128).T
    prm[:, 8] = np.tile(d["q_norm_gain"][L], 2)
    prm[:, 9] = np.tile(d["k_norm_gain"][L], 2)
    prm[:, 10] = d["attn_sub_gain"][L]
    prm[:, 11] = d["hgrn_out_gain"][L]
    for hd in range(2):
        prm[:, 12 + hd] = d["rel_bias"][31, 2 * h + hd]
        for lp in range(2):
            prm[:, 20 + hd * 2 + lp] = d["hgrn_lb_logits"][lp, (2 * h + hd) * 128:(2 * h + hd + 1) * 128]
    for ci in range(2):
        for k in range(3):
            prm[:, 14 + ci * 3 + k] = d["conv_w"][L][k, 256 * h + ci * 128: 256 * h + (ci + 1) * 128]
    prm[:, 24:280] = d["diff_lambda"][L].reshape(1, 256)
    cst, oh = phase1_consts()
    qk_cols = []
    for base in (0, 512):
        for hd in range(2):
            for m in range(2):
                c0 = base + m * 256 + (2 * h + hd) * 64
                qk_cols.append(np.arange(c0, c0 + 64))
    qk_cols = np.concatenate(qk_cols)
    sl = lambda base: w_in[:, base + 256 * h: base + 256 * (h + 1)]
    return {
        "hsT": hsT, "params": prm, "consts": cst, "oh": oh,
        "relb": np.ascontiguousarray(d["rel_bias"][:, 2 * h:2 * h + 2]),
        "wqk": np.ascontiguousarray(w_in[:, qk_cols]),
        "wv": np.ascontiguousarray(sl(1024)),
        "wconv": np.ascontiguousarray(np.concatenate([sl(1536), sl(2048), sl(2560)], axis=1)),
        "whg": np.ascontiguousarray(np.concatenate([sl(3072), sl(3584), sl(4608)], axis=1)),
        "wi": np.ascontiguousarray(sl(4096)),
    }


_NC_CACHE = {}


def _get_nc(key, fn):
    if key not in _NC_CACHE:
        _NC_CACHE[key] = fn()
    return _NC_CACHE[key]


def _lay_gain(g):
    return np.ascontiguousarray(g.reshape(8, 128).T)


P1_W = (("wqk", 512), ("wv", 256), ("wconv", 768), ("whg", 768), ("wi", 256))


def build_fused():
    T = T_ALL
    nc = bass.Bass("TRN2", target_bir_lowering=False)
    inp = lambda name, shape, dt=F32: nc.dram_tensor(name, shape, dt, kind="ExternalInput").ap()
    g = {}
    g["hsT"] = inp("hsT", [D, T])
    g["consts"] = inp("consts", [128, 896])
    g["oh"] = inp("oh", [33, RL])
    g["selv"] = inp("selv", [128, 2])
    for h in range(2):
        g[f"relb_{h}"] = inp(f"relb_{h}", [32, 2])
    for L in range(2):
        for h in range(2):
            g[f"params_{L}_{h}"] = inp(f"params_{L}_{h}", [128, 280])
            for nm, wd in P1_W:
                g[f"{nm}_{L}_{h}"] = inp(f"{nm}_{L}_{h}", [D, wd])
        g[f"wgate_{L}"] = inp(f"wgate_{L}", [D, 3072])
        g[f"wbr_{L}"] = inp(f"wbr_{L}", [1536, D])
        g[f"wout_{L}"] = inp(f"wout_{L}", [D, D])
        g[f"gains_{L}"] = inp(f"gains_{L}", [128, 16])
    g["fg0"] = inp("fg0", [1, D, 2816])
    g["fu0"] = inp("fu0", [1, D, 2816])
    g["fd0"] = inp("fd0", [1, 2816, D])
    g["fg1"] = inp("fg1", [8, D, 3584])
    g["fu1"] = inp("fu1", [8, D, 3584])
    g["fd1"] = inp("fd1", [8, 3584, D])
    g["wr"] = inp("wr", [D, 8])
    g["sel"] = inp("sel", [8, 1024])
    g["ident"] = inp("ident", [128, 128])
    out = nc.dram_tensor("out", [D, 2048], F32, kind="ExternalOutput").ap()
    rscr_t = nc.dram_tensor("rscr", [2, RL], F32, kind="Internal")
    ikind = "ExternalOutput" if FDEBUG else "Internal"
    u_scr = [nc.dram_tensor(f"u_scr{L}", [1536, T], BF16, kind=ikind).ap() for L in range(2)]
    hs1 = nc.dram_tensor("hs1", [D, T], F32, kind=ikind).ap()

    with contextlib.ExitStack() as st:
        C = Ctx(nc, st)
        K = emit_consts(C)
        for L in range(2):
            hs_src = g["hsT"] if L == 0 else hs1
            for h in range(2):
                dr = {"hsT": hs_src, "params": g[f"params_{L}_{h}"], "consts": g["consts"], "oh": g["oh"], "relb": g[f"relb_{h}"],
                      "rscr": rscr_t.ap(), "rscr_t": rscr_t, "reuse_hn": (h == 1)}
                for nm, _ in P1_W:
                    dr[nm] = g[f"{nm}_{L}_{h}"]
                u = u_scr[L]
                dr["o_att"] = [u[256 * h + hd * 128: 256 * h + (hd + 1) * 128, :] for hd in range(2)]
                dr["o_conv"] = [u[512 + 256 * h + ci * 128: 512 + 256 * h + (ci + 1) * 128, :] for ci in range(2)]
                dr["o_hgrn"] = [u[1024 + 256 * h + hd * 128: 1024 + 256 * h + (hd + 1) * 128, :] for hd in range(2)]
                emit_phase1(C, K, dr, L)
            base = {"wgate": g[f"wgate_{L}"], "wbr": g[f"wbr_{L}"], "wout": g[f"wout_{L}"], "gains": g[f"gains_{L}"]}
            if L == 0:
                for (a0, a1) in ((0, 2064), (2064, 4112)):
                    dr = dict(base)
                    dr.update({"hsT": g["hsT"][:, a0:a1], "uT": u_scr[0][:, a0:a1], "out": hs1[:, a0:a1],
                               "fg": g["fg0"], "fu": g["fu0"], "fd": g["fd0"]})
                    emit_phase2(C, K, dr, a1 - a0, False)
            else:
                dr = dict(base)
                dr.update({"hsT": (hs1[:, 16:2064], hs1[:, 2064:4112]), "uT": (u_scr[1][:, 16:2064], u_scr[1][:, 2064:4112]),
                           "out": out, "selv": g["selv"], "fg": g["fg1"], "fu": g["fu1"], "fd": g["fd1"],
                           "wr": g["wr"], "sel": g["sel"], "ident": g["ident"]})
                emit_phase2(C, K, dr, 2048, True)
        C.S.finalize(final_dma_keys=["out"])
        print("fused ops", len(C.S.ops), "sig", C.S.max_counts, "dma", max(C.S.dma_counts.values()))
    return nc


def kernel(**inputs):
    d = {k: np.asarray(v) for k, v in inputs.items()}
    x = d["x"].astype(np.float32, copy=False)
    B = x.shape[0]
    cores = list(range(8))
    nc = _get_nc("fused", build_fused)
    hsT = [np.ascontiguousarray(np.concatenate([d["meta_tokens"].astype(np.float32), x[b]], axis=0).T) for b in range(B)]
    sel = np.zeros((8, 1024), np.float32)
    for e in range(8):
        sel[e, e * 128:(e + 1) * 128] = 1.0
    cst, oh = phase1_consts()
    common = {"consts": cst, "oh": oh, "sel": sel, "ident": np.eye(128, dtype=np.float32),
              "fg0": d["ffn_w_gate"][0:1], "fu0": d["ffn_w_up"][0:1], "fd0": d["ffn_w_down"][0:1],
              "fg1": d["moe_w_gate"][0], "fu1": d["moe_w_up"][0], "fd1": d["moe_w_down"][0],
              "wr": np.ascontiguousarray(d["router_w"][0])}
    for L in range(2):
        for h in range(2):
            pi = phase1_inputs(d, L, h, None)
            common[f"params_{L}_{h}"] = pi["params"]
            common[f"relb_{h}"] = pi["relb"]
            for nm, _ in P1_W:
                common[f"{nm}_{L}_{h}"] = pi[nm]
        common[f"wgate_{L}"] = np.ascontiguousarray(d["w_in"][L][:, 5120:])
        common[f"wbr_{L}"] = np.ascontiguousarray(d["w_branch"][L].reshape(1536, 1024))
        common[f"wout_{L}"] = np.ascontiguousarray(d["w_out"][L])
        common[f"gains_{L}"] = np.concatenate([_lay_gain(d["norm1_gain"][L]), _lay_gain(d["norm2_gain"][L])], axis=1)
    ins = []
    for c in cores:
        b, h = c // 2, c % 2
        m = dict(common)
        m["hsT"] = hsT[b]
        selv = np.zeros((128, 2), np.float32)
        selv[:, 0] = 1.0 - h
        selv[:, 1] = float(h)
        m["selv"] = selv
        ins.append(m)
    if FDEBUG:
        return run_bass_kernel_spmd(nc, ins, core_ids=cores)
    res = run_bass_kernel_spmd(nc, ins, core_ids=cores)
    out = np.stack([np.concatenate([res.results[2 * b]["out"], res.results[2 * b + 1]["out"]], axis=1).T for b in range(B)])
    return np.ascontiguousarray(out.astype(np.float32))
```

```python
import contextlib
import math
import numpy as np
import ml_dtypes
import concourse.bass as bass
import concourse.mybir as mybir
from concourse.bass_utils import run_bass_kernel_spmd

F32 = mybir.dt.float32
BF16 = mybir.dt.bfloat16
AF = mybir.ActivationFunctionType
ALU = mybir.AluOpType
AX = mybir.AxisListType

D = 1024
T_ALL = 4112
N_META = 16
EPS = 1e-6
DEBUG = False
FDEBUG = False
STRICT_SAME_ENGINE = True
HG_BF16_STATE = False
DEBUG_G = 0
ENGINES = ("tensor", "vector", "scalar", "gpsimd", "sync")


class Buf:
    __slots__ = ("name", "last_w", "readers")

    def __init__(self, name):
        self.name = name
        self.last_w = None
        self.readers = []


class Op:
    __slots__ = ("eng", "fn", "deps", "is_dma", "dma_key", "signal", "sig_cnt", "idx", "dma_deps", "epoch")

    def __init__(self, eng, fn, is_dma=False, dma_key=None):
        self.eng = eng
        self.fn = fn
        self.deps = []
        self.dma_deps = []
        self.is_dma = is_dma
        self.dma_key = dma_key
        self.signal = False
        self.sig_cnt = 0


class Sched:
    def __init__(self, nc):
        self.nc = nc
        self.ops = []
        self.dma_counts = {}
        self.all_bufs = []
        self.epoch = 0

    def buf(self, name="b"):
        b = Buf(name)
        self.all_bufs.append(b)
        return b

    def add(self, eng, fn, reads=(), writes=(), dma_key=None):
        is_dma = dma_key is not None
        op = Op(eng, fn, is_dma, dma_key)
        op.idx = len(self.ops)
        op.epoch = self.epoch
        deps = {}

        def add_dep(p, kind):
            if p is None or p is op:
                return
            if p.eng == eng and not p.is_dma and not is_dma:
                if eng == "tensor" or (kind != "RAW" and not STRICT_SAME_ENGINE):
                    return
            deps[p.idx] = p

        for b in reads:
            add_dep(b.last_w, "RAW")
        for b in writes:
            add_dep(b.last_w, "WAW")
            for r in b.readers:
                add_dep(r, "WAR")
        for b in reads:
            b.readers.append(op)
        for b in writes:
            b.last_w = op
            b.readers = []
        for p in deps.values():
            if p.is_dma:
                op.dma_deps.append((p.dma_key, self.dma_counts[p.dma_key]))
            else:
                p.signal = True
                op.deps.append(p)
        if is_dma:
            self.dma_counts[dma_key] = self.dma_counts.get(dma_key, 0) + 16
        self.ops.append(op)
        return op

    def fence(self, new_epoch=False):
        last = {}
        for op in self.ops:
            if not op.is_dma and op.fn is not None:
                last[op.eng] = op
        dma_now = dict(self.dma_counts)
        for e in ENGINES:
            op = Op(e, None)
            op.idx = len(self.ops)
            op.epoch = self.epoch
            for pe, p in last.items():
                if pe != e:
                    p.signal = True
                    op.deps.append(p)
            for k, c in dma_now.items():
                op.dma_deps.append((k, c))
            self.ops.append(op)
        for b in self.all_bufs:
            b.last_w = None
            b.readers = []
        if new_epoch:
            self.epoch += 1

    def finalize(self, final_dma_keys=()):
        nc = self.nc
        cnt = {}
        for op in self.ops:
            if op.signal and not op.is_dma:
                k = (op.epoch, op.eng)
                cnt[k] = cnt.get(k, 0) + 1
                op.sig_cnt = cnt[k]
        self.max_counts = max(cnt.values()) if cnt else 0
        with contextlib.ExitStack() as st:
            sems = {(ep, e): st.enter_context(nc.semaphore(f"s_{e}{ep}")) for ep in range(self.epoch + 1) for e in ENGINES
                    if (ep, e) in cnt}
            dsems = {k: st.enter_context(nc.semaphore(f"d_{k}")) for k in self.dma_counts}
            block = st.enter_context(nc.Block())
            per_eng = {e: [op for op in self.ops if op.eng == e] for e in ENGINES}

            def emit(eng_name, eng):
                waited = {}
                for op in per_eng[eng_name]:
                    for p in op.deps:
                        key = ("c", p.epoch, p.eng)
                        if waited.get(key, 0) < p.sig_cnt:
                            eng.wait_ge(sems[(p.epoch, p.eng)], p.sig_cnt)
                            waited[key] = p.sig_cnt
                    for (k, c) in op.dma_deps:
                        key = ("d", k)
                        if waited.get(key, 0) < c:
                            eng.wait_ge(dsems[k], c)
                            waited[key] = c
                    if op.fn is None:
                        continue
                    ins = op.fn(eng)
                    if op.is_dma:
                        ins.then_inc(dsems[op.dma_key], 16)
                    elif op.signal:
                        ins.then_inc(sems[(op.epoch, eng_name)], 1)
                if eng_name == "sync":
                    for k in final_dma_keys:
                        eng.wait_ge(dsems[k], self.dma_counts[k])

            block.tensor(lambda e: emit("tensor", e))
            block.vector(lambda e: emit("vector", e))
            block.scalar(lambda e: emit("scalar", e))
            block.gpsimd(lambda e: emit("gpsimd", e))
            block.sync(lambda e: emit("sync", e))


class Ctx:
    ARENA_BYTES = 212480

    def __init__(self, nc, st):
        self.nc = nc
        self.st = st
        self.S = Sched(nc)
        self.arena = st.enter_context(nc.sbuf_tensor("arena", [128, self.ARENA_BYTES // 4], F32))
        self.arena_bf = self.arena.bitcast(BF16)
        self.off = 0
        self.psum = []
        for i in range(8):
            t = st.enter_context(nc.psum_tensor(f"ps{i}", [128, 512], F32))
            self.psum.append((t, self.S.buf(f"ps{i}")))
        self.pi = 0
        self.pool = list(range(8))
        self.tmps = {}

    def alloc(self, nelem, dt):
        sz = 4 if dt == F32 else 2
        nbytes = (nelem * sz + 3) // 4 * 4
        assert self.off + nbytes <= self.ARENA_BYTES, f"arena overflow {self.off + nbytes}"
        o = self.off
        self.off += nbytes
        if dt == F32:
            return self.arena[:, o // 4: o // 4 + nelem]
        return self.arena_bf[:, o // 2: o // 2 + nelem]

    def mark(self):
        return self.off

    def reset(self, m):
        self.off = m

    def ps(self):
        self.pi = (self.pi + 1) % len(self.pool)
        return self.psum[self.pool[self.pi]]

    def reserve(self, k):
        r = [self.psum[i] for i in self.pool[-k:]]
        self.pool = self.pool[:-k]
        self.pi = 0
        return r

    def release(self):
        self.pool = list(range(8))
        self.pi = 0

    def tmp(self, kind, nelem, dt, n=2):
        if kind not in self.tmps:
            self.tmps[kind] = [[(self.alloc(nelem, dt), self.S.buf(f"{kind}{i}")) for i in range(n)], 0]
        lst, i = self.tmps[kind]
        self.tmps[kind][1] = (i + 1) % len(lst)
        return lst[i]

    def drop_tmps(self):
        self.tmps = {}


GS = 512


def make_groups(n, gs=None):
    gs = gs or GS
    g = []
    t = 0
    while t < n:
        w = min(gs, n - t)
        g.append((t, w))
        t += w
    return g


def kview(ap, p=128):
    return ap.rearrange("(kc p) n -> p kc n", p=p)


def emit_consts(C):
    S = C.S
    k = {}
    k["ones"] = C.alloc(128, BF16)
    k["B_ones"] = S.buf("ones")
    S.add("vector", lambda e: e.memset(k["ones"], 1.0 / 1024.0), writes=[k["B_ones"]])
    k["eps"] = C.alloc(1, F32)
    k["B_eps"] = S.buf("eps")
    S.add("vector", lambda e: e.memset(k["eps"], EPS), writes=[k["B_eps"]])
    return k


def emit_rmsnorm_group(C, K, hs, N, B_hs, gain, B_gain, out, out_stride, B_out, gi, t0, w, out_t0):
    S = C.S
    ps, Bps = C.ps()
    for c in range(8):
        sq, Bsq = C.tmp("sq", 512, BF16, 2)
        S.add("scalar", lambda e, c=c, sq=sq: e.activation(out=sq[:, :w], in_=hs[:, c * N + t0: c * N + t0 + w], func=AF.Square),
              reads=[B_hs[c][gi]], writes=[Bsq])
        S.add("tensor", lambda e, c=c, sq=sq: e.matmul(ps[:, :w], K["ones"], sq[:, :w], start=(c == 0), stop=(c == 7)),
              reads=[Bsq, K["B_ones"]], writes=[Bps])
    rstd, Brstd = C.tmp("rstd", 512, F32, 2)
    S.add("scalar", lambda e: e.activation(out=rstd[:, :w], in_=ps[:, :w], func=AF.Ln, bias=K["eps"], scale=1.0),
          reads=[Bps, K["B_eps"]], writes=[Brstd])
    S.add("scalar", lambda e: e.activation(out=rstd[:, :w], in_=rstd[:, :w], func=AF.Exp, scale=-0.5),
          reads=[Brstd], writes=[Brstd])
    for c in range(8):
        S.add("vector", lambda e, c=c: e.scalar_tensor_tensor(
            out=out[:, c * out_stride + out_t0: c * out_stride + out_t0 + w], in0=hs[:, c * N + t0: c * N + t0 + w],
            scalar=gain[:, c:c + 1], in1=rstd[:, :w], op0=ALU.mult, op1=ALU.mult),
            reads=[B_hs[c][gi], Brstd, B_gain], writes=[B_out[c]])


def build_phase2(N, moe):
    nc = bass.Bass("TRN2", target_bir_lowering=False)
    dr = {}
    dr["hsT"] = nc.dram_tensor("hsT", [D, N], F32, kind="ExternalInput").ap()
    dr["uT"] = nc.dram_tensor("uT", [1536, N], BF16, kind="ExternalInput").ap()
    dr["wgate"] = nc.dram_tensor("wgate", [D, 3072], F32, kind="ExternalInput").ap()
    dr["wbr"] = nc.dram_tensor("wbr", [1536, D], F32, kind="ExternalInput").ap()
    dr["wout"] = nc.dram_tensor("wout", [D, D], F32, kind="ExternalInput").ap()
    dr["gains"] = nc.dram_tensor("gains", [128, 16], F32, kind="ExternalInput").ap()
    if moe:
        FF = 3584
        dr["fg"] = nc.dram_tensor("fg", [8, D, FF], F32, kind="ExternalInput").ap()
        dr["fu"] = nc.dram_tensor("fu", [8, D, FF], F32, kind="ExternalInput").ap()
        dr["fd"] = nc.dram_tensor("fd", [8, FF, D], F32, kind="ExternalInput").ap()
        dr["wr"] = nc.dram_tensor("wr", [D, 8], F32, kind="ExternalInput").ap()
        dr["sel"] = nc.dram_tensor("sel", [8, 8 * 128], F32, kind="ExternalInput").ap()
        dr["ident"] = nc.dram_tensor("ident", [128, 128], F32, kind="ExternalInput").ap()
    else:
        FF = 2816
        dr["fg"] = nc.dram_tensor("fg", [1, D, FF], F32, kind="ExternalInput").ap()
        dr["fu"] = nc.dram_tensor("fu", [1, D, FF], F32, kind="ExternalInput").ap()
        dr["fd"] = nc.dram_tensor("fd", [1, FF, D], F32, kind="ExternalInput").ap()
    dr["out"] = nc.dram_tensor("hsT_out", [D, N], F32, kind="ExternalOutput").ap()
    if DEBUG:
        dr["dbg1"] = nc.dram_tensor("dbg1", [128, 8 * 512], BF16, kind="ExternalOutput").ap()
        dr["dbg2"] = nc.dram_tensor("dbg2", [D, N], F32, kind="ExternalOutput").ap()
        dr["dbg3"] = nc.dram_tensor("dbg3", [128, 8 * 512], BF16, kind="ExternalOutput").ap()
        dr["dbg4"] = nc.dram_tensor("dbg4", [128, 512], F32, kind="ExternalOutput").ap()
        dr["dbg5"] = nc.dram_tensor("dbg5", [128, 512], F32, kind="ExternalOutput").ap()

    with contextlib.ExitStack() as st:
        C = Ctx(nc, st)
        K = emit_consts(C)
        emit_phase2(C, K, dr, N, moe)
        C.S.finalize(final_dma_keys=["out"])
        print("phase2 ops", len(C.S.ops), "sig", C.S.max_counts)
    return nc


def emit_phase2(C, K, dr, N, moe):
    S = C.S
    FF = 3584 if moe else 2816
    m_base = C.mark()
    groups = make_groups(N)
    NG = len(groups)
    hs = C.alloc(8 * N, F32)
    B_hs = [[S.buf(f"hs{c}_{g}") for g in range(NG)] for c in range(8)]
    gains = C.alloc(16, F32)
    B_gain = S.buf("gains")
    S.add("sync", lambda e: e.dma_start(out=gains, in_=dr["gains"]), writes=[B_gain], dma_key="gains")
    selv = None
    if isinstance(dr["hsT"], tuple):
        selv = C.alloc(2, F32)
        B_selv = S.buf("selv")
        S.add("sync", lambda e: e.dma_start(out=selv, in_=dr["selv"]), writes=[B_selv], dma_key="selv")
        hvA, hvB = kview(dr["hsT"][0]), kview(dr["hsT"][1])
        m_tmp = C.mark()
        tb = C.alloc(N, F32)
        B_tb = S.buf("tb")

        def hs_blend(c):
            S.add("sync", lambda e: e.dma_start(out=hs[:, c * N:(c + 1) * N], in_=hvA[:, c, :]), writes=B_hs[c], dma_key="hs_in")
            S.add("sync", lambda e: e.dma_start(out=tb, in_=hvB[:, c, :]), writes=[B_tb], dma_key="hs_inb")
            S.add("vector", lambda e: e.tensor_scalar(tb, tb, selv[:, 1:2], None, ALU.mult), reads=[B_tb, B_selv], writes=[B_tb])
            S.add("vector", lambda e: e.scalar_tensor_tensor(out=hs[:, c * N:(c + 1) * N], in0=hs[:, c * N:(c + 1) * N], scalar=selv[:, 0:1],
                                                             in1=tb, op0=ALU.mult, op1=ALU.add), reads=B_hs[c] + [B_tb, B_selv], writes=B_hs[c])
        for c in range(8):
            hs_blend(c)
        S.fence()
        C.reset(m_tmp)
    else:
        hsv = kview(dr["hsT"])
        for c in range(8):
            S.add("sync", lambda e, c=c: e.dma_start(out=hs[:, c * N:(c + 1) * N], in_=hsv[:, c, :]),
                  writes=B_hs[c], dma_key="hs_in")
    m_persist = C.mark()

    wg = C.alloc(8 * 3072, BF16)
    wb = C.alloc(12 * 1024, BF16)
    wo = C.alloc(8 * 1024, BF16)
    B_wg, B_wb, B_wo = S.buf("wg"), S.buf("wb"), S.buf("wo")
    wgv = kview(dr["wgate"])
    for b in range(3):
        S.add("gpsimd", lambda e, b=b: e.dma_start(
            out=wg.rearrange("p (k n) -> p k n", k=8)[:, :, b * 1024:(b + 1) * 1024], in_=wgv[:, :, b * 1024:(b + 1) * 1024]),
            writes=[B_wg], dma_key="wg")
    S.add("gpsimd", lambda e: e.dma_start(out=wb.rearrange("p (k n) -> p k n", k=12), in_=kview(dr["wbr"])),
          writes=[B_wb], dma_key="wb")
    S.add("gpsimd", lambda e: e.dma_start(out=wo.rearrange("p (k n) -> p k n", k=8), in_=kview(dr["wout"])),
          writes=[B_wo], dma_key="wo")
    hn_g = C.alloc(8 * 512, BF16)
    B_hng = [S.buf(f"hng{c}") for c in range(8)]
    u_g = C.alloc(12 * 512, BF16)
    B_ug = S.buf("ug")
    mixed = C.alloc(8 * 512, BF16)
    B_mixed = [S.buf(f"mixed{j}") for j in range(8)]
    if selv is None:
        uv = kview(dr["uT"])
    else:
        uvA, uvB = kview(dr["uT"][0]), kview(dr["uT"][1])
        u_b = C.alloc(12 * 512, BF16)
        B_ub = S.buf("ub")
    def stage_b_group(gi, t0, w):
        if selv is None:
            S.add("sync", lambda e: e.dma_start(
                out=u_g.rearrange("p (k n) -> p k n", k=12)[:, :, :w], in_=uv[:, :, t0:t0 + w]),
                writes=[B_ug], dma_key="ug")
        else:
            S.add("sync", lambda e: e.dma_start(
                out=u_g.rearrange("p (k n) -> p k n", k=12)[:, :, :w], in_=uvA[:, :, t0:t0 + w]), writes=[B_ug], dma_key="ug")
            S.add("sync", lambda e: e.dma_start(
                out=u_b.rearrange("p (k n) -> p k n", k=12)[:, :, :w], in_=uvB[:, :, t0:t0 + w]), writes=[B_ub], dma_key="ugb")
            S.add("vector", lambda e: e.tensor_scalar(u_b, u_b, selv[:, 1:2], None, ALU.mult), reads=[B_ub, B_selv], writes=[B_ub])
            S.add("vector", lambda e: e.scalar_tensor_tensor(out=u_g, in0=u_g, scalar=selv[:, 0:1], in1=u_b, op0=ALU.mult, op1=ALU.add),
                  reads=[B_ug, B_ub, B_selv], writes=[B_ug])
        emit_rmsnorm_group(C, K, hs, N, B_hs, gains, B_gain, hn_g, 512, B_hng, gi, t0, w, 0)
        for j in range(8):
            acc, Bacc = C.tmp("acc", 512, F32, 1)
            for b in range(3):
                psg, Bpsg = C.ps()
                for kc in range(8):
                    S.add("tensor", lambda e, kc=kc, b=b, j=j, psg=psg: e.matmul(
                        psg[:, :w], wg[:, kc * 3072 + b * 1024 + j * 128: kc * 3072 + b * 1024 + (j + 1) * 128],
                        hn_g[:, kc * 512: kc * 512 + w], start=(kc == 0), stop=(kc == 7)),
                        reads=[B_wg, B_hng[kc]], writes=[Bpsg])
                sg, Bsg = C.tmp("sg", 512, F32, 2)
                S.add("scalar", lambda e, psg=psg, sg=sg: e.activation(out=sg[:, :w], in_=psg[:, :w], func=AF.Sigmoid),
                      reads=[Bpsg], writes=[Bsg])
                psu, Bpsu = C.ps()
                for kc in range(4):
                    S.add("tensor", lambda e, kc=kc, b=b, j=j, psu=psu: e.matmul(
                        psu[:, :w], wb[:, (b * 4 + kc) * 1024 + j * 128: (b * 4 + kc) * 1024 + (j + 1) * 128],
                        u_g[:, (b * 4 + kc) * 512: (b * 4 + kc) * 512 + w], start=(kc == 0), stop=(kc == 3)),
                        reads=[B_wb, B_ug], writes=[Bpsu])
                if b == 0:
                    S.add("vector", lambda e, psu=psu, sg=sg, acc=acc: e.tensor_tensor(
                        out=acc[:, :w], in0=psu[:, :w], in1=sg[:, :w], op=ALU.mult), reads=[Bpsu, Bsg], writes=[Bacc])
                    if DEBUG and gi == DEBUG_G and j == 0:
                        S.add("sync", lambda e, sg=sg: e.dma_start(out=dr["dbg4"], in_=sg), reads=[Bsg], dma_key="dbg")
                        S.add("sync", lambda e, acc=acc: e.dma_start(out=dr["dbg5"], in_=acc), reads=[Bacc], dma_key="dbg")
                else:
                    t2, Bt2 = C.tmp("t2", 512, F32, 1)
                    S.add("vector", lambda e, psu=psu, sg=sg, t2=t2: e.tensor_tensor(
                        out=t2[:, :w], in0=psu[:, :w], in1=sg[:, :w], op=ALU.mult), reads=[Bpsu, Bsg], writes=[Bt2])
                    if b == 1:
                        S.add("vector", lambda e, t2=t2, acc=acc: e.tensor_tensor(
                            out=acc[:, :w], in0=acc[:, :w], in1=t2[:, :w], op=ALU.add), reads=[Bacc, Bt2], writes=[Bacc])
                    else:
                        S.add("vector", lambda e, t2=t2, acc=acc, j=j: e.tensor_tensor(
                            out=mixed[:, j * 512: j * 512 + w], in0=acc[:, :w], in1=t2[:, :w], op=ALU.add),
                            reads=[Bacc, Bt2], writes=[B_mixed[j]])
        if DEBUG and gi == DEBUG_G:
            S.add("sync", lambda e: e.dma_start(out=dr["dbg1"], in_=hn_g), reads=B_hng, dma_key="dbg")
            S.add("sync", lambda e: e.dma_start(out=dr["dbg3"], in_=mixed), reads=B_mixed, dma_key="dbg")
        for j2 in range(8):
            pso, Bpso = C.ps()
            for kc in range(8):
                S.add("tensor", lambda e, kc=kc, j2=j2, pso=pso: e.matmul(
                    pso[:, :w], wo[:, kc * 1024 + j2 * 128: kc * 1024 + (j2 + 1) * 128],
                    mixed[:, kc * 512: kc * 512 + w], start=(kc == 0), stop=(kc == 7)),
                    reads=[B_wo, B_mixed[kc]], writes=[Bpso])
            S.add("vector", lambda e, j2=j2, pso=pso, t0=t0: e.tensor_tensor(
                out=hs[:, j2 * N + t0: j2 * N + t0 + w], in0=pso[:, :w], in1=hs[:, j2 * N + t0: j2 * N + t0 + w], op=ALU.add),
                reads=[Bpso, B_hs[j2][gi]], writes=[B_hs[j2][gi]])

    for gi, (t0, w) in enumerate(groups):
        stage_b_group(gi, t0, w)
    if DEBUG:
        for c in range(8):
            S.add("sync", lambda e, c=c: e.dma_start(out=kview(dr["dbg2"])[:, c, :], in_=hs[:, c * N:(c + 1) * N]),
                  reads=B_hs[c], dma_key="dbg")
    S.fence()
    C.reset(m_persist)
    C.drop_tmps()
    hn2 = C.alloc(8 * N, BF16)
    B_hn2 = [[S.buf(f"hn2_{c}_{g}") for g in range(NG)] for c in range(8)]
    for gi, (t0, w) in enumerate(groups):
        emit_rmsnorm_group(C, K, hs, N, B_hs, gains[:, 8:16], B_gain, hn2, N, [B_hn2[c][gi] for c in range(8)], gi, t0, w, t0)
    n_exp = 8 if moe else 1
    nch = FF // 128
    slabs = []
    c0 = 0
    while c0 < nch:
        n = min(4, nch - c0)
        slabs.append((c0, n))
        c0 += n
    NB = 2
    wgs = [C.alloc(8 * 512, BF16) for _ in range(NB)]
    wus = [C.alloc(8 * 512, BF16) for _ in range(NB)]
    wds = [C.alloc(4 * 1024, BF16) for _ in range(NB)]
    B_wgs = [S.buf("wgs") for _ in range(NB)]
    B_wus = [S.buf("wus") for _ in range(NB)]
    B_wds = [S.buf("wds") for _ in range(NB)]
    acts = [C.alloc(4 * 512, BF16) for _ in range(2)]
    B_act = [[S.buf("act") for _ in range(4)] for _ in range(2)]
    if moe:
        emit_router(C, K, dr, hn2, B_hn2, N, groups)
    def stage_d_group(ex, n, sl, a, gi, t0, w):
        act = acts[a]
        if moe:
            cb, Bcb = emit_cb(C, K, ex, gi, t0, w)
        for i in range(n):
            ps1, Bps1 = C.ps()
            for kc in range(8):
                S.add("tensor", lambda e, kc=kc, i=i, ps1=ps1, sl=sl, t0=t0: e.matmul(
                    ps1[:, :w], wgs[sl][:, kc * 512 + i * 128: kc * 512 + (i + 1) * 128],
                    hn2[:, kc * N + t0: kc * N + t0 + w], start=(kc == 0), stop=(kc == 7)),
                    reads=[B_wgs[sl], B_hn2[kc][gi]], writes=[Bps1])
            ps2, Bps2 = C.ps()
            for kc in range(8):
                S.add("tensor", lambda e, kc=kc, i=i, ps2=ps2, sl=sl, t0=t0: e.matmul(
                    ps2[:, :w], wus[sl][:, kc * 512 + i * 128: kc * 512 + (i + 1) * 128],
                    hn2[:, kc * N + t0: kc * N + t0 + w], start=(kc == 0), stop=(kc == 7)),
                    reads=[B_wus[sl], B_hn2[kc][gi]], writes=[Bps2])
            sl_t, Bsl = C.tmp("silu", 512, F32, 2)
            S.add("scalar", lambda e, ps1=ps1, sl_t=sl_t: e.activation(out=sl_t[:, :w], in_=ps1[:, :w], func=AF.Silu),
                  reads=[Bps1], writes=[Bsl])
            if moe:
                S.add("vector", lambda e, sl_t=sl_t, cb=cb: e.tensor_tensor(
                    out=sl_t[:, :w], in0=sl_t[:, :w], in1=cb[:, :w], op=ALU.mult), reads=[Bsl, Bcb], writes=[Bsl])
            S.add("vector", lambda e, ps2=ps2, sl_t=sl_t, act=act, i=i: e.tensor_tensor(
                out=act[:, i * 512: i * 512 + w], in0=ps2[:, :w], in1=sl_t[:, :w], op=ALU.mult),
                reads=[Bps2, Bsl], writes=[B_act[a][i]])
        for j in range(8):
            psd, Bpsd = C.ps()
            for i in range(n):
                S.add("tensor", lambda e, i=i, j=j, psd=psd, sl=sl, act=act: e.matmul(
                    psd[:, :w], wds[sl][:, i * 1024 + j * 128: i * 1024 + (j + 1) * 128],
                    act[:, i * 512: i * 512 + w], start=(i == 0), stop=(i == n - 1)),
                    reads=[B_wds[sl], B_act[a][i]], writes=[Bpsd])
            S.add("vector", lambda e, j=j, psd=psd, t0=t0: e.tensor_tensor(
                out=hs[:, j * N + t0: j * N + t0 + w], in0=psd[:, :w], in1=hs[:, j * N + t0: j * N + t0 + w], op=ALU.add),
                reads=[Bpsd, B_hs[j][gi]], writes=[B_hs[j][gi]])

    si = 0
    ai = 0
    for ex in range(n_exp):
        for (c0, n) in slabs:
            sl = si % NB
            si += 1
            S.add("gpsimd", lambda e, ex=ex, c0=c0, n=n, sl=sl: e.dma_start(
                out=wgs[sl].rearrange("p (k n) -> p k n", k=8)[:, :, :n * 128],
                in_=kview(dr["fg"][ex])[:, :, c0 * 128:(c0 + n) * 128]), writes=[B_wgs[sl]], dma_key=f"wgs{sl}")
            S.add("gpsimd", lambda e, ex=ex, c0=c0, n=n, sl=sl: e.dma_start(
                out=wus[sl].rearrange("p (k n) -> p k n", k=8)[:, :, :n * 128],
                in_=kview(dr["fu"][ex])[:, :, c0 * 128:(c0 + n) * 128]), writes=[B_wus[sl]], dma_key=f"wus{sl}")
            S.add("gpsimd", lambda e, ex=ex, c0=c0, n=n, sl=sl: e.dma_start(
                out=wds[sl].rearrange("p (k n) -> p k n", k=4)[:, :n, :],
                in_=kview(dr["fd"][ex][c0 * 128:(c0 + n) * 128, :])), writes=[B_wds[sl]], dma_key=f"wds{sl}")
            for gi, (t0, w) in enumerate(groups):
                a = ai % 2
                ai += 1
                stage_d_group(ex, n, sl, a, gi, t0, w)
    ov = kview(dr["out"])
    for c in range(8):
        S.add("sync", lambda e, c=c: e.dma_start(out=ov[:, c, :], in_=hs[:, c * N:(c + 1) * N]),
              reads=B_hs[c], dma_key="out")
    S.fence(new_epoch=True)
    C.reset(m_base)
    C.drop_tmps()


def emit_router(C, K, dr, hn2, B_hn2, N, groups):
    S = C.S
    wr = C.alloc(8 * 8, BF16)
    B_wr = S.buf("wr")
    S.add("gpsimd", lambda e: e.dma_start(out=wr.rearrange("p (k n) -> p k n", k=8), in_=kview(dr["wr"])), writes=[B_wr], dma_key="wr")
    ident = C.alloc(128, F32)
    B_id = S.buf("ident")
    S.add("sync", lambda e: e.dma_start(out=ident, in_=dr["ident"]), writes=[B_id], dma_key="ident")
    sel = C.alloc(1024, BF16)
    B_sel = S.buf("sel")
    S.add("gpsimd", lambda e: e.dma_start(out=sel[0:8, :], in_=dr["sel"]), writes=[B_sel], dma_key="sel")
    combT = C.alloc(N, BF16)
    B_combT = [S.buf(f"combT{g}") for g in range(len(groups))]
    K["sel"], K["B_sel"], K["combT"], K["B_combT"] = sel, B_sel, combT, B_combT

    def tile(tt):
        c0 = tt * 128
        gi = c0 // 512
        ps, Bps = C.ps()
        for kc in range(8):
            S.add("tensor", lambda e, kc=kc: e.matmul(ps[:, 0:8], hn2[:, kc * N + c0: kc * N + c0 + 128], wr[:, kc * 8:(kc + 1) * 8],
                                                       start=(kc == 0), stop=(kc == 7)), reads=[B_hn2[kc][gi], B_wr], writes=[Bps])
        lg, Blg = C.tmp("lg", 8, F32, 2)
        S.add("vector", lambda e: e.tensor_copy(out=lg, in_=ps[:, 0:8]), reads=[Bps], writes=[Blg])
        mx, Bmx = C.tmp("mx", 8, F32, 2)
        S.add("vector", lambda e: e.max(out=mx, in_=lg), reads=[Blg], writes=[Bmx])
        nm1, Bnm1 = C.tmp("nm1", 1, F32, 2)
        S.add("vector", lambda e: e.tensor_scalar(nm1, mx[:, 0:1], -1.0, None, ALU.mult), reads=[Bmx], writes=[Bnm1])
        ex, Bex = C.tmp("ex", 8, F32, 2)
        S.add("scalar", lambda e: e.activation(out=ex, in_=lg, func=AF.Exp, bias=nm1, scale=1.0), reads=[Blg, Bnm1], writes=[Bex])
        mask, Bmask = C.tmp("mask", 8, F32, 2)
        S.add("vector", lambda e: e.tensor_scalar(mask, lg, mx[:, 1:2], None, ALU.is_ge), reads=[Blg, Bmx], writes=[Bmask])
        num, Bnum = C.tmp("num", 8, F32, 2)
        S.add("vector", lambda e: e.tensor_tensor(out=num, in0=mask, in1=ex, op=ALU.mult), reads=[Bmask, Bex], writes=[Bnum])
        den, Bden = C.tmp("den", 1, F32, 2)
        S.add("vector", lambda e: e.tensor_reduce(out=den, in_=num, axis=AX.X, op=ALU.add), reads=[Bnum], writes=[Bden])
        S.add("vector", lambda e: e.reciprocal(out=den, in_=den), reads=[Bden], writes=[Bden])
        comb, Bcomb = C.tmp("comb", 8, F32, 2)
        S.add("vector", lambda e: e.tensor_scalar(comb, num, den[:, 0:1], None, ALU.mult), reads=[Bnum, Bden], writes=[Bcomb])
        pt, Bpt = C.ps()
        S.add("tensor", lambda e: e.matmul(pt[0:8, 0:128], comb, ident, start=True, stop=True), reads=[Bcomb, B_id], writes=[Bpt])
        S.add("scalar", lambda e: e.activation(out=combT[0:8, c0:c0 + 128], in_=pt[0:8, 0:128], func=AF.Copy),
              reads=[Bpt], writes=[B_combT[gi]])

    for tt in range(N // 128):
        tile(tt)


def emit_cb(C, K, ex, gi, t0, w):
    S = C.S
    ps, Bps = C.ps()
    S.add("tensor", lambda e: e.matmul(ps[:, :w], K["sel"][0:8, ex * 128:(ex + 1) * 128], K["combT"][0:8, t0:t0 + w], start=True, stop=True),
          reads=[K["B_sel"], K["B_combT"][gi]], writes=[Bps])
    cb, Bcb = C.tmp("cb", 512, F32, 2)
    S.add("scalar", lambda e: e.activation(out=cb[:, :w], in_=ps[:, :w], func=AF.Copy), reads=[Bps], writes=[Bcb])
    return cb, Bcb


TP = 4224
NT = 33
RL = 1152


def build_phase1(layer, parts=("conv", "hgrn", "att")):
    T = T_ALL
    nc = bass.Bass("TRN2", target_bir_lowering=False)
    dr = {}
    dr["hsT"] = nc.dram_tensor("hsT", [D, T], F32, kind="ExternalInput").ap()
    dr["params"] = nc.dram_tensor("params", [128, 280], F32, kind="ExternalInput").ap()
    dr["consts"] = nc.dram_tensor("consts", [128, 896], F32, kind="ExternalInput").ap()
    dr["oh"] = nc.dram_tensor("oh", [33, RL], F32, kind="ExternalInput").ap()
    dr["relb"] = nc.dram_tensor("relb", [32, 2], F32, kind="ExternalInput").ap()
    dr["wqk"] = nc.dram_tensor("wqk", [D, 512], F32, kind="ExternalInput").ap()
    dr["wv"] = nc.dram_tensor("wv", [D, 256], F32, kind="ExternalInput").ap()
    dr["wconv"] = nc.dram_tensor("wconv", [D, 768], F32, kind="ExternalInput").ap()
    dr["whg"] = nc.dram_tensor("whg", [D, 768], F32, kind="ExternalInput").ap()
    dr["wi"] = nc.dram_tensor("wi", [D, 256], F32, kind="ExternalInput").ap()
    rscr_t = nc.dram_tensor("rscr", [2, RL], F32, kind="Internal")
    dr["rscr"] = rscr_t.ap()
    dr["rscr_t"] = rscr_t
    dr["out"] = nc.dram_tensor("uT", [768, T], BF16, kind="ExternalOutput").ap()
    dr["o_att"] = [dr["out"][hd * 128:(hd + 1) * 128, :] for hd in range(2)]
    dr["o_conv"] = [dr["out"][256 + ci * 128: 256 + (ci + 1) * 128, :] for ci in range(2)]
    dr["o_hgrn"] = [dr["out"][512 + hd * 128: 512 + (hd + 1) * 128, :] for hd in range(2)]

    with contextlib.ExitStack() as st:
        C = Ctx(nc, st)
        K = emit_consts(C)
        emit_phase1(C, K, dr, layer, parts)
        C.S.finalize(final_dma_keys=["out"])
        print("phase1 ops", len(C.S.ops), "sig", C.S.max_counts)
    return nc


def emit_phase1(C, K, dr, layer, parts=("conv", "hgrn", "att")):
    S = C.S
    T = T_ALL
    lam_init = 0.8 - 0.6 * math.exp(-0.3 * layer)
    m_base = C.mark()
    groups = make_groups(T)
    NG = len(groups)
    reuse = bool(dr.get("reuse_hn"))
    prm = C.alloc(280, F32)
    B_prm = S.buf("prm")
    S.add("sync", lambda e: e.dma_start(out=prm, in_=dr["params"]), writes=[B_prm], dma_key="prm")
    cst = C.alloc(896, F32)
    B_cst = S.buf("cst")
    if not reuse:
        S.add("sync", lambda e: e.dma_start(out=cst, in_=dr["consts"]), writes=[B_cst], dma_key="cst")
    ident_f = cst[:, 0:128]
    J_f = cst[:, 128:256]
    hmask = cst[:, 256:384]
    scanmask = cst[:, 384:896]
    ident_b = C.alloc(128, BF16)
    B_idb = S.buf("identb")
    ones1 = C.alloc(128, BF16)
    B_ones1 = S.buf("ones1")
    onesd = C.alloc(128, BF16)
    B_onesd = S.buf("onesd")
    blk64 = C.alloc(128, BF16)
    B_blk = S.buf("blk64")
    if not reuse:
        S.add("vector", lambda e: e.tensor_copy(out=ident_b, in_=ident_f), reads=[B_cst], writes=[B_idb])
        S.add("vector", lambda e: e.memset(ones1, 1.0), writes=[B_ones1])
        S.add("vector", lambda e: e.memset(onesd, 1.0 / 128.0), writes=[B_onesd])
        S.add("vector", lambda e: e.memset(blk64, 0.0), writes=[B_blk])
        S.add("vector", lambda e: e.memset(blk64[0:64, 0:64], 1.0 / 64.0), writes=[B_blk])
        S.add("vector", lambda e: e.memset(blk64[64:128, 64:128], 1.0 / 64.0), writes=[B_blk])

    hn = C.alloc(8 * TP, BF16)
    B_hn = [[S.buf(f"hn{c}_{g}") for g in range(NG + 1)] for c in range(8)]
    if not reuse:
        for c in range(8):
            S.add("gpsimd", lambda e, c=c: e.memset(hn[:, c * TP + T: (c + 1) * TP], 0.0), writes=[B_hn[c][NG]])
    m_main = C.mark()
    hsb = [C.alloc(8 * 512, F32) for _ in range(2)]
    B_hsb = [[S.buf(f"hsb{i}_{c}") for c in range(8)] for i in range(2)]
    hsv = kview(dr["hsT"])

    def norm_group(gi, t0, w):
        i = gi % 2
        S.add("sync", lambda e: e.dma_start(out=hsb[i].rearrange("p (k n) -> p k n", k=8)[:, :, :w], in_=hsv[:, :, t0:t0 + w]),
              writes=B_hsb[i], dma_key=f"hsb{i}")
        emit_rmsnorm_group(C, K, hsb[i], 512, [[B_hsb[i][c]] for c in range(8)], prm, B_prm, hn, TP,
                           [B_hn[c][gi] for c in range(8)], 0, 0, w, t0)

    def hn_bufs(kc, t0, w):
        g0 = t0 // 512
        g1 = min((t0 + w - 1) // 512, NG)
        return [B_hn[kc][g] for g in range(g0, g1 + 1)]

    do_conv = "conv" in parts
    if do_conv:
        wc = C.alloc(8 * 768, BF16)
        B_wc = S.buf("wc")
        S.add("gpsimd", lambda e: e.dma_start(out=wc.rearrange("p (k n) -> p k n", k=8), in_=kview(dr["wconv"])), writes=[B_wc], dma_key="wc")
        zb = [C.alloc(516, F32) for _ in range(2)]
        B_z = [S.buf("z0"), S.buf("z1")]
        for ci in range(2):
            S.add("vector", lambda e, ci=ci: e.memset(zb[ci][:, 0:2], 0.0), writes=[B_z[ci]])

    def conv_group(gi, t0, w):
        for ci in range(2):
            conv_chunk(gi, t0, w, ci)

    def conv_chunk(gi, t0, w, ci):
        if True:
            pss = []
            for br in range(3):
                ps, Bps = C.ps()
                for kc in range(8):
                    S.add("tensor", lambda e, kc=kc, br=br, ps=ps: e.matmul(
                        ps[:, :w], wc[:, kc * 768 + br * 256 + ci * 128: kc * 768 + br * 256 + (ci + 1) * 128],
                        hn[:, kc * TP + t0: kc * TP + t0 + w], start=(kc == 0), stop=(kc == 7)),
                        reads=[B_wc] + hn_bufs(kc, t0, w), writes=[Bps])
                pss.append((ps, Bps))
            (pb, Bpb), (pc, Bpc), (ph, Bph) = pss
            z = zb[ci]
            th, Bth = C.tmp("ch", 512, F32, 2)
            S.add("scalar", lambda e: e.activation(out=th[:, :w], in_=ph[:, :w], func=AF.Copy), reads=[Bph], writes=[Bth])
            S.add("vector", lambda e: e.tensor_tensor(out=z[:, 2:2 + w], in0=pc[:, :w], in1=th[:, :w], op=ALU.mult),
                  reads=[Bpc, Bth], writes=[B_z[ci]])
            y, By = C.tmp("y", 512, F32, 2)
            wcol = 14 + ci * 3
            S.add("vector", lambda e: e.tensor_scalar(y[:, :w], z[:, 2:2 + w], prm[:, wcol + 2:wcol + 3], None, ALU.mult),
                  reads=[B_z[ci], B_prm], writes=[By])
            S.add("vector", lambda e: e.scalar_tensor_tensor(out=y[:, :w], in0=z[:, 1:1 + w], scalar=prm[:, wcol + 1:wcol + 2],
                                                             in1=y[:, :w], op0=ALU.mult, op1=ALU.add),
                  reads=[B_z[ci], B_prm, By], writes=[By])
            S.add("vector", lambda e: e.scalar_tensor_tensor(out=y[:, :w], in0=z[:, 0:w], scalar=prm[:, wcol:wcol + 1],
                                                             in1=y[:, :w], op0=ALU.mult, op1=ALU.add),
                  reads=[B_z[ci], B_prm, By], writes=[By])
            uo, Buo = C.tmp("uo", 512, BF16, 3)
            S.add("vector", lambda e: e.tensor_tensor(out=uo[:, :w], in0=pb[:, :w], in1=y[:, :w], op=ALU.mult),
                  reads=[Bpb, By], writes=[Buo])
            S.add("sync", lambda e: e.dma_start(out=dr["o_conv"][ci][:, t0:t0 + w], in_=uo[:, :w]),
                  reads=[Buo], dma_key="out")
            if w >= 2:
                S.add("vector", lambda e: e.tensor_copy(out=z[:, 0:2], in_=z[:, w:w + 2]), reads=[B_z[ci]], writes=[B_z[ci]])


    if not reuse:
        for gi, (t0, w) in enumerate(groups):
            norm_group(gi, t0, w)
    if do_conv:
        for gi, (t0, w) in enumerate(groups):
            conv_group(gi, t0, w)
    S.fence()
    C.reset(m_main)
    C.drop_tmps()

    if "hgrn" in parts:
        m0 = C.mark()
        whg = C.alloc(8 * 768, BF16)
        B_whg = S.buf("whg")
        S.add("gpsimd", lambda e: e.dma_start(out=whg.rearrange("p (k n) -> p k n", k=8), in_=kview(dr["whg"])), writes=[B_whg], dma_key="whg")
        wi = C.alloc(8 * 256, BF16)
        B_wi = S.buf("wi")
        S.add("gpsimd", lambda e: e.dma_start(out=wi.rearrange("p (k n) -> p k n", k=8), in_=kview(dr["wi"])), writes=[B_wi], dma_key="wi")
        hmask_b = hmask
        lb = C.alloc(2, F32)
        oml = C.alloc(2, F32)
        B_lb = S.buf("lb")
        if layer == 0:
            S.add("vector", lambda e: e.memset(lb, 0.0), writes=[B_lb])
        else:
            dl = C.alloc(2, F32)
            B_dl = S.buf("dl")
            for hd in range(2):
                S.add("vector", lambda e, hd=hd: e.tensor_tensor(out=dl[:, hd:hd + 1], in0=prm[:, 21 + 2 * hd:22 + 2 * hd],
                                                               in1=prm[:, 20 + 2 * hd:21 + 2 * hd], op=ALU.subtract),
                      reads=[B_prm], writes=[B_dl])
            S.add("scalar", lambda e: e.activation(out=lb, in_=dl, func=AF.Sigmoid), reads=[B_dl], writes=[B_lb])
        S.add("vector", lambda e: e.tensor_scalar(oml, lb, -1.0, 1.0, ALU.mult, ALU.add), reads=[B_lb], writes=[B_lb])
        hgroups = make_groups(TP)
        rb = C.reserve(2)
        po_b = [rb[0], rb[1]]
        kv_s = [[C.alloc(128, F32) for _ in range(8)] for _ in range(2)]
        B_kv = [[S.buf("kv") for _ in range(8)] for _ in range(2)]
        Sf = [[C.alloc(128, F32) for _ in range(2)] for _ in range(2)]
        B_Sf = [[S.buf("Sf") for _ in range(2)] for _ in range(2)]
        for hd in range(2):
            S.add("vector", lambda e, hd=hd: e.memset(Sf[hd][0], 0.0), writes=[B_Sf[hd][0]])
        kTz = [[[C.alloc(128, BF16) for _ in range(2)] for _ in range(4)] for _ in range(2)]
        B_kTz = [[[S.buf("kTz") for _ in range(2)] for _ in range(4)] for _ in range(2)]
        for hd in range(2):
            for tl in range(4):
                for cc in range(2):
                    S.add("gpsimd", lambda e, hd=hd, tl=tl, cc=cc: e.memset(kTz[hd][tl][cc], 0.0), writes=[B_kTz[hd][tl][cc]])
        vts = [[C.alloc(128, BF16) for _ in range(4)] for _ in range(2)]
        B_vts = [[S.buf("vt") for _ in range(4)] for _ in range(2)]
        Ats = [[C.alloc(128, BF16) for _ in range(4)] for _ in range(2)]
        B_Ats = [[S.buf("At") for _ in range(4)] for _ in range(2)]
        step = [0, 0]

        def phase_a2(gi, t0, w):
            nch = w // 64
            H = (0, 1)
            Xs = [{"nch": nch, "ntl": w // 128, "t0": t0, "w": w, "gi": gi} for _ in H]
            CARRY = ("qi", "q2", "k2", "kht", "gs")

            def T_(kind, hd, dt=F32):
                return C.tmp(f"{kind}{hd}", 512, dt, 2 if kind in CARRY else 1)

            def proj(br, kind, func):
                outs = []
                pss = []
                for hd in H:
                    ps, Bps = C.ps()
                    for kc in range(8):
                        S.add("tensor", lambda e, kc=kc, hd=hd, ps=ps: e.matmul(
                            ps[:, :w], whg[:, kc * 768 + br * 256 + hd * 128: kc * 768 + br * 256 + (hd + 1) * 128],
                            hn[:, kc * TP + t0: kc * TP + t0 + w], start=(kc == 0), stop=(kc == 7)),
                            reads=[B_whg] + hn_bufs(kc, t0, w), writes=[Bps])
                    pss.append((ps, Bps))
                for hd in H:
                    ps, Bps = pss[hd]
                    o, Bo = T_(kind, hd)
                    S.add("scalar", lambda e, o=o, ps=ps: e.activation(out=o[:, :w], in_=ps[:, :w], func=func), reads=[Bps], writes=[Bo])
                    outs.append((o, Bo))
                return outs

            def act(kind, src, func, scale=1.0, dt=F32, extra=()):
                outs = []
                for hd in H:
                    o, Bo = T_(kind, hd, dt)
                    a, Ba = src[hd]
                    S.add("scalar", lambda e, o=o, a=a: e.activation(out=o[:, :w], in_=a[:, :w], func=func, scale=scale),
                          reads=[Ba] + [x[hd][1] for x in extra], writes=[Bo])
                    outs.append((o, Bo))
                return outs

            def mul(kind, a_, b_, dt=F32):
                outs = []
                for hd in H:
                    o, Bo = T_(kind, hd, dt)
                    (a, Ba), (b, Bb) = a_[hd], b_[hd]
                    S.add("vector", lambda e, o=o, a=a, b=b: e.tensor_tensor(out=o[:, :w], in0=a[:, :w], in1=b[:, :w], op=ALU.mult),
                          reads=[Ba, Bb], writes=[Bo])
                    outs.append((o, Bo))
                return outs

            qs = proj(0, "qs", AF.Silu)
            gs = proj(2, "gs", AF.Silu)
            f = proj(1, "f", AF.Sigmoid)
            for hd in H:
                ff, Bf = f[hd]
                S.add("vector", lambda e, ff=ff, hd=hd: e.tensor_scalar(ff[:, :w], ff[:, :w], oml[:, hd:hd + 1], lb[:, hd:hd + 1], ALU.mult, ALU.add),
                      reads=[Bf, B_lb], writes=[Bf])
            lf = act("lf", f, AF.Ln)
            kh = []
            for hd in H:
                o, Bo = T_("kh", hd)
                ff, Bf = f[hd]
                S.add("vector", lambda e, o=o, ff=ff: e.tensor_scalar(o[:, :w], ff[:, :w], -1.0, 1.0, ALU.mult, ALU.add), reads=[Bf], writes=[Bo])
                kh.append((o, Bo))
            G = []
            for hd in H:
                o, Bo = T_("G", hd)
                l_, Bl = lf[hd]
                S.add("vector", lambda e, o=o, l_=l_: e.tensor_tensor_scan(out=o[:, :w], data0=scanmask[:, :w], data1=l_[:, :w], initial=0.0,
                                                                         op0=ALU.mult, op1=ALU.add), reads=[Bl, B_cst], writes=[Bo])
                G.append((o, Bo))
            G3 = [G[hd][0][:, :w].rearrange("p (c t) -> p c t", t=64) for hd in H]
            E = act("E", G, AF.Exp)
            qi = mul("qi", qs, E)
            scl = []
            for hd in H:
                o, Bo = C.tmp(f"scl{hd}", 8, F32, 2)
                e_, Be = E[hd]
                S.add("vector", lambda e, o=o, e_=e_: e.tensor_copy(out=o[:, :nch].rearrange("p (c o) -> p c o", o=1),
                                                                     in_=e_[:, :w].rearrange("p (c t) -> p c t", t=64)[:, :, 63:64]), reads=[Be], writes=[Bo])
                scl.append((o, Bo))
            Dm = []
            for hd in H:
                o, Bo = T_("Dm", hd)
                S.add("vector", lambda e, o=o, hd=hd: e.tensor_tensor(out=o[:, :w].rearrange("p (c t) -> p c t", t=64), in0=G3[hd],
                                                                      in1=G3[hd][:, :, 31:32].broadcast_to([128, nch, 64]), op=ALU.subtract),
                      reads=[G[hd][1]], writes=[Bo])
                Dm.append((o, Bo))
            E2 = act("E2", Dm, AF.Exp)
            q2 = mul("q2", qs, E2, BF16)
            E3 = act("E3", Dm, AF.Exp, scale=-1.0)
            k2 = mul("k2", kh, E3, BF16)
            Dl = []
            for hd in H:
                o, Bo = T_("Dl", hd)
                S.add("vector", lambda e, o=o, hd=hd: e.tensor_tensor(out=o[:, :w].rearrange("p (c t) -> p c t", t=64), in0=G3[hd],
                                                                      in1=G3[hd][:, :, 63:64].broadcast_to([128, nch, 64]), op=ALU.subtract),
                      reads=[G[hd][1]], writes=[Bo])
                Dl.append((o, Bo))
            E4 = act("E4", Dl, AF.Exp, scale=-1.0)
            kht = mul("kht", kh, E4, BF16)
            for hd in H:
                Xs[hd].update(dict(qi=qi[hd][0], Bqi=qi[hd][1], q2=q2[hd][0], Bq2=q2[hd][1], k2=k2[hd][0], Bk2=k2[hd][1],
                                   kht=kht[hd][0], Bkht=kht[hd][1], scl=scl[hd][0], Bscl=scl[hd][1], gs=gs[hd][0], Bgs=gs[hd][1]))
            return Xs

        def phase_b(hd, X):
            t0 = X["t0"]
            for tl in range(X["ntl"]):
                def tile(tl):
                    c0 = tl * 128
                    pv, Bpv = C.ps()
                    for kc in range(8):
                        S.add("tensor", lambda e, kc=kc: e.matmul(
                            pv[:, 0:128], hn[:, kc * TP + t0 + c0: kc * TP + t0 + c0 + 128], wi[:, kc * 256 + hd * 128: kc * 256 + (hd + 1) * 128],
                            start=(kc == 0), stop=(kc == 7)), reads=[B_wi] + hn_bufs(kc, t0 + c0, 128), writes=[Bpv])
                    vt, Bvt = vts[hd][tl], B_vts[hd][tl]
                    S.add("scalar", lambda e: e.activation(out=vt, in_=pv[:, 0:128], func=AF.Copy), reads=[Bpv], writes=[Bvt])
                    pk, Bpk = C.ps()
                    S.add("tensor", lambda e: e.matmul(pk[:, 0:128], X["kht"][:, c0:c0 + 128], ident_b, start=True, stop=True),
                          reads=[X["Bkht"], B_idb], writes=[Bpk])
                    for cc in range(2):
                        S.add("vector", lambda e, cc=cc: e.tensor_copy(out=kTz[hd][tl][cc][cc * 64:(cc + 1) * 64, :], in_=pk[cc * 64:(cc + 1) * 64, 0:128]),
                              reads=[Bpk], writes=[B_kTz[hd][tl][cc]])
                    pa, Bpa = C.ps()
                    S.add("tensor", lambda e: e.matmul(pa[:, 0:128], X["k2"][:, c0:c0 + 128], X["q2"][:, c0:c0 + 128], start=True, stop=True),
                          reads=[X["Bk2"], X["Bq2"]], writes=[Bpa])
                    S.add("vector", lambda e: e.tensor_tensor(out=Ats[hd][tl], in0=pa[:, 0:128], in1=hmask_b, op=ALU.mult),
                          reads=[Bpa, B_cst], writes=[B_Ats[hd][tl]])
                    for cc in range(2):
                        def kvprod(cc):
                            ch = tl * 2 + cc
                            pkv, Bpkv = C.ps()
                            S.add("tensor", lambda e: e.matmul(pkv[:, 0:128], kTz[hd][tl][cc], vt, start=True, stop=True),
                                  reads=[B_kTz[hd][tl][cc], Bvt], writes=[Bpkv])
                            S.add("scalar", lambda e: e.activation(out=kv_s[hd][ch], in_=pkv[:, 0:128], func=AF.Copy), reads=[Bpkv], writes=[B_kv[hd][ch]])
                        kvprod(cc)
                tile(tl)

        def chain_step(hd, X, ch):
            po, Bpo = po_b[hd]
            cs = ch * 64
            i = step[hd] % 2
            Sc, BSc = Sf[hd][i], B_Sf[hd][i]
            Sn, BSn = Sf[hd][1 - i], B_Sf[hd][1 - i]
            step[hd] += 1
            if HG_BF16_STATE:
                Sbb, BSbb = C.tmp(f"Sbb{hd}", 128, BF16, 2)
                qib, Bqib = C.tmp(f"qib{hd}", 64, BF16, 2)
                S.add("scalar", lambda e: e.activation(out=Sbb, in_=Sc, func=AF.Copy), reads=[BSc], writes=[BSbb])
                S.add("gpsimd", lambda e: e.tensor_copy(out=qib, in_=X["qi"][:, cs:cs + 64]), reads=[X["Bqi"]], writes=[Bqib])
                S.add("tensor", lambda e: e.matmul(po[:, cs:cs + 64], Sbb, qib, start=(cs == 0), stop=False, skip_group_check=True),
                      reads=[BSbb, Bqib], writes=[Bpo])
            else:
                S.add("tensor", lambda e: e.matmul(po[:, cs:cs + 64], Sc, X["qi"][:, cs:cs + 64], start=(cs == 0), stop=False, skip_group_check=True),
                      reads=[BSc, X["Bqi"]], writes=[Bpo])
            S.add("vector", lambda e: e.scalar_tensor_tensor(out=Sn, in0=Sc, scalar=X["scl"][:, ch:ch + 1], in1=kv_s[hd][ch],
                                                             op0=ALU.mult, op1=ALU.add), reads=[BSc, X["Bscl"], B_kv[hd][ch]], writes=[BSn])
            if ch % 2 == 1:
                tl = ch // 2
                c0 = tl * 128
                S.add("tensor", lambda e: e.matmul(po[:, c0:c0 + 128], vts[hd][tl], Ats[hd][tl], start=False, stop=True, skip_group_check=True),
                      reads=[B_vts[hd][tl], B_Ats[hd][tl]], writes=[Bpo])

        def phase_d(hd, X):
            t0, w = X["t0"], X["w"]
            po, Bpo = po_b[hd]
            wv_ = min(w, T - t0)
            if wv_ <= 0:
                return
            osb, Bosb = C.tmp(f"osb{hd}", 512, F32, 1)
            S.add("scalar", lambda e: e.activation(out=osb[:, :w], in_=po[:, :w], func=AF.Copy), reads=[Bpo], writes=[Bosb])
            sq, Bsq = C.tmp("sq", 512, BF16, 2)
            S.add("scalar", lambda e: e.activation(out=sq[:, :w], in_=osb[:, :w], func=AF.Square), reads=[Bosb], writes=[Bsq])
            pm, Bpm = C.ps()
            S.add("tensor", lambda e: e.matmul(pm[:, :w], onesd, sq[:, :w], start=True, stop=True), reads=[Bsq, B_onesd], writes=[Bpm])
            rs, Brs = C.tmp("rs", 512, F32, 2)
            S.add("scalar", lambda e: e.activation(out=rs[:, :w], in_=pm[:, :w], func=AF.Ln, bias=K["eps"], scale=1.0),
                  reads=[Bpm, K["B_eps"]], writes=[Brs])
            S.add("scalar", lambda e: e.activation(out=rs[:, :w], in_=rs[:, :w], func=AF.Exp, scale=-0.5), reads=[Brs], writes=[Brs])
            S.add("vector", lambda e: e.scalar_tensor_tensor(out=osb[:, :w], in0=osb[:, :w], scalar=prm[:, 11:12], in1=rs[:, :w],
                                                             op0=ALU.mult, op1=ALU.mult), reads=[Bosb, Brs, B_prm], writes=[Bosb])
            uo, Buo = C.tmp("uoh", 512, BF16, 2)
            S.add("vector", lambda e: e.tensor_tensor(out=uo[:, :w], in0=osb[:, :w], in1=X["gs"][:, :w], op=ALU.mult),
                  reads=[Bosb, X["Bgs"]], writes=[Buo])
            S.add("sync", lambda e: e.dma_start(out=dr["o_hgrn"][hd][:, t0:t0 + wv_], in_=uo[:, :wv_]), reads=[Buo], dma_key="out")

        Xn = phase_a2(0, hgroups[0][0], hgroups[0][1])
        for gi, (t0, w) in enumerate(hgroups):
            Xs = Xn
            if gi + 1 < len(hgroups):
                Xn = phase_a2(gi + 1, hgroups[gi + 1][0], hgroups[gi + 1][1])
            for hd in range(2):
                phase_b(hd, Xs[hd])
            for ch in range(Xs[0]["nch"]):
                for hd in range(2):
                    chain_step(hd, Xs[hd], ch)
            for hd in range(2):
                phase_d(hd, Xs[hd])
        C.release()
        S.fence()
        C.reset(m0)
        C.drop_tmps()

    if "att" in parts:
        emit_attention(C, K, dr, hn, hn_bufs, prm, B_prm, cst, B_cst, dr["rscr_t"], lam_init, groups,
                       dict(ones1=ones1, B_ones1=B_ones1, onesd=onesd, B_onesd=B_onesd, blk64=blk64, B_blk=B_blk))
    S.fence(new_epoch=True)
    C.reset(m_base)
    C.drop_tmps()


def emit_attention(C, K, dr, hn, hn_bufs, prm, B_prm, cst, B_cst, rscr_t, lam_init, groups, X):
    S = C.S
    T = T_ALL
    ones1, B_ones1, onesd, B_onesd, blk64, B_blk = X["ones1"], X["B_ones1"], X["onesd"], X["B_onesd"], X["blk64"], X["B_blk"]
    J_f = cst[:, 128:256]
    qn = C.alloc(4 * TP, BF16)
    kn = C.alloc(2 * TP, BF16)
    v_sb = C.alloc(NT * 256, BF16)
    bt_tiles = {(dl, hd): C.alloc(512, F32) for dl in (1, 0, -1, -2, -3) for hd in range(2)}
    m_setup = None

    lt = C.alloc(128, F32)
    ssum = C.alloc(2, F32)
    nlam = C.alloc(1, F32)
    lnc = C.alloc(1, F32)
    B_l = S.buf("lam")
    B_nlam = S.buf("nlam")
    m_setup = C.mark()
    wqk = C.alloc(8 * 512, BF16)
    B_wqk = S.buf("wqk")
    S.add("gpsimd", lambda e: e.dma_start(out=wqk.rearrange("p (k n) -> p k n", k=8), in_=kview(dr["wqk"])), writes=[B_wqk], dma_key="wqk")
    wv = C.alloc(8 * 256, BF16)
    B_wv = S.buf("wv")
    S.add("gpsimd", lambda e: e.dma_start(out=wv.rearrange("p (k n) -> p k n", k=8), in_=kview(dr["wv"])), writes=[B_wv], dma_key="wv")
    S.add("vector", lambda e: e.tensor_tensor(out=lt[:, 0:64], in0=prm[:, 24:88], in1=prm[:, 88:152], op=ALU.mult), reads=[B_prm], writes=[B_l])
    S.add("vector", lambda e: e.tensor_tensor(out=lt[:, 64:128], in0=prm[:, 152:216], in1=prm[:, 216:280], op=ALU.mult), reads=[B_prm], writes=[B_l])
    S.add("vector", lambda e: e.tensor_reduce(out=ssum[:, 0:1], in_=lt[:, 0:64], axis=AX.X, op=ALU.add), reads=[B_l], writes=[B_l])
    S.add("vector", lambda e: e.tensor_reduce(out=ssum[:, 1:2], in_=lt[:, 64:128], axis=AX.X, op=ALU.add), reads=[B_l], writes=[B_l])
    S.add("scalar", lambda e: e.activation(out=ssum, in_=ssum, func=AF.Exp), reads=[B_l], writes=[B_l])
    S.add("vector", lambda e: e.tensor_tensor(out=nlam, in0=ssum[:, 1:2], in1=ssum[:, 0:1], op=ALU.subtract), reads=[B_l], writes=[B_nlam])
    S.add("vector", lambda e: e.tensor_scalar(nlam, nlam, -lam_init, None, ALU.add), reads=[B_nlam], writes=[B_nlam])
    S.add("vector", lambda e: e.memset(lnc, math.log(1.0 - lam_init)), writes=[B_nlam])

    relb = C.alloc(2, F32)
    B_relb = S.buf("relb")
    S.add("vector", lambda e: e.memset(relb[32:33, :], 1.0), writes=[B_relb])
    S.add("sync", lambda e: e.dma_start(out=relb[0:32, :], in_=dr["relb"]), writes=[B_relb], dma_key="relb")
    oh = C.alloc(RL, F32)
    B_oh = S.buf("oh")
    S.add("sync", lambda e: e.dma_start(out=oh[0:33, :], in_=dr["oh"]), writes=[B_oh], dma_key="oh")
    rsb = C.alloc(RL, F32)
    B_rsb = S.buf("rsb")
    for cc in range(3):
        def rchunk(cc):
            ps, Bps = C.ps()
            S.add("tensor", lambda e: e.matmul(ps[0:2, 0:384], relb[0:33, :], oh[0:33, cc * 384:(cc + 1) * 384], start=True, stop=True),
                  reads=[B_relb, B_oh], writes=[Bps])
            S.add("vector", lambda e: e.tensor_copy(out=rsb[0:2, cc * 384:(cc + 1) * 384], in_=ps[0:2, 0:384]), reads=[Bps], writes=[B_rsb])
        rchunk(cc)
    B_rscr = S.buf("rscr")
    S.add("sync", lambda e: e.dma_start(out=dr["rscr"], in_=rsb[0:2, :]), reads=[B_rsb], writes=[B_rscr], dma_key="rscr")
    Bt = {}
    B_Bt = S.buf("Bt")
    for dl in (1, 0, -1, -2, -3):
        for hd in range(2):
            def mk(dl, hd):
                Hs, BHs = C.tmp("Hs", 512, F32, 1)
                src = bass.AP(rscr_t, hd * RL + 128 * dl + 384, [[1, 128], [1, 512]])
                S.add("sync", lambda e: e.dma_start(out=Hs, in_=src), reads=[B_rscr], writes=[BHs], dma_key="hank")
                ps, Bps = C.ps()
                S.add("tensor", lambda e: e.matmul(ps[:, :], J_f, Hs, start=True, stop=True), reads=[BHs, B_cst], writes=[Bps])
                bt = bt_tiles[(dl, hd)]
                S.add("scalar", lambda e: e.activation(out=bt, in_=ps[:, :], func=AF.Copy), reads=[Bps], writes=[B_Bt])
                Bt[(dl, hd)] = bt
            mk(dl, hd)

    B_qz = S.buf("qz")
    S.add("gpsimd", lambda e: e.memset(qn, 0.0), writes=[B_qz])
    hgroups = make_groups(TP)
    B_qn = [[S.buf("qn") for _ in hgroups] for _ in range(2)]
    B_kn = [[S.buf("kn") for _ in hgroups] for _ in range(2)]
    B_v = [S.buf("v") for _ in range(NT)]

    def qk_stage1(gi, t0, w, x, hd):
        ps, Bps = C.ps()
        col = x * 256 + hd * 128
        for kc in range(8):
            S.add("tensor", lambda e, kc=kc: e.matmul(ps[:, :w], wqk[:, kc * 512 + col: kc * 512 + col + 128],
                                                       hn[:, kc * TP + t0: kc * TP + t0 + w], start=(kc == 0), stop=(kc == 7)),
                  reads=[B_wqk] + hn_bufs(kc, t0, w), writes=[Bps])
        sq, Bsq = C.tmp("sqq", 512, BF16, 4)
        S.add("scalar", lambda e: e.activation(out=sq[:, :w], in_=ps[:, :w], func=AF.Square), reads=[Bps], writes=[Bsq])
        return (gi, t0, w, x, hd, ps, Bps, sq, Bsq)

    def qk_stage2(ctx):
        gi, t0, w, x, hd, ps, Bps, sq, Bsq = ctx
        pm, Bpm = C.ps()
        S.add("tensor", lambda e: e.matmul(pm[:, :w], blk64, sq[:, :w], start=True, stop=True), reads=[Bsq, B_blk], writes=[Bpm])
        rs, Brs = C.tmp("rs", 512, F32, 2)
        S.add("scalar", lambda e: e.activation(out=rs[:, :w], in_=pm[:, :w], func=AF.Ln, bias=K["eps"], scale=1.0),
              reads=[Bpm, K["B_eps"]], writes=[Brs])
        S.add("scalar", lambda e: e.activation(out=rs[:, :w], in_=rs[:, :w], func=AF.Exp, scale=-0.5), reads=[Brs], writes=[Brs])
        if x == 1:
            S.add("vector", lambda e: e.scalar_tensor_tensor(out=kn[:, hd * TP + t0: hd * TP + t0 + w], in0=ps[:, :w], scalar=prm[:, 9:10],
                                                             in1=rs[:, :w], op0=ALU.mult, op1=ALU.mult), reads=[Bps, Brs, B_prm], writes=[B_kn[hd][gi]])
        else:
            for m in range(2):
                def qwrite(m):
                    r0, r1 = m * 64, (m + 1) * 64
                    o0 = (hd * 2 + m) * TP + t0
                    S.add("vector", lambda e: e.scalar_tensor_tensor(out=qn[r0:r1, o0:o0 + w], in0=ps[r0:r1, :w], scalar=prm[r0:r1, 8:9],
                                                                     in1=rs[r0:r1, :w], op0=ALU.mult, op1=ALU.mult),
                          reads=[Bps, Brs, B_prm, B_qz], writes=[B_qn[hd][gi]])
                qwrite(m)

    pend = []
    for gi, (t0, w) in enumerate(hgroups):
        for x in range(2):
            for hd in range(2):
                pend.append(qk_stage1(gi, t0, w, x, hd))
                if len(pend) > 2:
                    qk_stage2(pend.pop(0))
    for ctx in pend:
        qk_stage2(ctx)

    def v_tile(tt):
        ps, Bps = C.ps()
        for kc in range(8):
            S.add("tensor", lambda e, kc=kc: e.matmul(ps[:, 0:256], hn[:, kc * TP + tt * 128: kc * TP + (tt + 1) * 128], wv[:, kc * 256:(kc + 1) * 256],
                                                       start=(kc == 0), stop=(kc == 7)), reads=[B_wv] + hn_bufs(kc, tt * 128, 128), writes=[Bps])
        S.add("vector", lambda e: e.tensor_copy(out=v_sb[:, tt * 256:(tt + 1) * 256], in_=ps[:, 0:256]), reads=[Bps], writes=[B_v[tt]])

    for tt in range(NT):
        v_tile(tt)

    S.fence()
    C.reset(m_setup)
    C.drop_tmps()
    acc = C.reserve(4)

    def att_group(hd, g, t0, w):
        nk = min(4 * g + 4, NT)
        (O1, BO1), (O2, BO2), (s1, Bs1), (s2, Bs2) = acc
        Os = ((O1, BO1), (O2, BO2))
        ss = ((s1, Bs1), (s2, Bs2))

        def s_unit(j, m):
            dl = 4 * g - j
            ps, Bps = C.ps()
            S.add("tensor", lambda e: e.matmul(ps[:, :w], kn[:, hd * TP + j * 128: hd * TP + (j + 1) * 128],
                                               qn[:, (hd * 2 + m) * TP + t0: (hd * 2 + m) * TP + t0 + w], start=True, stop=True),
                  reads=[B_kn[hd][j // 4], B_qn[hd][g]], writes=[Bps])
            P, BP = C.tmp("P", 512, BF16, 6)
            if dl >= 2:
                S.add("scalar", lambda e: e.activation(out=P[:, :w], in_=ps[:, :w], func=AF.Exp, bias=prm[:, 12 + hd:13 + hd], scale=0.125),
                      reads=[Bps, B_prm], writes=[BP])
            else:
                nb, Bnb = C.tmp("nb", 512, F32, 2)
                bt = Bt[(dl, hd)]
                S.add("vector", lambda e: e.scalar_tensor_tensor(out=nb[:, :w], in0=ps[:, :w], scalar=0.125, in1=bt[:, :w],
                                                                 op0=ALU.mult, op1=ALU.add), reads=[Bps, B_Bt], writes=[Bnb])
                S.add("scalar", lambda e: e.activation(out=P[:, :w], in_=nb[:, :w], func=AF.Exp), reads=[Bnb], writes=[BP])
            return P, BP

        def pv_unit(j, m, P, BP):
            Om, BOm = Os[m]
            sm, Bsm = ss[m]
            S.add("tensor", lambda e: e.matmul(Om[:, :w], v_sb[:, j * 256 + hd * 128: j * 256 + (hd + 1) * 128], P[:, :w],
                                               start=(j == 0), stop=(j == nk - 1)), reads=[B_v[j], BP], writes=[BOm])
            S.add("tensor", lambda e: e.matmul(sm[:, :w], ones1, P[:, :w], start=(j == 0), stop=(j == nk - 1)),
                  reads=[B_ones1, BP], writes=[Bsm])

        LA = 3
        pend = []
        for j in range(nk):
            for m in range(2):
                P, BP = s_unit(j, m)
                pend.append((j, m, P, BP))
                if len(pend) > LA:
                    pv_unit(*pend.pop(0))
        for u in pend:
            pv_unit(*u)
        r1, Br1 = C.tmp("r1", 512, F32, 1)
        r2, Br2 = C.tmp("r2", 512, F32, 1)
        S.add("vector", lambda e: e.reciprocal(out=r1[:, :w], in_=s1[:, :w]), reads=[Bs1], writes=[Br1])
        S.add("vector", lambda e: e.reciprocal(out=r2[:, :w], in_=s2[:, :w]), reads=[Bs2], writes=[Br2])
        a1, Ba1 = C.tmp("a1", 512, F32, 1)
        a2, Ba2 = C.tmp("a2", 512, F32, 1)
        S.add("vector", lambda e: e.tensor_tensor(out=a1[:, :w], in0=O1[:, :w], in1=r1[:, :w], op=ALU.mult), reads=[BO1, Br1], writes=[Ba1])
        S.add("vector", lambda e: e.tensor_tensor(out=a2[:, :w], in0=O2[:, :w], in1=r2[:, :w], op=ALU.mult), reads=[BO2, Br2], writes=[Ba2])
        S.add("vector", lambda e: e.scalar_tensor_tensor(out=a1[:, :w], in0=a2[:, :w], scalar=nlam[:, 0:1], in1=a1[:, :w], op0=ALU.mult, op1=ALU.add),
              reads=[Ba1, Ba2, B_nlam], writes=[Ba1])
        sq, Bsq = C.tmp("sq", 512, BF16, 2)
        S.add("scalar", lambda e: e.activation(out=sq[:, :w], in_=a1[:, :w], func=AF.Square), reads=[Ba1], writes=[Bsq])
        pm, Bpm = C.ps()
        S.add("tensor", lambda e: e.matmul(pm[:, :w], onesd, sq[:, :w], start=True, stop=True), reads=[Bsq, B_onesd], writes=[Bpm])
        rs, Brs = C.tmp("rs", 512, F32, 2)
        S.add("scalar", lambda e: e.activation(out=rs[:, :w], in_=pm[:, :w], func=AF.Ln, bias=K["eps"], scale=1.0),
              reads=[Bpm, K["B_eps"]], writes=[Brs])
        S.add("scalar", lambda e: e.activation(out=rs[:, :w], in_=rs[:, :w], func=AF.Exp, bias=lnc, scale=-0.5), reads=[Brs, B_nlam], writes=[Brs])
        uo, Buo = C.tmp("uoa", 512, BF16, 2)
        S.add("vector", lambda e: e.scalar_tensor_tensor(out=uo[:, :w], in0=a1[:, :w], scalar=prm[:, 10:11], in1=rs[:, :w], op0=ALU.mult, op1=ALU.mult),
              reads=[Ba1, Brs, B_prm], writes=[Buo])
        S.add("sync", lambda e: e.dma_start(out=dr["o_att"][hd][:, t0:t0 + w], in_=uo[:, :w]), reads=[Buo], dma_key="out")

    for hd in range(2):
        for g, (t0, w) in enumerate(groups):
            att_group(hd, g, t0, w)
    C.release()


def _rel_bucket_np(n):
    n = np.maximum(n, 0)
    nf = np.maximum(n, 1).astype(np.float32)
    large = 16 + (np.log(nf / np.float32(16)) / np.float32(math.log(128 / 16)) * np.float32(16)).astype(np.int32)
    large = np.minimum(large, 31)
    return np.where(n < 16, n, large)


_CONST_CACHE = {}


def phase1_consts():
    if "c" in _CONST_CACHE:
        return _CONST_CACHE["c"]
    cst = np.zeros((128, 896), np.float32)
    cst[:, 0:128] = np.eye(128, dtype=np.float32)
    cst[:, 128:256] = np.eye(128, dtype=np.float32)[::-1]
    s_i = np.arange(128)[:, None]
    t_i = np.arange(128)[None, :]
    cst[:, 256:384] = ((s_i // 64 == t_i // 64) & (s_i <= t_i)).astype(np.float32)
    cst[:, 384:896] = (np.arange(512) % 64 != 0).astype(np.float32)[None, :]
    oh = np.zeros((33, RL), np.float32)
    n = np.arange(RL) - 511
    bk = _rel_bucket_np(n)
    for i in range(RL):
        if n[i] >= 0:
            oh[bk[i], i] = 1.0
        else:
            oh[32, i] = -30000.0
    _CONST_CACHE["c"] = (cst, oh)
    return cst, oh


def phase1_inputs(d, L, h, hsT):
    w_in = d["w_in"][L]
    prm = np.zeros((128, 280), np.float32)
    prm[:, 0:8] = d["norm1_gain"][L].reshape(8, 128).T
    prm[:, 8] = np.tile(d["q_norm_gain"][L], 2)
    prm[:, 9] = np.tile(d["k_norm_gain"][L], 2)
    prm[:, 10] = d["attn_sub_gain"][L]
    prm[:, 11] = d["hgrn_out_gain"][L]
    for hd in range(2):
        prm[:, 12 + hd] = d["rel_bias"][31, 2 * h + hd]
        for lp in range(2):
            prm[:, 20 + hd * 2 + lp] = d["hgrn_lb_logits"][lp, (2 * h + hd) * 128:(2 * h + hd + 1) * 128]
    for ci in range(2):
        for k in range(3):
            prm[:, 14 + ci * 3 + k] = d["conv_w"][L][k, 256 * h + ci * 128: 256 * h + (ci + 1) * 128]
    prm[:, 24:280] = d["diff_lambda"][L].reshape(1, 256)
    cst, oh = phase1_consts()
    qk_cols = []
    for base in (0, 512):
        for hd in range(2):
            for m in range(2):
                c0 = base + m * 256 + (2 * h + hd) * 64
                qk_cols.append(np.arange(c0, c0 + 64))
    qk_cols = np.concatenate(qk_cols)
    sl = lambda base: w_in[:, base + 256 * h: base + 256 * (h + 1)]
    return {
        "hsT": hsT, "params": prm, "consts": cst, "oh": oh,
        "relb": np.ascontiguousarray(d["rel_bias"][:, 2 * h:2 * h + 2]),
        "wqk": np.ascontiguousarray(w_in[:, qk_cols]),
        "wv": np.ascontiguousarray(sl(1024)),
        "wconv": np.ascontiguousarray(np.concatenate([sl(1536), sl(2048), sl(2560)], axis=1)),
        "whg": np.ascontiguousarray(np.concatenate([sl(3072), sl(3584), sl(4608)], axis=1)),
        "wi": np.ascontiguousarray(sl(4096)),
    }


_NC_CACHE = {}


def _get_nc(key, fn):
    if key not in _NC_CACHE:
        _NC_CACHE[key] = fn()
    return _NC_CACHE[key]


def _lay_gain(g):
    return np.ascontiguousarray(g.reshape(8, 128).T)


P1_W = (("wqk", 512), ("wv", 256), ("wconv", 768), ("whg", 768), ("wi", 256))


def build_fused():
    T = T_ALL
    nc = bass.Bass("TRN2", target_bir_lowering=False)
    inp = lambda name, shape, dt=F32: nc.dram_tensor(name, shape, dt, kind="ExternalInput").ap()
    g = {}
    g["hsT"] = inp("hsT", [D, T])
    g["consts"] = inp("consts", [128, 896])
    g["oh"] = inp("oh", [33, RL])
    g["selv"] = inp("selv", [128, 2])
    for h in range(2):
        g[f"relb_{h}"] = inp(f"relb_{h}", [32, 2])
    for L in range(2):
        for h in range(2):
            g[f"params_{L}_{h}"] = inp(f"params_{L}_{h}", [128, 280])
            for nm, wd in P1_W:
                g[f"{nm}_{L}_{h}"] = inp(f"{nm}_{L}_{h}", [D, wd])
        g[f"wgate_{L}"] = inp(f"wgate_{L}", [D, 3072])
        g[f"wbr_{L}"] = inp(f"wbr_{L}", [1536, D])
        g[f"wout_{L}"] = inp(f"wout_{L}", [D, D])
        g[f"gains_{L}"] = inp(f"gains_{L}", [128, 16])
    g["fg0"] = inp("fg0", [1, D, 2816])
    g["fu0"] = inp("fu0", [1, D, 2816])
    g["fd0"] = inp("fd0", [1, 2816, D])
    g["fg1"] = inp("fg1", [8, D, 3584])
    g["fu1"] = inp("fu1", [8, D, 3584])
    g["fd1"] = inp("fd1", [8, 3584, D])
    g["wr"] = inp("wr", [D, 8])
    g["sel"] = inp("sel", [8, 1024])
    g["ident"] = inp("ident", [128, 128])
    out = nc.dram_tensor("out", [D, 2048], F32, kind="ExternalOutput").ap()
    rscr_t = nc.dram_tensor("rscr", [2, RL], F32, kind="Internal")
    ikind = "ExternalOutput" if FDEBUG else "Internal"
    u_scr = [nc.dram_tensor(f"u_scr{L}", [1536, T], BF16, kind=ikind).ap() for L in range(2)]
    hs1 = nc.dram_tensor("hs1", [D, T], F32, kind=ikind).ap()

    with contextlib.ExitStack() as st:
        C = Ctx(nc, st)
        K = emit_consts(C)
        for L in range(2):
            hs_src = g["hsT"] if L == 0 else hs1
            for h in range(2):
                dr = {"hsT": hs_src, "params": g[f"params_{L}_{h}"], "consts": g["consts"], "oh": g["oh"], "relb": g[f"relb_{h}"],
                      "rscr": rscr_t.ap(), "rscr_t": rscr_t, "reuse_hn": (h == 1)}
                for nm, _ in P1_W:
                    dr[nm] = g[f"{nm}_{L}_{h}"]
                u = u_scr[L]
                dr["o_att"] = [u[256 * h + hd * 128: 256 * h + (hd + 1) * 128, :] for hd in range(2)]
                dr["o_conv"] = [u[512 + 256 * h + ci * 128: 512 + 256 * h + (ci + 1) * 128, :] for ci in range(2)]
                dr["o_hgrn"] = [u[1024 + 256 * h + hd * 128: 1024 + 256 * h + (hd + 1) * 128, :] for hd in range(2)]
                emit_phase1(C, K, dr, L)
            base = {"wgate": g[f"wgate_{L}"], "wbr": g[f"wbr_{L}"], "wout": g[f"wout_{L}"], "gains": g[f"gains_{L}"]}
            if L == 0:
                for (a0, a1) in ((0, 2064), (2064, 4112)):
                    dr = dict(base)
                    dr.update({"hsT": g["hsT"][:, a0:a1], "uT": u_scr[0][:, a0:a1], "out": hs1[:, a0:a1],
                               "fg": g["fg0"], "fu": g["fu0"], "fd": g["fd0"]})
                    emit_phase2(C, K, dr, a1 - a0, False)
            else:
                dr = dict(base)
                dr.update({"hsT": (hs1[:, 16:2064], hs1[:, 2064:4112]), "uT": (u_scr[1][:, 16:2064], u_scr[1][:, 2064:4112]),
                           "out": out, "selv": g["selv"], "fg": g["fg1"], "fu": g["fu1"], "fd": g["fd1"],
                           "wr": g["wr"], "sel": g["sel"], "ident": g["ident"]})
                emit_phase2(C, K, dr, 2048, True)
        C.S.finalize(final_dma_keys=["out"])
        print("fused ops", len(C.S.ops), "sig", C.S.max_counts, "dma", max(C.S.dma_counts.values()))
    return nc


def kernel(**inputs):
    d = {k: np.asarray(v) for k, v in inputs.items()}
    x = d["x"].astype(np.float32, copy=False)
    B = x.shape[0]
    cores = list(range(8))
    nc = _get_nc("fused", build_fused)
    hsT = [np.ascontiguousarray(np.concatenate([d["meta_tokens"].astype(np.float32), x[b]], axis=0).T) for b in range(B)]
    sel = np.zeros((8, 1024), np.float32)
    for e in range(8):
        sel[e, e * 128:(e + 1) * 128] = 1.0
    cst, oh = phase1_consts()
    common = {"consts": cst, "oh": oh, "sel": sel, "ident": np.eye(128, dtype=np.float32),
              "fg0": d["ffn_w_gate"][0:1], "fu0": d["ffn_w_up"][0:1], "fd0": d["ffn_w_down"][0:1],
              "fg1": d["moe_w_gate"][0], "fu1": d["moe_w_up"][0], "fd1": d["moe_w_down"][0],
              "wr": np.ascontiguousarray(d["router_w"][0])}
    for L in range(2):
        for h in range(2):
            pi = phase1_inputs(d, L, h, None)
            common[f"params_{L}_{h}"] = pi["params"]
            common[f"relb_{h}"] = pi["relb"]
            for nm, _ in P1_W:
                common[f"{nm}_{L}_{h}"] = pi[nm]
        common[f"wgate_{L}"] = np.ascontiguousarray(d["w_in"][L][:, 5120:])
        common[f"wbr_{L}"] = np.ascontiguousarray(d["w_branch"][L].reshape(1536, 1024))
        common[f"wout_{L}"] = np.ascontiguousarray(d["w_out"][L])
        common[f"gains_{L}"] = np.concatenate([_lay_gain(d["norm1_gain"][L]), _lay_gain(d["norm2_gain"][L])], axis=1)
    ins = []
    for c in cores:
        b, h = c // 2, c % 2
        m = dict(common)
        m["hsT"] = hsT[b]
        selv = np.zeros((128, 2), np.float32)
        selv[:, 0] = 1.0 - h
        selv[:, 1] = float(h)
        m["selv"] = selv
        ins.append(m)
    if FDEBUG:
        return run_bass_kernel_spmd(nc, ins, core_ids=cores)
    res = run_bass_kernel_spmd(nc, ins, core_ids=cores)
    out = np.stack([np.concatenate([res.results[2 * b]["out"], res.results[2 * b + 1]["out"]], axis=1).T for b in range(B)])
    return np.ascontiguousarray(out.astype(np.float32))
```
